# Optimizing a Trainium2 kernel written in Bass

```python
import jax, jax.numpy as jnp
from jax import lax
import numpy as np

D_MODEL = 1024
BATCH = 2
SEQ = 8192
DEPTH = 2

W_SC = 1024
SC_KERNEL = 3
W_CF = 1024
CF_KERNEL = 31
W_POOL = 1024
POOL_WINDOWS = (2, 4, 8, 16)
N_POOL_GROUPS = len(POOL_WINDOWS)
POOL_GROUP = W_POOL // N_POOL_GROUPS
N_BRANCH = 3
D_FF = 3584
N_EXPERTS = 8
TOP_K = 2
N_DENSE = (DEPTH + 1) // 2
N_MOE = DEPTH // 2
RMS_EPS = 1e-6
LN_EPS = 1e-5
IN_SPLITS = [W_SC, 2 * W_SC, 3 * W_SC, 3 * W_SC + W_CF, 3 * W_SC + 2 * W_CF,
             3 * W_SC + 2 * W_CF + W_POOL]
IN_COLS = 3 * W_SC + 2 * W_CF + W_POOL + N_BRANCH * D_MODEL

kernel_name = "hybrid_conv_pool_moe_trunk"


def rms_norm(x, g):
    xf = x.astype(jnp.float32)
    y = xf * lax.rsqrt(jnp.mean(xf * xf, axis=-1, keepdims=True) + RMS_EPS)
    return (y * g.astype(jnp.float32)).astype(x.dtype)


def layer_norm(x, g, b):
    xf = x.astype(jnp.float32)
    mu = jnp.mean(xf, axis=-1, keepdims=True)
    xc = xf - mu
    var = jnp.mean(xc * xc, axis=-1, keepdims=True)
    y = xc * lax.rsqrt(var + LN_EPS) * g.astype(jnp.float32) + b.astype(jnp.float32)
    return y.astype(x.dtype)


def causal_depthwise_conv(u, w):
    k, c = w.shape
    return lax.conv_general_dilated(
        u, w[:, None, :].astype(u.dtype), window_strides=(1,), padding=[(k - 1, 0)],
        dimension_numbers=('NWC', 'WIO', 'NWC'), feature_group_count=c)


def multiscale_causal_pool(u):
    uf = u.astype(jnp.float32)
    s = uf.shape[1]
    max_w = max(POOL_WINDOWS)
    cs = jnp.cumsum(uf, axis=1)
    cs_pad = jnp.pad(cs, ((0, 0), (max_w, 0), (0, 0)))
    pos = jnp.arange(s, dtype=jnp.float32)[None, :, None]
    outs = []
    for gi, w in enumerate(POOL_WINDOWS):
        lo, hi = gi * POOL_GROUP, (gi + 1) * POOL_GROUP
        win_sum = cs[:, :, lo:hi] - cs_pad[:, max_w - w:max_w - w + s, lo:hi]
        count = jnp.minimum(pos + 1.0, float(w))
        outs.append(win_sum / count - uf[:, :, lo:hi])
    return jnp.stack(outs, axis=2).astype(u.dtype)


def hybrid_mixer(h, w_in, sc_conv_w, cf_conv_w, cf_conv_b, cf_ln_g, cf_ln_b,
                 pool_w, pool_scale, w_sc_out, w_cf_out, w_pool_out, w_o):
    b, s, _ = h.shape
    proj = jnp.einsum('bsd,dc->bsc', h, w_in)
    sc_b, sc_c, sc_x, cf_v, cf_g, pool_u, gates = jnp.split(proj, IN_SPLITS, axis=-1)

    y_a = sc_b * causal_depthwise_conv(sc_c * sc_x, sc_conv_w)
    y_a = jnp.einsum('bsc,cd->bsd', y_a, w_sc_out)

    v = cf_v * jax.nn.sigmoid(cf_g)
    v = causal_depthwise_conv(v, cf_conv_w) + cf_conv_b.astype(v.dtype)
    v = jax.nn.silu(layer_norm(v, cf_ln_g, cf_ln_b))
    y_b = jnp.einsum('bsc,cd->bsd', v, w_cf_out)

    p = multiscale_causal_pool(pool_u)
    p = jnp.einsum('bsgc,gce->bsge', p, pool_w).reshape(b, s, W_POOL) * pool_scale.astype(p.dtype)
    y_c = jnp.einsum('bsc,cd->bsd', p, w_pool_out)

    g = jax.nn.sigmoid(gates.reshape(b, s, N_BRANCH, D_MODEL))
    merged = g[:, :, 0] * y_a + g[:, :, 1] * y_b + g[:, :, 2] * y_c
    return jnp.einsum('bsd,de->bse', merged, w_o)


def swiglu(h, w1, w3, w2):
    a = jnp.einsum('bsd,df->bsf', h, w1)
    c = jnp.einsum('bsd,df->bsf', h, w3)
    return jnp.einsum('bsf,fd->bsd', jax.nn.silu(a) * c, w2)


def moe_swiglu(h, router_w, w1, w3, w2):
    logits = jnp.einsum('bsd,de->bse', h, router_w).astype(jnp.float32)
    top_v, top_i = lax.top_k(logits, TOP_K)
    probs = jax.nn.softmax(top_v, axis=-1)
    combine = jnp.sum(jax.nn.one_hot(top_i, N_EXPERTS, dtype=jnp.float32) * probs[..., None], axis=-2)
    combine = combine.astype(h.dtype)
    out = jnp.zeros_like(h)
    for e in range(N_EXPERTS):
        out = out + combine[..., e:e + 1] * swiglu(h, w1[e], w3[e], w2[e])
    return out


def setup_inputs(seed: int = 0) -> dict:
    key = jax.random.key(seed)
    ks = jax.random.split(key, 24)

    def nrm(k, shape, scale):
        return jax.random.normal(k, shape, dtype=jnp.float32) * scale

    return {
        "x": nrm(ks[0], (BATCH, SEQ, D_MODEL), 1.0),
        "norm_mix_g": 1.0 + nrm(ks[1], (DEPTH, D_MODEL), 0.02),
        "w_in": nrm(ks[2], (DEPTH, D_MODEL, IN_COLS), D_MODEL ** -0.5),
        "sc_conv_w": nrm(ks[3], (DEPTH, SC_KERNEL, W_SC), SC_KERNEL ** -0.5),
        "cf_conv_w": nrm(ks[4], (DEPTH, CF_KERNEL, W_CF), CF_KERNEL ** -0.5),
        "cf_conv_b": nrm(ks[5], (DEPTH, W_CF), 0.02),
        "cf_ln_g": 1.0 + nrm(ks[6], (DEPTH, W_CF), 0.02),
        "cf_ln_b": nrm(ks[7], (DEPTH, W_CF), 0.02),
        "pool_w": nrm(ks[8], (DEPTH, N_POOL_GROUPS, POOL_GROUP, POOL_GROUP), POOL_GROUP ** -0.5),
        "pool_scale": 1.0 + nrm(ks[9], (DEPTH, W_POOL), 0.02),
        "w_sc_out": nrm(ks[10], (DEPTH, W_SC, D_MODEL), W_SC ** -0.5),
        "w_cf_out": nrm(ks[11], (DEPTH, W_CF, D_MODEL), W_CF ** -0.5),
        "w_pool_out": nrm(ks[12], (DEPTH, W_POOL, D_MODEL), W_POOL ** -0.5),
        "w_o": nrm(ks[13], (DEPTH, D_MODEL, D_MODEL), D_MODEL ** -0.5),
        "norm_ffn_g": 1.0 + nrm(ks[14], (DEPTH, D_MODEL), 0.02),
        "dense_w1": nrm(ks[15], (N_DENSE, D_MODEL, D_FF), D_MODEL ** -0.5),
        "dense_w3": nrm(ks[16], (N_DENSE, D_MODEL, D_FF), D_MODEL ** -0.5),
        "dense_w2": nrm(ks[17], (N_DENSE, D_FF, D_MODEL), D_FF ** -0.5),
        "moe_router": nrm(ks[18], (N_MOE, D_MODEL, N_EXPERTS), D_MODEL ** -0.5),
        "moe_w1": nrm(ks[19], (N_MOE, N_EXPERTS, D_MODEL, D_FF), D_MODEL ** -0.5),
        "moe_w3": nrm(ks[20], (N_MOE, N_EXPERTS, D_MODEL, D_FF), D_MODEL ** -0.5),
        "moe_w2": nrm(ks[21], (N_MOE, N_EXPERTS, D_FF, D_MODEL), D_FF ** -0.5),
        "norm_final_g": 1.0 + nrm(ks[22], (D_MODEL,), 0.02),
    }


def reference(x, norm_mix_g, w_in, sc_conv_w, cf_conv_w, cf_conv_b, cf_ln_g, cf_ln_b,
              pool_w, pool_scale, w_sc_out, w_cf_out, w_pool_out, w_o, norm_ffn_g,
              dense_w1, dense_w3, dense_w2, moe_router, moe_w1, moe_w3, moe_w2,
              norm_final_g):
    for layer in range(DEPTH):
        h = rms_norm(x, norm_mix_g[layer])
        x = x + hybrid_mixer(h, w_in[layer], sc_conv_w[layer], cf_conv_w[layer],
                             cf_conv_b[layer], cf_ln_g[layer], cf_ln_b[layer],
                             pool_w[layer], pool_scale[layer], w_sc_out[layer],
                             w_cf_out[layer], w_pool_out[layer], w_o[layer])
        h = rms_norm(x, norm_ffn_g[layer])
        i = layer // 2
        if layer % 2 == 0:
            x = x + swiglu(h, dense_w1[i], dense_w3[i], dense_w2[i])
        else:
            x = x + moe_swiglu(h, moe_router[i], moe_w1[i], moe_w3[i], moe_w2[i])
    return rms_norm(x, norm_final_g)
```

```python
import numpy as np
import concourse.bass as bass
import concourse.mybir as mybir
from concourse.bass_utils import run_bass_kernel_spmd
from contextlib import ExitStack

F32 = mybir.dt.float32
BF16 = mybir.dt.bfloat16
I32 = mybir.dt.int32
AF = mybir.ActivationFunctionType
ALU = mybir.AluOpType
AX = mybir.AxisListType

D = 1024
NCH = 8
SEQ = 8192
TOK = 2048
HALO = 64
TT = TOK + HALO
TH = 1088
DFF = 3584
NFG = 7
NE = 8
NS = 5
NTF = 5
NTB = 2
NSLT = 5
BLK = 576
STL = [(sl, min(128, BLK - sl * 128)) for sl in range(NSLT)]
assert (NSLT - 1) * 128 < BLK <= NSLT * 128 and BLK % 32 == 0
NRND = (TOK + BLK - 1) // BLK
CAP = NRND * BLK
CB = [(c, min(512, BLK - c)) for c in range(0, BLK, 512)]
NTT = TOK // 128
WINS = (2, 4, 8, 16)
RMS_EPS = 1e-6
LN_EPS = 1e-5

C_SCB, C_SCC, C_SCX, C_CFV, C_CFG, C_PU, C_GA, C_GB, C_GC = [i * 1024 for i in range(9)]

P_LAYER = 320
P_NMG, P_SCW, P_CFW, P_CFB, P_LNG, P_LNB, P_PSC, P_NFG = 0, 8, 32, 280, 288, 296, 304, 312
P_FIN = 2 * P_LAYER
P_ROUT = P_FIN + 8
P_MASK = P_ROUT + 64
P_INVC = P_MASK + 64
P_ECAP = P_INVC + 64
P_UT = P_ECAP + 8
NPRM = P_UT + 128


class Op:
    __slots__ = ("idx", "eng", "fn", "deps", "signals", "val", "sem", "dma", "writes", "ndma", "region")

    def __init__(self, idx, eng, fn):
        self.idx = idx
        self.eng = eng
        self.fn = fn
        self.deps = []
        self.signals = False
        self.val = 0
        self.sem = None
        self.dma = None
        self.writes = ()
        self.ndma = 0
        self.region = None


class Sched:
    COMPUTE = ("pe", "act", "dve", "pool")

    def __init__(self):
        self.ops = []
        self.last_w = {}
        self.readers = {}
        self.dma_count = {}
        self.dma_last = {}
        self.bar = None
        self.bar_done = set()
        self.last_on = {}

    def barrier(self):
        self.bar = [self.last_on[e] for e in self.COMPUTE if e in self.last_on]
        self.bar_done = set()

    def add(self, eng, fn, reads=(), writes=(), dma=None, ndma=1, region=None):
        idx = len(self.ops)
        op = Op(idx, eng, fn)
        op.region = region
        deps = {}

        def need(d, raw):
            Dp = self.ops[d]
            if Dp.dma is not None:
                key = ("dma", Dp.dma)
            else:
                if Dp.eng == eng and eng == "pe":
                    return
                key = ("eng", Dp.eng)
            if key not in deps or deps[key] < d:
                deps[key] = d

        for r in reads:
            d = self.last_w.get(r)
            if d is not None:
                need(d, True)
        for w in writes:
            d = self.last_w.get(w)
            if d is not None:
                need(d, False)
            rd = self.readers.get(w)
            if rd:
                for d in rd.values():
                    need(d, False)
        if dma is not None:
            d = self.dma_last.get(dma)
            if d is not None:
                need(d, False)
        if self.bar is not None and eng in self.COMPUTE and eng not in self.bar_done:
            self.bar_done.add(eng)
            for d in self.bar:
                if self.ops[d].eng != eng:
                    need(d, False)
        for d in deps.values():
            self.ops[d].signals = True
        op.deps = sorted(deps.values())
        op.writes = frozenset(writes)
        if dma is not None:
            op.dma = dma
            op.ndma = ndma
            self.dma_count[dma] = self.dma_count.get(dma, 0) + ndma
            op.val = 16 * self.dma_count[dma]
            self.dma_last[dma] = idx
        for r in reads:
            self.readers.setdefault(r, {})[eng if dma is None else ("dma", dma)] = idx
        for w in writes:
            self.last_w[w] = idx
            self.readers[w] = {}
        self.ops.append(op)
        if dma is None:
            self.last_on[eng] = idx
        return idx


def U(name, ch, c0, c1):
    return [(name, ch, u) for u in range(c0 // 32, (c1 + 31) // 32)]


class Builder:
    def __init__(self, stop="full", debug=False):
        self.stop = stop
        self.debug = debug
        self.nc = bass.Bass("TRN2", target_bir_lowering=False)
        self.S = Sched()
        self.dry = False
        self.plan = []
        self.es = ExitStack()

    def declare(self):
        nc = self.nc

        def din(name, shape):
            return nc.dram_tensor(name, list(shape), F32, kind="ExternalInput").ap()

        self.xT = din("xT", [D, TT])
        self.prm_d = din("prm", [128, NPRM])
        self.w_in = din("w_in", [2, D, 9216])
        self.w_sc_out = din("w_sc_out", [2, D, D])
        self.w_cf_out = din("w_cf_out", [2, D, D])
        self.w_pool_out = din("w_pool_out", [2, D, D])
        self.w_o = din("w_o", [2, D, D])
        self.pool_w = din("pool_w", [2, 4, 256, 256])
        self.dense_w1 = din("dense_w1", [1, D, DFF])
        self.dense_w3 = din("dense_w3", [1, D, DFF])
        self.dense_w2 = din("dense_w2", [1, DFF, D])
        self.moe_w1 = din("moe_w1", [1, NE, D, DFF])
        self.moe_w3 = din("moe_w3", [1, NE, D, DFF])
        self.moe_w2 = din("moe_w2", [1, NE, DFF, D])
        self.outT = nc.dram_tensor("outT", [D, TOK], F32, kind="ExternalOutput").ap()

        def sb(name, shape, dt):
            return self.es.enter_context(nc.sbuf_tensor(name, list(shape), dt))

        self.X = sb("X", [128, NCH, TT], F32)
        self.WORK = sb("WORK", [128, 13056], F32)
        hw = 4352
        self.H = self.WORK[:, 0:hw].bitcast(BF16).rearrange("p (c t) -> p c t", c=NCH)
        self.T1 = self.WORK[:, hw:2 * hw].bitcast(BF16).rearrange("p (c t) -> p c t", c=NCH)
        self.T2 = self.WORK[:, 2 * hw:3 * hw].bitcast(BF16).rearrange("p (c t) -> p c t", c=NCH)
        self.H2 = self.WORK[:, 0:8448].bitcast(BF16).rearrange("p (c t) -> p c t", c=NCH)
        self.GS = self.WORK[:, 8448:8448 + 4224].bitcast(BF16).rearrange("p (c t) -> p c t", c=4)
        self.MGB = sb("mgb", [128, NCH * TH], BF16)
        self.MG = self.MGB[:, :].rearrange("p (c t) -> p c t", c=NCH)
        self.WS = [sb(f"ws{i}", [128, 4096], BF16)[:, :] for i in range(NS - 1)] + [self.MGB[:, 0:4096]]
        self.TF = [sb(f"tf{i}", [128, 512], F32) for i in range(NTF)]
        self.TB = [sb(f"tb{i}", [128, 512], BF16) for i in range(NTB)]
        self.MEAN = sb("mean", [128, 512], F32)
        self.RSTD = sb("rstd", [128, 512], F32)
        self.DG = sb("diag", [128, 3968], F32)
        dgb = self.DG[:, 0:3968].bitcast(BF16)
        self.DIAG = [dgb[:, 0:3968].rearrange("p (k m) -> p k m", k=31),
                     dgb[:, 3968:7936].rearrange("p (k m) -> p k m", k=31)]
        dga = self.DG[:, :].bitcast(BF16)
        self.HT = [dga[:, i * 1024:(i + 1) * 1024] for i in range(4)]
        self.HGS = dga[:, 0:NSLT * 1024].rearrange("p (s d) -> p s d", s=NSLT)
        self.GSR = dga[:, NSLT * 1024:NSLT * 1024 + 4 * BLK].rearrange("p (j t) -> p j t", j=4)
        hgw = NCH * BLK // 2
        self.HG = self.WORK[:, 0:hgw].bitcast(BF16).rearrange("p (c t) -> p c t", c=NCH)
        yw = NSLT * 1024
        self.YACC = [self.WORK[:, hgw + i * yw:hgw + (i + 1) * yw].rearrange("p (s d) -> p s d", s=NSLT)
                     for i in range(2)]
        assert hgw + 2 * yw <= 13056 and NSLT * 1024 + 4 * BLK <= 7936
        self.GT = [self.WORK[:, i * 1024:(i + 1) * 1024] for i in range(8)]
        if self.debug:
            self.HD = nc.dram_tensor("hd_scr", [NE * CAP, D], BF16, kind="ExternalOutput").ap()
            self.YD = nc.dram_tensor("yd_scr", [NE * CAP, D], F32, kind="ExternalOutput").ap()
            self.DBG = nc.dram_tensor("dbg_i", [128, 40], I32, kind="ExternalOutput").ap()
        else:
            self.HD = nc.dram_tensor("hd_scr", [NE * CAP, D], BF16).ap()
            self.YD = nc.dram_tensor("yd_scr", [NE * CAP, D], F32).ap()
        self.RT = sb("rt", [128, 4 * NTT * NE + NTT * NE // 2], F32)
        self.ZT = self.RT[:, 0:512].bitcast(BF16)
        self.UTB = sb("utb", [128, 128], BF16)
        nte = NTT * NE

        def r3(lo):
            return self.RT[:, lo:lo + nte].rearrange("p (t e) -> p t e", e=NE)
        self.CNT, self.OFF, self.POS, self.TMP = r3(0), r3(nte), r3(2 * nte), r3(3 * nte)
        self.MSKB = self.RT[:, 4 * nte:4 * nte + nte // 2].bitcast(BF16).rearrange("p (t e) -> p t e", e=NE)
        self.D1 = sb("d1", [128, NTT], F32)
        self.D2 = sb("d2", [128, NTT], F32)
        self.DI1 = sb("di1", [128, NTT], I32)
        self.DI2 = sb("di2", [128, NTT], I32)
        self.CNTI = sb("cnti", [128, NE], I32)
        self.CMAXF = sb("cmaxf", [128, 1], F32)
        self.CMAXI = sb("cmaxi", [128, 1], I32)
        self.IDF = sb("idf", [128, 128], F32)
        self.IDB = sb("idb", [128, 128], BF16)
        self.ONESB = sb("onesb", [128, 128], BF16)
        self.PRM = sb("prmsb", [128, NPRM], F32)
        self.LG = sb("lg", [128, 16, NE], F32)
        self.L2 = sb("l2", [128, 16, NE], F32)
        self.EQ1 = sb("eq1", [128, 16, NE], F32)
        self.EQ2 = sb("eq2", [128, 16, NE], F32)
        self.M1 = sb("m1", [128, 16], F32)
        self.M2 = sb("m2", [128, 16], F32)
        self.P1 = sb("p1", [128, 16], F32)
        self.P2 = sb("p2", [128, 16], F32)
        self.PS = [self.es.enter_context(nc.psum_tensor(f"ps{i}", [128, 512], F32)) for i in range(8)]
        self.sems = {}

    def reset_rot(self):
        self.ib = 0
        self.iht = 0
        self.igt = 0
        self.mg_live = False
        self.cur_region = None
        self.itf = 0
        self.itb = 0
        self.wk = 0
        self.w_issued = 0
        self.w_rel = []
        self.idg = 0

    def bank(self):
        i = self.ib % 8
        self.ib += 1
        return i

    def tf(self):
        i = self.itf % NTF
        self.itf += 1
        return i

    def tb(self):
        i = self.itb % NTB
        self.itb += 1
        return i

    def ht(self):
        i = self.iht % 4
        self.iht += 1
        return i

    def gt(self):
        i = self.igt % 8
        self.igt += 1
        return i

    def add(self, eng, fn, reads=(), writes=(), dma=None, ndma=1):
        if self.dry:
            return
        if self.cur_region is not None:
            reads = list(reads) + [("cnti",)]
        return self.S.add(eng, fn, reads, writes, dma, ndma, region=self.cur_region)

    def wget(self, issue_fn):
        k = self.wk
        self.wk += 1
        if self.dry:
            slot = self.rr % self.ring_n
            self.rr = slot + 1
            self.plan.append((issue_fn, self.cur_region))
            self.slot_plan.append(slot)
            return k, self.WS[slot]
        self.pump()
        assert self.w_issued > k, "weight ring deadlock: too many slots held"
        return k, self.WS[self.slot_plan[k]]

    def wrel(self, k):
        if self.dry:
            return
        self.w_rel.append(k)
        self.pump()

    def pump(self):
        while self.w_issued < len(self.plan):
            j = self.w_issued
            if self.prev_same[j] is not None and self.prev_same[j] not in self.w_rel:
                break
            slot = self.slot_plan[j]
            if slot == NS - 1 and self.mg_live:
                break
            fn, reg = self.plan[j]
            ws = self.WS[slot]

            def f(eng, sem, fn=fn, ws=ws):
                return fn(eng, ws, sem)
            self.S.add("pool", f, reads=([("cnti",)] if reg is not None else ()), writes=[("w", slot)],
                       dma=("w", slot), ndma=fn.ndma, region=reg)
            self.w_issued += 1

    def ld_cols(self, src2d, c0, ncols=512):
        def fn(eng, ws, sem):
            dst = ws[:, 0:8 * ncols].rearrange("p (k c) -> p k c", k=8)
            src = src2d[:, c0:c0 + ncols].rearrange("(k p) c -> p k c", p=128)
            return eng.dma_start(out=dst, in_=src).then_inc(sem, 16)
        fn.ndma = 1
        return fn

    def ld_rows(self, src2d, r0):
        def fn(eng, ws, sem):
            dst = ws[:, :].rearrange("p (f d) -> p f d", f=4)
            src = src2d[r0:r0 + 512, :].rearrange("(f p) d -> p f d", p=128)
            return eng.dma_start(out=dst, in_=src).then_inc(sem, 16)
        fn.ndma = 1
        return fn

    def ld_poolw(self, src3d):
        def fn(eng, ws, sem):
            dst = ws[:, 0:2048].rearrange("p (k c) -> p k c", k=8)
            src = src3d.rearrange("g (k p) e -> p (g k) e", p=128)
            return eng.dma_start(out=dst, in_=src).then_inc(sem, 16)
        fn.ndma = 1
        return fn

    def mm_group(self, bank, n, pairs, reads):
        ps = self.PS[bank]
        nc = self.nc

        def fn(eng, pairs=pairs, ps=ps, n=n):
            last = None
            m = pairs[0][0].shape[-1]
            for i, (l, r) in enumerate(pairs):
                last = eng.matmul(ps[0:m, 0:n], lhsT=l, rhs=r, start=(i == 0), stop=(i == len(pairs) - 1))
            return last
        self.add("pe", fn, reads=reads, writes=[("ps", bank)])

    def proj(self, wsl, k, j, rhs_buf, rname, c0, n, bank, extra=()):
        wv = wsl[:, 0:4096].rearrange("p (k c) -> p k c", k=8)
        pairs = [(wv[:, kc, j * 128:(j + 1) * 128], rhs_buf[:, kc, c0:c0 + n]) for kc in range(NCH)]
        reads = [("w", self.slot_plan[k])] + list(extra)
        for kc in range(NCH):
            reads += U(rname, kc, c0, c0 + n)
        self.mm_group(bank, n, pairs, reads)

    def st_init(self):
        nc = self.nc
        S = self
        for (lo, hi) in ((1024, TT), (0, 1024)):
            for c in range(NCH):
                def f(eng, sem, c=c, lo=lo, hi=hi):
                    return eng.dma_start(out=S.X[:, c, lo:hi], in_=S.xT[c * 128:(c + 1) * 128, lo:hi]).then_inc(sem, 16)
                self.add("sp", f, writes=U("X", c, lo, hi), dma=("xl", c, lo))

        def f(eng, sem):
            return eng.dma_start(out=S.PRM[:, :], in_=S.prm_d[:, :]).then_inc(sem, 16)
        self.add("sp", f, writes=[("prm",)], dma=("prm",))

        def f(eng):
            eng.memset(S.ONESB[:, :], 1.0)
            return eng.memset(S.IDF[:, :], 0.0)
        self.add("pool", f, writes=[("idf0",), ("ones",)])

        def f(eng):
            return eng.affine_select(out=S.IDF[:, :], in_=S.IDF[:, :], compare_op=ALU.not_equal, fill=1.0,
                                     base=0, pattern=[[-1, 128]], channel_multiplier=1)
        self.add("pool", f, reads=[("idf0",)], writes=[("idf",)])

        def f(eng):
            return eng.tensor_copy(out=S.IDB[:, :], in_=S.IDF[:, :])
        self.add("pool", f, reads=[("idf",)], writes=[("idb",)])

        if self.stop != "full":
            return

        def f(eng):
            eng.memset(S.ZT[:, :], 0.0)
            return eng.tensor_copy(out=S.UTB[:, :], in_=S.PRM[:, P_UT:P_UT + 128])
        self.add("pool", f, reads=[("prm",)], writes=[("zt",), ("utb",)])
        assert (NE * CAP) % 128 == 0
        hdv = S.HD.rearrange("(q p) d -> q p d", p=128)
        nq = NE * CAP // 128
        per = 20
        for q0 in range(0, nq, per):
            def f(eng, sem, q0=q0):
                last = None
                for q in range(q0, min(nq, q0 + per)):
                    last = eng.dma_start(out=hdv[q], in_=S.ZT[:, :]).then_inc(sem, 16)
                return last
            self.add("sp", f, reads=[("zt",)], writes=[("hdz",)], dma=("zi",), ndma=min(nq, q0 + per) - q0)

    def st_norm(self, blocks, xoff, hbuf, hname, gcol, mask=False, router=False):
        S = self
        for (c0, n) in blocks:
            g0 = xoff + c0
            bk = self.bank()
            for c in range(NCH):
                t = self.tb()

                def f(eng, c=c, t=t, g0=g0, n=n):
                    return eng.activation(out=S.TB[t][:, 0:n], in_=S.X[:, c, g0:g0 + n], func=AF.Square)
                self.add("act", f, reads=U("X", c, g0, g0 + n), writes=[("tb", t)])

                def f(eng, c=c, t=t, n=n, bk=bk):
                    return eng.matmul(S.PS[bk][:, 0:n], lhsT=S.ONESB[:, :], rhs=S.TB[t][:, 0:n],
                                      start=(c == 0), stop=(c == NCH - 1))
                self.add("pe", f, reads=[("tb", t), ("ones",)], writes=[("ps", bk)])
            t1 = self.tf()

            def f(eng, t1=t1, n=n, bk=bk):
                return eng.activation(out=S.TF[t1][:, 0:n], in_=S.PS[bk][:, 0:n], func=AF.Sqrt,
                                      scale=1.0 / D, bias=S.EPSR[:, 0:1])
            self.add("act", f, reads=[("ps", bk), ("eps",)], writes=[("tf", t1)])

            def f(eng, t1=t1, n=n):
                return eng.reciprocal(out=S.RSTD[:, 0:n], in_=S.TF[t1][:, 0:n])
            self.add("dve", f, reads=[("tf", t1)], writes=[("rstd",)])
            if mask and c0 < HALO:
                def f(eng, c0=c0, n=n):
                    return eng.tensor_tensor(out=S.RSTD[:, 0:n], in0=S.RSTD[:, 0:n],
                                             in1=S.PRM[:, P_MASK + c0:P_MASK + c0 + n], op=ALU.mult)
                self.add("dve", f, reads=[("rstd",), ("prm",)], writes=[("rstd",)])
            if router:
                rb = self.bank()
            for c in range(NCH):
                if not router:
                    def f(eng, c=c, c0=c0, g0=g0, n=n):
                        return eng.scalar_tensor_tensor(out=hbuf[:, c, c0:c0 + n], in0=S.X[:, c, g0:g0 + n],
                                                        scalar=S.PRM[:, gcol + c:gcol + c + 1],
                                                        in1=S.RSTD[:, 0:n], op0=ALU.mult, op1=ALU.mult)
                    self.add("dve", f, reads=U("X", c, g0, g0 + n) + [("rstd",), ("prm",)],
                             writes=U(hname, c, c0, c0 + n))
                else:
                    t2 = self.tf()

                    def f(eng, c=c, g0=g0, n=n, t2=t2):
                        return eng.scalar_tensor_tensor(out=S.TF[t2][:, 0:n], in0=S.X[:, c, g0:g0 + n],
                                                        scalar=S.PRM[:, gcol + c:gcol + c + 1],
                                                        in1=S.RSTD[:, 0:n], op0=ALU.mult, op1=ALU.mult)
                    self.add("dve", f, reads=U("X", c, g0, g0 + n) + [("rstd",), ("prm",)], writes=[("tf", t2)])

                    def f(eng, c=c, c0=c0, n=n, t2=t2):
                        return eng.activation(out=hbuf[:, c, c0:c0 + n], in_=S.TF[t2][:, 0:n], func=AF.Copy)
                    self.add("act", f, reads=[("tf", t2)], writes=U(hname, c, c0, c0 + n))

                    def f(eng, c=c, n=n, t2=t2, rb=rb):
                        return eng.matmul(S.PS[rb][0:NE, 0:n], lhsT=S.PRM[:, P_ROUT + c * NE:P_ROUT + (c + 1) * NE],
                                          rhs=S.TF[t2][:, 0:n], start=(c == 0), stop=(c == NCH - 1))
                    self.add("pe", f, reads=[("tf", t2), ("prm",)], writes=[("ps", rb)])
            if router:
                t3 = self.tf()

                def f(eng, n=n, t3=t3, rb=rb):
                    return eng.activation(out=S.TF[t3][0:NE, 0:n], in_=S.PS[rb][0:NE, 0:n], func=AF.Copy)
                self.add("act", f, reads=[("ps", rb)], writes=[("tf", t3)])
                tb_ = self.bank()
                nt = n // 128

                def f(eng, t3=t3, tb_=tb_, nt=nt):
                    last = None
                    for j in range(nt):
                        last = eng.transpose(out=S.PS[tb_][:, j * NE:(j + 1) * NE],
                                             in_=S.TF[t3][0:NE, j * 128:(j + 1) * 128],
                                             identity=S.IDF[0:NE, 0:NE])
                    return last
                self.add("pe", f, reads=[("tf", t3), ("idf",)], writes=[("ps", tb_)])
                j0 = (g0 - HALO) // 128

                def f(eng, tb_=tb_, nt=nt, j0=j0):
                    return eng.tensor_copy(out=S.LG[:, j0:j0 + nt, :],
                                           in_=S.PS[tb_][:, 0:nt * NE].rearrange("p (j e) -> p j e", e=NE))
                self.add("dve", f, reads=[("ps", tb_)], writes=[("lg", j0)])

    def blocks_of(self, first):
        b = []
        if first < HALO:
            b.append((first, HALO - first))
        b += [(HALO, 512), (HALO + 512, 512)]
        return b

    def st_outproj(self, l, w_out, gbase, post, xoff, first, last):
        S = self
        mgres = [("w", NS - 1)]
        pending = None
        for grp in range(2):
            kw, wsl = self.wget(self.ld_cols(w_out[l], grp * 512))
            kg, gsl = self.wget(self.ld_cols(self.w_in[l], gbase + grp * 512))
            for j in range(4):
                d = grp * 4 + j
                for (c0, n) in post:
                    by = self.bank()
                    self.proj(wsl, kw, j, S.T1, "T1", c0, n, by)
                    bg = self.bank()
                    self.proj(gsl, kg, j, S.H, "H", c0, n, bg)
                    t = self.tf()

                    def f(eng, t=t, n=n, bg=bg):
                        return eng.activation(out=S.TF[t][:, 0:n], in_=S.PS[bg][:, 0:n], func=AF.Sigmoid)
                    self.add("act", f, reads=[("ps", bg)], writes=[("tf", t)])
                    if first:
                        def f(eng, t=t, n=n, by=by, d=d, c0=c0):
                            return eng.tensor_tensor(out=S.MG[:, d, c0:c0 + n], in0=S.PS[by][:, 0:n],
                                                     in1=S.TF[t][:, 0:n], op=ALU.mult)
                        self.add("dve", f, reads=[("ps", by), ("tf", t)], writes=U("MG", d, c0, c0 + n) + mgres)
                    else:
                        def f(eng, t=t, n=n, by=by):
                            return eng.tensor_tensor(out=S.TF[t][:, 0:n], in0=S.PS[by][:, 0:n],
                                                     in1=S.TF[t][:, 0:n], op=ALU.mult)
                        self.add("dve", f, reads=[("ps", by), ("tf", t)], writes=[("tf", t)])
                        if pending is not None:
                            pending()

                        def pend(t=t, n=n, d=d, c0=c0):
                            def f(eng):
                                return eng.tensor_tensor(out=S.MG[:, d, c0:c0 + n], in0=S.MG[:, d, c0:c0 + n],
                                                         in1=S.TF[t][:, 0:n], op=ALU.add)
                            self.add("dve", f, reads=[("tf", t)] + U("MG", d, c0, c0 + n),
                                     writes=U("MG", d, c0, c0 + n) + mgres)
                        pending = pend
            self.wrel(kw)
            self.wrel(kg)
        if pending is not None:
            pending()
        if not last:
            return
        for grp in range(2):
            ko, osl = self.wget(self.ld_cols(self.w_o[l], grp * 512))
            for j in range(4):
                e = grp * 4 + j
                for (c0, n) in post:
                    b = self.bank()
                    self.proj(osl, ko, j, S.MG, "MG", c0, n, b, extra=mgres)
                    g0 = xoff + c0

                    def f(eng, e=e, g0=g0, n=n, b=b):
                        return eng.tensor_tensor(out=S.X[:, e, g0:g0 + n], in0=S.X[:, e, g0:g0 + n],
                                                 in1=S.PS[b][:, 0:n], op=ALU.add)
                    self.add("dve", f, reads=[("ps", b)] + U("X", e, g0, g0 + n), writes=U("X", e, g0, g0 + n))
            self.wrel(ko)

    def build_diag(self, col0, ntap):
        S = self
        db = self.idg % 2
        self.idg += 1

        def f(eng, db=db, col0=col0, ntap=ntap):
            return eng.tensor_tensor(out=S.DIAG[db][:, 0:ntap, :],
                                     in0=S.IDB[:, :].unsqueeze(1).to_broadcast([128, ntap, 128]),
                                     in1=S.PRM[:, col0:col0 + ntap].unsqueeze(2).to_broadcast([128, ntap, 128]),
                                     op=ALU.mult)
        self.add("dve", f, reads=[("idb",), ("prm",)], writes=[("diag", db, 0)])
        return db

    def st_mixer_half(self, l, half, ci0, po0):
        S = self
        xoff = 0 if half == 0 else 1024
        ci = self.blocks_of(ci0)
        post = self.blocks_of(po0)
        pl = l * P_LAYER
        win = self.w_in[l]
        self.st_norm(ci, xoff, S.H, "H", pl + P_NMG, mask=(half == 0))

        for grp in range(2):
            k, sl = self.wget(self.ld_cols(win, C_PU + grp * 512))
            for j in range(4):
                c = grp * 4 + j
                for (c0, n) in ci:
                    b = self.bank()
                    self.proj(sl, k, j, S.H, "H", c0, n, b)

                    def f(eng, c=c, c0=c0, n=n, b=b):
                        return eng.activation(out=S.T2[:, c, c0:c0 + n], in_=S.PS[b][:, 0:n], func=AF.Copy)
                    self.add("act", f, reads=[("ps", b)], writes=U("T2", c, c0, c0 + n))
            self.wrel(k)
        kp, psl = self.wget(self.ld_poolw(self.pool_w[l]))
        pwv = psl[:, 0:2048].rearrange("p (k c) -> p k c", k=8)
        for g in range(4):
            w = WINS[g]
            for cc in range(2):
                c = 2 * g + cc
                for (c0, n) in post:
                    b = self.bank()
                    pairs = [(S.IDB[:, :], S.T2[:, c, c0 - jj:c0 - jj + n]) for jj in range(w)]
                    self.mm_group(b, n, pairs, [("idb",)] + U("T2", c, c0 - w + 1, c0 + n))

                    def f(eng, c=c, c0=c0, n=n, b=b, w=w):
                        return eng.scalar_tensor_tensor(out=S.T1[:, c, c0:c0 + n], in0=S.PS[b][:, 0:n],
                                                        scalar=1.0 / w, in1=S.T2[:, c, c0:c0 + n],
                                                        op0=ALU.mult, op1=ALU.subtract)
                    self.add("dve", f, reads=[("ps", b)] + U("T2", c, c0, c0 + n), writes=U("T1", c, c0, c0 + n))
                    if half == 0 and c0 == HALO:
                        t = self.tf()

                        def f(eng, t=t, b=b, g=g):
                            return eng.tensor_tensor(out=S.TF[t][:, 0:16], in0=S.PS[b][:, 0:16],
                                                     in1=S.PRM[:, P_INVC + g * 16:P_INVC + (g + 1) * 16], op=ALU.mult)
                        self.add("dve", f, reads=[("ps", b), ("prm",)], writes=[("tf", t)])

                        def f(eng, t=t, c=c):
                            return eng.tensor_tensor(out=S.T1[:, c, HALO:HALO + 16], in0=S.TF[t][:, 0:16],
                                                     in1=S.T2[:, c, HALO:HALO + 16], op=ALU.subtract)
                        self.add("dve", f, reads=[("tf", t)] + U("T2", c, HALO, HALO + 16),
                                 writes=U("T1", c, HALO, HALO + 16))
            for (c0, n) in post:
                bs = []
                for e2 in range(2):
                    b = self.bank()
                    bs.append(b)
                    pairs = [(pwv[:, g * 2 + kc, e2 * 128:(e2 + 1) * 128], S.T1[:, 2 * g + kc, c0:c0 + n])
                             for kc in range(2)]
                    rd = [("w", self.slot_plan[kp])] + U("T1", 2 * g, c0, c0 + n) + U("T1", 2 * g + 1, c0, c0 + n)
                    self.mm_group(b, n, pairs, rd)
                for e2 in range(2):
                    ch = 2 * g + e2

                    def f(eng, ch=ch, c0=c0, n=n, b=bs[e2]):
                        return eng.activation(out=S.T1[:, ch, c0:c0 + n], in_=S.PS[b][:, 0:n], func=AF.Copy,
                                              scale=S.PRM[:, pl + P_PSC + ch:pl + P_PSC + ch + 1])
                    self.add("act", f, reads=[("ps", bs[e2]), ("prm",)], writes=U("T1", ch, c0, c0 + n))
        self.wrel(kp)
        self.st_outproj(l, self.w_pool_out, C_GC, post, xoff, True, False)

        for grp in range(2):
            kc_, csl = self.wget(self.ld_cols(win, C_SCC + grp * 512))
            kx_, xsl = self.wget(self.ld_cols(win, C_SCX + grp * 512))
            for j in range(4):
                c = grp * 4 + j
                for (c0, n) in ci:
                    b1 = self.bank()
                    self.proj(csl, kc_, j, S.H, "H", c0, n, b1)
                    b2 = self.bank()
                    self.proj(xsl, kx_, j, S.H, "H", c0, n, b2)
                    t = self.tf()

                    def f(eng, t=t, n=n, b1=b1):
                        return eng.activation(out=S.TF[t][:, 0:n], in_=S.PS[b1][:, 0:n], func=AF.Copy)
                    self.add("act", f, reads=[("ps", b1)], writes=[("tf", t)])

                    def f(eng, t=t, n=n, b2=b2, c=c, c0=c0):
                        return eng.tensor_tensor(out=S.T2[:, c, c0:c0 + n], in0=S.PS[b2][:, 0:n],
                                                 in1=S.TF[t][:, 0:n], op=ALU.mult)
                    self.add("dve", f, reads=[("ps", b2), ("tf", t)], writes=U("T2", c, c0, c0 + n))
            self.wrel(kc_)
            self.wrel(kx_)
        for grp in range(2):
            kb_, bsl = self.wget(self.ld_cols(win, C_SCB + grp * 512))
            for j in range(4):
                c = grp * 4 + j
                db = self.build_diag(pl + P_SCW + c * 3, 3)
                for (c0, n) in post:
                    b1 = self.bank()
                    pairs = [(S.DIAG[db][:, kk, :], S.T2[:, c, c0 - 2 + kk:c0 - 2 + kk + n]) for kk in range(3)]
                    self.mm_group(b1, n, pairs, [("diag", db, 0)] + U("T2", c, c0 - 2, c0 + n))
                    b2 = self.bank()
                    self.proj(bsl, kb_, j, S.H, "H", c0, n, b2)
                    t = self.tf()

                    def f(eng, t=t, n=n, b2=b2):
                        return eng.activation(out=S.TF[t][:, 0:n], in_=S.PS[b2][:, 0:n], func=AF.Copy)
                    self.add("act", f, reads=[("ps", b2)], writes=[("tf", t)])

                    def f(eng, t=t, n=n, b1=b1, c=c, c0=c0):
                        return eng.tensor_tensor(out=S.T1[:, c, c0:c0 + n], in0=S.PS[b1][:, 0:n],
                                                 in1=S.TF[t][:, 0:n], op=ALU.mult)
                    self.add("dve", f, reads=[("ps", b1), ("tf", t)], writes=U("T1", c, c0, c0 + n))
            self.wrel(kb_)
        self.st_outproj(l, self.w_sc_out, C_GA, post, xoff, False, False)

        for grp in range(2):
            kg_, gsl = self.wget(self.ld_cols(win, C_CFG + grp * 512))
            kv_, vsl = self.wget(self.ld_cols(win, C_CFV + grp * 512))
            for j in range(4):
                c = grp * 4 + j
                for (c0, n) in ci:
                    b1 = self.bank()
                    self.proj(gsl, kg_, j, S.H, "H", c0, n, b1)
                    b2 = self.bank()
                    self.proj(vsl, kv_, j, S.H, "H", c0, n, b2)
                    t = self.tf()

                    def f(eng, t=t, n=n, b1=b1):
                        return eng.activation(out=S.TF[t][:, 0:n], in_=S.PS[b1][:, 0:n], func=AF.Sigmoid)
                    self.add("act", f, reads=[("ps", b1)], writes=[("tf", t)])

                    def f(eng, t=t, n=n, b2=b2, c=c, c0=c0):
                        return eng.tensor_tensor(out=S.T2[:, c, c0:c0 + n], in0=S.PS[b2][:, 0:n],
                                                 in1=S.TF[t][:, 0:n], op=ALU.mult)
                    self.add("dve", f, reads=[("ps", b2), ("tf", t)], writes=U("T2", c, c0, c0 + n))
            self.wrel(kg_)
            self.wrel(kv_)
        for c in range(NCH):
            db = self.build_diag(pl + P_CFW + c * 31, 31)
            for (c0, n) in post:
                b = self.bank()
                pairs = [(S.DIAG[db][:, kk, :], S.T2[:, c, c0 - 30 + kk:c0 - 30 + kk + n]) for kk in range(31)]
                self.mm_group(b, n, pairs, [("diag", db, 0)] + U("T2", c, c0 - 30, c0 + n))

                def f(eng, c=c, c0=c0, n=n, b=b):
                    return eng.activation(out=S.T1[:, c, c0:c0 + n], in_=S.PS[b][:, 0:n], func=AF.Identity,
                                          bias=S.PRM[:, pl + P_CFB + c:pl + P_CFB + c + 1])
                self.add("act", f, reads=[("ps", b), ("prm",)], writes=U("T1", c, c0, c0 + n))
        for (c0, n) in post:
            b1 = self.bank()
            b2 = self.bank()
            for c in range(NCH):
                t = self.tb()

                def f(eng, c=c, t=t, c0=c0, n=n):
                    return eng.activation(out=S.TB[t][:, 0:n], in_=S.T1[:, c, c0:c0 + n], func=AF.Square)
                self.add("act", f, reads=U("T1", c, c0, c0 + n), writes=[("tb", t)])

                def f(eng, c=c, t=t, c0=c0, n=n, b1=b1, b2=b2):
                    eng.matmul(S.PS[b1][:, 0:n], lhsT=S.ONESB[:, :], rhs=S.T1[:, c, c0:c0 + n],
                               start=(c == 0), stop=(c == NCH - 1))
                    return eng.matmul(S.PS[b2][:, 0:n], lhsT=S.ONESB[:, :], rhs=S.TB[t][:, 0:n],
                                      start=(c == 0), stop=(c == NCH - 1))
                self.add("pe", f, reads=[("tb", t), ("ones",)] + U("T1", c, c0, c0 + n),
                         writes=[("ps", b1), ("ps", b2)])

            def f(eng, n=n, b1=b1):
                return eng.activation(out=S.MEAN[:, 0:n], in_=S.PS[b1][:, 0:n], func=AF.Copy, scale=1.0 / D)
            self.add("act", f, reads=[("ps", b1)], writes=[("mean",)])
            ta = self.tf()

            def f(eng, n=n, ta=ta):
                return eng.tensor_tensor(out=S.TF[ta][:, 0:n], in0=S.MEAN[:, 0:n], in1=S.MEAN[:, 0:n], op=ALU.mult)
            self.add("dve", f, reads=[("mean",)], writes=[("tf", ta)])
            tv = self.tf()

            def f(eng, n=n, ta=ta, tv=tv, b2=b2):
                return eng.scalar_tensor_tensor(out=S.TF[tv][:, 0:n], in0=S.PS[b2][:, 0:n], scalar=1.0 / D,
                                                in1=S.TF[ta][:, 0:n], op0=ALU.mult, op1=ALU.subtract)
            self.add("dve", f, reads=[("ps", b2), ("tf", ta)], writes=[("tf", tv)])
            tr = self.tf()

            def f(eng, n=n, tv=tv, tr=tr):
                return eng.activation(out=S.TF[tr][:, 0:n], in_=S.TF[tv][:, 0:n], func=AF.Sqrt,
                                      bias=S.EPSL[:, 0:1])
            self.add("act", f, reads=[("tf", tv), ("eps",)], writes=[("tf", tr)])

            def f(eng, n=n, tr=tr):
                return eng.reciprocal(out=S.RSTD[:, 0:n], in_=S.TF[tr][:, 0:n])
            self.add("dve", f, reads=[("tf", tr)], writes=[("rstd",)])
            for c in range(NCH):
                t1 = self.tf()

                def f(eng, c=c, c0=c0, n=n, t1=t1):
                    return eng.tensor_tensor(out=S.TF[t1][:, 0:n], in0=S.T1[:, c, c0:c0 + n], in1=S.MEAN[:, 0:n],
                                             op=ALU.subtract)
                self.add("dve", f, reads=U("T1", c, c0, c0 + n) + [("mean",)], writes=[("tf", t1)])
                t2 = self.tf()

                def f(eng, n=n, t1=t1, t2=t2):
                    return eng.tensor_tensor(out=S.TF[t2][:, 0:n], in0=S.TF[t1][:, 0:n], in1=S.RSTD[:, 0:n],
                                             op=ALU.mult)
                self.add("dve", f, reads=[("tf", t1), ("rstd",)], writes=[("tf", t2)])

                def f(eng, c=c, c0=c0, n=n, t2=t2):
                    return eng.activation(out=S.T1[:, c, c0:c0 + n], in_=S.TF[t2][:, 0:n], func=AF.Silu,
                                          scale=S.PRM[:, pl + P_LNG + c:pl + P_LNG + c + 1],
                                          bias=S.PRM[:, pl + P_LNB + c:pl + P_LNB + c + 1])
                self.add("act", f, reads=[("tf", t2), ("prm",)], writes=U("T1", c, c0, c0 + n))
        self.st_outproj(l, self.w_cf_out, C_GB, post, xoff, False, True)

    def st_ffn(self, l, blocks, experts):
        S = self
        for (w1, w3, w2) in experts:
            for fg in range(NFG):
                k1, s1 = self.wget(self.ld_cols(w1, fg * 512))
                k3, s3 = self.wget(self.ld_cols(w3, fg * 512))
                k2, s2 = self.wget(self.ld_rows(w2, fg * 512))
                for (c0, n) in blocks:
                    for j in range(4):
                        ba = self.bank()
                        self.proj(s1, k1, j, S.H2, "H2", c0, n, ba)
                        bc = self.bank()
                        self.proj(s3, k3, j, S.H2, "H2", c0, n, bc)
                        t = self.tf()

                        def f(eng, t=t, n=n, ba=ba):
                            return eng.activation(out=S.TF[t][:, 0:n], in_=S.PS[ba][:, 0:n], func=AF.Silu)
                        self.add("act", f, reads=[("ps", ba)], writes=[("tf", t)])

                        def f(eng, t=t, n=n, bc=bc, j=j, c0=c0):
                            return eng.tensor_tensor(out=S.GS[:, j, c0:c0 + n], in0=S.PS[bc][:, 0:n],
                                                     in1=S.TF[t][:, 0:n], op=ALU.mult)
                        self.add("dve", f, reads=[("ps", bc), ("tf", t)], writes=U("GS", j, c0, c0 + n))
                self.wrel(k1)
                self.wrel(k3)
                w2v = s2[:, :].rearrange("p (f d) -> p f d", f=4)
                for (c0, n) in blocks:
                    for d in range(NCH):
                        b = self.bank()
                        pairs = [(w2v[:, j, d * 128:(d + 1) * 128], S.GS[:, j, c0:c0 + n]) for j in range(4)]
                        rd = [("w", self.slot_plan[k2])]
                        for j in range(4):
                            rd += U("GS", j, c0, c0 + n)
                        self.mm_group(b, n, pairs, rd)

                        def f(eng, d=d, c0=c0, n=n, b=b):
                            return eng.tensor_tensor(out=S.X[:, d, c0:c0 + n], in0=S.X[:, d, c0:c0 + n],
                                                     in1=S.PS[b][:, 0:n], op=ALU.add)
                        self.add("dve", f, reads=[("ps", b)] + U("X", d, c0, c0 + n), writes=U("X", d, c0, c0 + n))
                self.wrel(k2)

    def st_route(self):
        S = self
        allg = [("lg", j0) for j0 in range(0, 16, 4)]

        def bc(ap):
            return ap.unsqueeze(2).to_broadcast([128, NTT, NE])

        def f(eng):
            return eng.tensor_reduce(out=S.M1[:, :], in_=S.LG[:, :, :], axis=AX.X, op=ALU.max)
        self.add("dve", f, reads=allg, writes=[("m1",)])

        def f(eng):
            return eng.tensor_tensor(out=S.EQ1[:, :, :], in0=S.LG[:, :, :], in1=bc(S.M1[:, :]), op=ALU.is_equal)
        self.add("dve", f, reads=allg + [("m1",)], writes=[("eq1",)])

        def f(eng):
            return eng.scalar_tensor_tensor(out=S.L2[:, :, :], in0=S.EQ1[:, :, :], scalar=-1e30, in1=S.LG[:, :, :],
                                            op0=ALU.mult, op1=ALU.add)
        self.add("dve", f, reads=allg + [("eq1",)], writes=[("l2",)])

        def f(eng):
            return eng.tensor_reduce(out=S.M2[:, :], in_=S.L2[:, :, :], axis=AX.X, op=ALU.max)
        self.add("dve", f, reads=[("l2",)], writes=[("m2",)])

        def f(eng):
            return eng.tensor_tensor(out=S.EQ2[:, :, :], in0=S.L2[:, :, :], in1=bc(S.M2[:, :]), op=ALU.is_equal)
        self.add("dve", f, reads=[("l2",), ("m2",)], writes=[("eq2",)])

        def f(eng):
            return eng.tensor_tensor(out=S.M2[:, :], in0=S.M1[:, :], in1=S.M2[:, :], op=ALU.subtract)
        self.add("dve", f, reads=[("m1",), ("m2",), ("eq2",)], writes=[("dd",)])

        def f(eng):
            eng.activation(out=S.P1[:, :], in_=S.M2[:, :], func=AF.Sigmoid)
            return eng.activation(out=S.P2[:, :], in_=S.M2[:, :], func=AF.Sigmoid, scale=-1.0)
        self.add("act", f, reads=[("dd",)], writes=[("p12",)])

        def f(eng):
            return eng.tensor_tensor(out=S.MSKB[:, :, :], in0=S.EQ1[:, :, :], in1=S.EQ2[:, :, :], op=ALU.add)
        self.add("dve", f, reads=[("eq1",), ("eq2",), ("hdz",)], writes=[("mskb",)])
        ba = self.bank()
        bc_ = self.bank()
        mflat = S.MSKB[:, :, :].rearrange("p t e -> p (t e)")

        def f(eng, ba=ba, bc_=bc_):
            eng.matmul(S.PS[ba][:, 0:NTT * NE], lhsT=S.UTB[:, :], rhs=mflat, start=True, stop=True)
            return eng.matmul(S.PS[bc_][:, 0:NTT * NE], lhsT=S.ONESB[:, :], rhs=mflat, start=True, stop=True)
        self.add("pe", f, reads=[("mskb",), ("utb",), ("ones",)], writes=[("ps", ba), ("ps", bc_)])

        def v3(ap):
            return ap.rearrange("p (t e) -> p t e", e=NE)

        def f(eng, bc_=bc_):
            return eng.tensor_copy(out=S.CNT[:, :, :], in_=v3(S.PS[bc_][:, 0:NTT * NE]))
        self.add("dve", f, reads=[("ps", bc_)], writes=[("cntb",)])
        bufs = [(S.CNT, "cntb"), (S.OFF, "offb"), (S.TMP, "tmpb"), (S.OFF, "offb"), (S.TMP, "tmpb")]
        for k, sh in enumerate((1, 2, 4, 8)):
            (src, sn), (dst, dn) = bufs[k], bufs[k + 1]

            def f(eng, src=src, dst=dst, sh=sh):
                eng.tensor_copy(out=dst[:, 0:sh, :], in_=src[:, 0:sh, :])
                return eng.tensor_tensor(out=dst[:, sh:NTT, :], in0=src[:, sh:NTT, :], in1=src[:, 0:NTT - sh, :], op=ALU.add)
            self.add("dve", f, reads=[(sn,)], writes=[(dn,)])

        def f(eng):
            return eng.tensor_copy(out=S.CNTI[:, :], in_=S.TMP[:, NTT - 1, :])
        self.add("dve", f, reads=[("tmpb",)], writes=[("cnti0",)])

        def f(eng):
            return eng.tensor_reduce(out=S.CMAXF[:, :], in_=S.TMP[:, NTT - 1, :], axis=AX.X, op=ALU.max)
        self.add("dve", f, reads=[("tmpb",)], writes=[("cmaxf",)])

        def f(eng):
            return eng.tensor_copy(out=S.CMAXI[:, :], in_=S.CMAXF[:, :])
        self.cnti_op = self.add("dve", f, reads=[("cmaxf",), ("cnti0",)], writes=[("cnti",)])

        def f(eng):
            return eng.tensor_tensor(out=S.OFF[:, :, :], in0=S.TMP[:, :, :], in1=S.CNT[:, :, :], op=ALU.subtract)
        self.add("dve", f, reads=[("tmpb",), ("cntb",)], writes=[("offb",)])

        def f(eng):
            return eng.tensor_tensor(out=S.CNT[:, :, :], in0=S.OFF[:, :, :],
                                     in1=S.PRM[:, P_ECAP:P_ECAP + NE].unsqueeze(1).to_broadcast([128, NTT, NE]),
                                     op=ALU.add)
        self.add("dve", f, reads=[("offb",), ("prm",)], writes=[("cntb",)])

        def f(eng, ba=ba):
            return eng.tensor_tensor(out=S.POS[:, :, :], in0=v3(S.PS[ba][:, 0:NTT * NE]), in1=S.CNT[:, :, :], op=ALU.add)
        self.add("dve", f, reads=[("ps", ba), ("cntb",)], writes=[("pos",)])

        def f(eng):
            return eng.tensor_tensor(out=S.TMP[:, :, :], in0=S.EQ1[:, :, :], in1=S.POS[:, :, :], op=ALU.mult)
        self.add("dve", f, reads=[("pos",), ("eq1",), ("cnti",)], writes=[("tmpb",)])

        def f(eng):
            return eng.tensor_reduce(out=S.D1[:, :], in_=S.TMP[:, :, :], axis=AX.X, op=ALU.add)
        self.add("dve", f, reads=[("tmpb",)], writes=[("d1",)])

        def f(eng):
            return eng.tensor_tensor(out=S.OFF[:, :, :], in0=S.EQ2[:, :, :], in1=S.POS[:, :, :], op=ALU.mult)
        self.add("dve", f, reads=[("pos",), ("eq2",)], writes=[("offb",)])

        def f(eng):
            return eng.tensor_reduce(out=S.D2[:, :], in_=S.OFF[:, :, :], axis=AX.X, op=ALU.add)
        self.add("dve", f, reads=[("offb",)], writes=[("d2",)])

        def f(eng):
            return eng.tensor_copy(out=S.DI1[:, :], in_=S.D1[:, :])
        self.add("dve", f, reads=[("d1",)], writes=[("di1",)])

        def f(eng):
            return eng.tensor_copy(out=S.DI2[:, :], in_=S.D2[:, :])
        self.add("dve", f, reads=[("d2",)], writes=[("di",)])
        if self.debug:
            def f(eng, sem):
                eng.dma_start(out=S.DBG[:, 0:16], in_=S.DI1[:, :]).then_inc(sem, 16)
                eng.dma_start(out=S.DBG[:, 16:32], in_=S.DI2[:, :]).then_inc(sem, 16)
                return eng.dma_start(out=S.DBG[:, 32:40], in_=S.CNTI[:, :]).then_inc(sem, 16)
            self.add("sp", f, reads=[("di",), ("di1",), ("cnti",)], writes=[("dbg",)], dma=("dbg",), ndma=3)

    def st_scatter(self):
        S = self
        for tt in range(NTT):
            t0 = HALO + tt * 128
            bs = [self.bank(), self.bank()]

            def f(eng, t0=t0, bs=bs):
                last = None
                for kc in range(NCH):
                    last = eng.matmul(S.PS[bs[kc // 4]][:, (kc % 4) * 128:(kc % 4 + 1) * 128],
                                      lhsT=S.H2[:, kc, t0:t0 + 128], rhs=S.IDB[:, :], start=True, stop=True)
                return last
            rd = [("idb",)]
            for kc in range(NCH):
                rd += U("H2", kc, t0, t0 + 128)
            self.add("pe", f, reads=rd, writes=[("ps", bs[0]), ("ps", bs[1])])
            i = self.ht()

            def f(eng, i=i, b=bs[0]):
                return eng.activation(out=S.HT[i][:, 0:512], in_=S.PS[b][:, 0:512], func=AF.Copy)
            self.add("act", f, reads=[("ps", bs[0])], writes=[("ht", i, 0)])

            def f(eng, i=i, b=bs[1]):
                return eng.tensor_copy(out=S.HT[i][:, 512:1024], in_=S.PS[b][:, 0:512])
            self.add("dve", f, reads=[("ps", bs[1])], writes=[("ht", i, 1)])

            def f(eng, sem, i=i, tt=tt):
                eng.indirect_dma_start(out=S.HD[:, :], out_offset=bass.IndirectOffsetOnAxis(ap=S.DI1[:, tt:tt + 1], axis=0),
                                       in_=S.HT[i], in_offset=None, bounds_check=S.rb_pool,
                                       oob_is_err=False).then_inc(sem, 16)
                return eng.indirect_dma_start(out=S.HD[:, :],
                                              out_offset=bass.IndirectOffsetOnAxis(ap=S.DI2[:, tt:tt + 1], axis=0),
                                              in_=S.HT[i], in_offset=None, bounds_check=S.rb_pool,
                                              oob_is_err=False).then_inc(sem, 16)
            self.add("pool", f, reads=[("ht", i, 0), ("ht", i, 1), ("di",), ("di1",), ("hdz",)], writes=[("hd", tt)],
                     dma=("scat", tt % 4), ndma=2)

    def pass_load(self, e, r):
        S = self
        row0 = e * CAP + r * BLK

        nfull = BLK // 128
        rem = BLK - nfull * 128

        def f(eng, sem, row0=row0):
            last = eng.dma_start(out=S.HGS[:, 0:nfull, :],
                                 in_=S.HD[row0:row0 + nfull * 128, :].rearrange("(s p) d -> p s d", p=128)).then_inc(sem, 16)
            if rem:
                last = eng.dma_start(out=S.HGS[0:rem, nfull, :],
                                     in_=S.HD[row0 + nfull * 128:row0 + BLK, :]).then_inc(sem, 16)
            return last
        self.add("sp", f, reads=[("hdz",)] + [("hd", tt) for tt in range(NTT)], writes=[("hgs",)], dma=("hgl",),
                 ndma=2 if rem else 1)

    def st_pass(self, e, r, yi, prefetch=None):
        S = self
        row0 = e * CAP + r * BLK
        if prefetch is None or not prefetch[0]:
            self.pass_load(e, r)
        alt = 0
        for kc in range(NCH):
            for (c0, n) in CB:
                b = self.bank()

                def f(eng, kc=kc, c0=c0, n=n, b=b):
                    last = None
                    for (sl, rows) in STL:
                        if not (c0 <= sl * 128 < c0 + n):
                            continue
                        o = sl * 128 - c0
                        last = eng.matmul(S.PS[b][:, o:o + rows], lhsT=S.HGS[0:rows, sl, kc * 128:(kc + 1) * 128],
                                          rhs=S.IDB[0:rows, 0:rows], start=True, stop=True)
                    return last
                self.add("pe", f, reads=[("hgs",), ("idb",)], writes=[("ps", b)])
                if alt % 2 == 0:
                    def f(eng, kc=kc, c0=c0, n=n, b=b):
                        return eng.activation(out=S.HG[:, kc, c0:c0 + n], in_=S.PS[b][:, 0:n], func=AF.Copy)
                    self.add("act", f, reads=[("ps", b)], writes=U("HG", kc, c0, c0 + n))
                else:
                    def f(eng, kc=kc, c0=c0, n=n, b=b):
                        return eng.tensor_copy(out=S.HG[:, kc, c0:c0 + n], in_=S.PS[b][:, 0:n])
                    self.add("dve", f, reads=[("ps", b)], writes=U("HG", kc, c0, c0 + n))
                alt += 1
        if prefetch is not None and prefetch[1] is not None:
            self.pass_load(prefetch[1], 0)
        w1, w3, w2 = self.moe_w1[0, e], self.moe_w3[0, e], self.moe_w2[0, e]
        Y = S.YACC[yi]
        for fg in range(NFG):
            k1, s1 = self.wget(self.ld_cols(w1, fg * 512))
            k3, s3 = self.wget(self.ld_cols(w3, fg * 512))
            k2, s2 = self.wget(self.ld_rows(w2, fg * 512))
            for (c0, n) in CB:
                for j in range(4):
                    ba = self.bank()
                    self.proj(s1, k1, j, S.HG, "HG", c0, n, ba)
                    bc = self.bank()
                    self.proj(s3, k3, j, S.HG, "HG", c0, n, bc)
                    t = self.tf()

                    def f(eng, t=t, n=n, ba=ba):
                        return eng.activation(out=S.TF[t][:, 0:n], in_=S.PS[ba][:, 0:n], func=AF.Silu)
                    self.add("act", f, reads=[("ps", ba)], writes=[("tf", t)])

                    def f(eng, t=t, n=n, bc=bc, j=j, c0=c0):
                        return eng.tensor_tensor(out=S.GSR[:, j, c0:c0 + n], in0=S.PS[bc][:, 0:n],
                                                 in1=S.TF[t][:, 0:n], op=ALU.mult)
                    self.add("dve", f, reads=[("ps", bc), ("tf", t)], writes=U("GSR", j, c0, c0 + n))
            self.wrel(k1)
            self.wrel(k3)
            w2v = s2[:, :].rearrange("p (f d) -> p f d", f=4)
            for (sl, rows) in STL:
                for hf in range(2):
                    b = self.bank()
                    pairs = [(S.GSR[:, j, sl * 128:sl * 128 + rows], w2v[:, j, hf * 512:(hf + 1) * 512]) for j in range(4)]
                    rd = [("w", self.slot_plan[k2])]
                    for j in range(4):
                        rd += U("GSR", j, sl * 128, sl * 128 + rows)
                    self.mm_group(b, 512, pairs, rd)
                    yres = ("yacc", yi, sl, hf)
                    if fg == 0:
                        def f(eng, sl=sl, hf=hf, b=b, Y=Y, rows=rows):
                            return eng.activation(out=Y[0:rows, sl, hf * 512:(hf + 1) * 512], in_=S.PS[b][0:rows, 0:512],
                                                  func=AF.Copy)
                        self.add("act", f, reads=[("ps", b)], writes=[yres])
                    else:
                        def f(eng, sl=sl, hf=hf, b=b, Y=Y, rows=rows):
                            return eng.tensor_tensor(out=Y[0:rows, sl, hf * 512:(hf + 1) * 512],
                                                     in0=Y[0:rows, sl, hf * 512:(hf + 1) * 512], in1=S.PS[b][0:rows, 0:512],
                                                     op=ALU.add)
                        self.add("dve", f, reads=[("ps", b), yres], writes=[yres])
            self.wrel(k2)

        nfull = BLK // 128
        rem = BLK - nfull * 128

        def f(eng, sem, row0=row0, Y=Y):
            last = eng.dma_start(out=S.YD[row0:row0 + nfull * 128, :].rearrange("(s p) d -> p s d", p=128),
                                 in_=Y[:, 0:nfull, :]).then_inc(sem, 16)
            if rem:
                last = eng.dma_start(out=S.YD[row0 + nfull * 128:row0 + BLK, :], in_=Y[0:rem, nfull, :]).then_inc(sem, 16)
            return last
        self.add("sp", f, reads=[("yacc", yi, sl, hf) for sl in range(NSLT) for hf in range(2)], writes=[("yd",)],
                 dma=("yst",), ndma=2 if rem else 1)

    def st_combine(self):
        S = self
        for tt in range(NTT):
            t0 = HALO + tt * 128
            g1 = self.gt()
            g2 = self.gt()

            def f(eng, sem, g1=g1, g2=g2, tt=tt):
                eng.indirect_dma_start(out=S.GT[g1], out_offset=None, in_=S.YD[:, :],
                                       in_offset=bass.IndirectOffsetOnAxis(ap=S.DI1[:, tt:tt + 1], axis=0),
                                       bounds_check=S.rb_pool, oob_is_err=False).then_inc(sem, 16)
                return eng.indirect_dma_start(out=S.GT[g2], out_offset=None, in_=S.YD[:, :],
                                              in_offset=bass.IndirectOffsetOnAxis(ap=S.DI2[:, tt:tt + 1], axis=0),
                                              bounds_check=S.rb_pool, oob_is_err=False).then_inc(sem, 16)
            self.add("pool", f, reads=[("yd",), ("di",), ("di1",)], writes=[("gt", g1), ("gt", g2)], dma=("gath", tt % 4),
                     ndma=2)

            def f(eng, g1=g1, tt=tt):
                return eng.activation(out=S.GT[g1], in_=S.GT[g1], func=AF.Copy, scale=S.P1[:, tt:tt + 1])
            self.add("act", f, reads=[("gt", g1), ("p12",)], writes=[("gt", g1)])

            def f(eng, g1=g1, g2=g2, tt=tt):
                return eng.scalar_tensor_tensor(out=S.GT[g1], in0=S.GT[g2], scalar=S.P2[:, tt:tt + 1], in1=S.GT[g1],
                                                op0=ALU.mult, op1=ALU.add)
            self.add("dve", f, reads=[("gt", g1), ("gt", g2), ("p12",)], writes=[("gt", g1)])
            for hf in range(2):
                b = self.bank()

                def f(eng, g1=g1, hf=hf, b=b):
                    last = None
                    for q in range(4):
                        kc = hf * 4 + q
                        last = eng.transpose(out=S.PS[b][:, q * 128:(q + 1) * 128], in_=S.GT[g1][:, kc * 128:(kc + 1) * 128],
                                             identity=S.IDF[:, :])
                    return last
                self.add("pe", f, reads=[("gt", g1), ("idf",)], writes=[("ps", b)])

                def f(eng, hf=hf, b=b, t0=t0):
                    return eng.tensor_tensor(out=S.X[:, hf * 4:(hf + 1) * 4, t0:t0 + 128],
                                             in0=S.X[:, hf * 4:(hf + 1) * 4, t0:t0 + 128],
                                             in1=S.PS[b][:, 0:512].rearrange("p (q t) -> p q t", q=4), op=ALU.add)
                xr = []
                for kc in range(hf * 4, hf * 4 + 4):
                    xr += U("X", kc, t0, t0 + 128)
                self.add("dve", f, reads=[("ps", b)] + xr, writes=xr)
            if tt % 4 == 3:
                self.st_final(True, only=tt // 4, finish=(tt == NTT - 1))

    def st_final(self, norm, only=None, finish=True):
        S = self
        mains = [(HALO + i * 512, 512) for i in range(4)]
        ov = self.outT.rearrange("(c p) t -> p c t", p=128)
        for bi, (c0, n) in enumerate(mains):
            if only is not None and bi != only:
                continue
            if norm:
                bk = self.bank()
                for c in range(NCH):
                    t = self.tb()

                    def f(eng, c=c, t=t, c0=c0, n=n):
                        return eng.activation(out=S.TB[t][:, 0:n], in_=S.X[:, c, c0:c0 + n], func=AF.Square)
                    self.add("act", f, reads=U("X", c, c0, c0 + n), writes=[("tb", t)])

                    def f(eng, c=c, t=t, n=n, bk=bk):
                        return eng.matmul(S.PS[bk][:, 0:n], lhsT=S.ONESB[:, :], rhs=S.TB[t][:, 0:n],
                                          start=(c == 0), stop=(c == NCH - 1))
                    self.add("pe", f, reads=[("tb", t), ("ones",)], writes=[("ps", bk)])
                t1 = self.tf()

                def f(eng, t1=t1, n=n, bk=bk):
                    return eng.activation(out=S.TF[t1][:, 0:n], in_=S.PS[bk][:, 0:n], func=AF.Sqrt,
                                          scale=1.0 / D, bias=S.EPSR[:, 0:1])
                self.add("act", f, reads=[("ps", bk), ("eps",)], writes=[("tf", t1)])

                def f(eng, t1=t1, n=n):
                    return eng.reciprocal(out=S.RSTD[:, 0:n], in_=S.TF[t1][:, 0:n])
                self.add("dve", f, reads=[("tf", t1)], writes=[("rstd",)])
                for c in range(NCH):
                    def f(eng, c=c, c0=c0, n=n):
                        return eng.scalar_tensor_tensor(out=S.X[:, c, c0:c0 + n], in0=S.X[:, c, c0:c0 + n],
                                                        scalar=S.PRM[:, P_FIN + c:P_FIN + c + 1],
                                                        in1=S.RSTD[:, 0:n], op0=ALU.mult, op1=ALU.mult)
                    self.add("dve", f, reads=U("X", c, c0, c0 + n) + [("rstd",), ("prm",)],
                             writes=U("X", c, c0, c0 + n))
            rd = []
            for c in range(NCH):
                rd += U("X", c, c0, c0 + n)

            def f(eng, sem, c0=c0, n=n):
                return eng.dma_start(out=ov[:, :, c0 - HALO:c0 - HALO + n], in_=S.X[:, :, c0:c0 + n]).then_inc(sem, 16)
            self.add("sp", f, reads=rd, writes=[("out", bi)], dma=("out", bi))
        if not finish:
            return

        def f(eng):
            return None
        self.add("sp", f, reads=[("out", i) for i in range(4)], writes=[("done",)])

    def program(self):
        S = self
        mains = [(HALO + i * 512, 512) for i in range(4)]
        self.st_init()
        order = ["mix0", "l0", "mix1", "full"]
        lim = order.index(self.stop)
        self.ring_n = NS - 1
        self.mg_live = True
        self.st_mixer_half(0, 1, 32, 64)
        self.st_mixer_half(0, 0, 0, 32)
        self.mg_live = False
        if lim >= 1:
            if not self.dry:
                self.S.barrier()
            fb = [(32, 32)] + mains
            self.ring_n = NS
            self.st_norm(fb, 0, S.H2, "H2", P_NFG)
            self.st_ffn(0, fb, [(self.dense_w1[0], self.dense_w3[0], self.dense_w2[0])])
        if lim >= 2:
            if not self.dry:
                self.S.barrier()
            self.ring_n = NS - 1
            self.mg_live = True
            self.st_mixer_half(1, 1, 32, 64)
            self.st_mixer_half(1, 0, 32, 64)
            self.mg_live = False
        if lim >= 3:
            if not self.dry:
                self.S.barrier()
            self.ring_n = NS
            self.st_norm(mains, 0, S.H2, "H2", P_LAYER + P_NFG, router=True)
            self.st_route()
            self.st_scatter()
            if not self.dry:
                self.S.barrier()
            for e in range(NE):
                self.st_pass(e, 0, e % 2, prefetch=(e > 0, e + 1 if e + 1 < NE else None))
            yi = 0
            for e in range(NE):
                for r in range(1, NRND):
                    self.cur_region = (e, r)
                    self.st_pass(e, r, yi)
                    yi ^= 1
                    self.cur_region = None
            if not self.dry:
                self.S.barrier()
            self.st_combine()
        else:
            self.st_final(norm=False)

    def build(self):
        nc = self.nc
        self.declare()
        self.EPSR = self.es.enter_context(nc.sbuf_tensor("epsr", [128, 1], F32))
        self.EPSL = self.es.enter_context(nc.sbuf_tensor("epsl", [128, 1], F32))
        self.dry = True
        self.slot_plan = []
        self.rr = 0
        self.ring_n = NS
        self.reset_rot()
        self.program()
        last = {}
        self.prev_same = []
        for j, sl in enumerate(self.slot_plan):
            self.prev_same.append(last.get(sl))
            last[sl] = j
        self.dry = False
        self.reset_rot()
        S = self

        def f(eng):
            eng.memset(S.EPSR[:, :], RMS_EPS)
            return eng.memset(S.EPSL[:, :], LN_EPS)
        self.add("dve", f, writes=[("eps",)])
        self.program()
        assert self.wk == len(self.plan)
        self.emit()
        return nc

    def emit(self):
        nc = self.nc
        ops = self.S.ops
        es = self.es
        eng_sem = {}
        for e in Sched.COMPUTE:
            eng_sem[e] = es.enter_context(nc.semaphore(f"s_{e}"))
        dma_sem = {}
        for op in ops:
            if op.dma is not None and op.dma not in dma_sem:
                dma_sem[op.dma] = es.enter_context(nc.semaphore("d_" + "_".join(str(x) for x in op.dma)))
        if getattr(self, "cnti_op", None) is not None:
            ops[self.cnti_op].signals = True
        cnt = {e: 0 for e in Sched.COMPUTE}
        for op in ops:
            if op.dma is not None:
                op.sem = dma_sem[op.dma]
            elif op.eng in cnt:
                if op.signals:
                    cnt[op.eng] += 1
                    op.val = cnt[op.eng]
                op.sem = eng_sem[op.eng]
            else:
                assert not op.signals, "queue-engine non-dma op cannot signal"
        by = {e: [] for e in ("pe", "act", "dve", "pool", "sp")}
        for op in ops:
            by[op.eng].append(op)
        block = es.enter_context(nc.Block())

        cnti_op = getattr(self, "cnti_op", None)
        S = self

        def emit_op(eng, op, known):
            for d in op.deps:
                Dp = ops[d]
                key = id(Dp.sem)
                if known.get(key, 0) >= Dp.val:
                    continue
                eng.wait_ge(Dp.sem, Dp.val)
                known[key] = Dp.val
            if op.dma is not None:
                op.fn(eng, op.sem)
            else:
                inst = op.fn(eng)
                if op.signals:
                    inst.then_inc(op.sem, 1)

        def emit_comp(eng, ename, grp):
            nsig = sum(1 for op in grp if op.dma is None and op.signals)
            if nsig:
                eng.drain().then_inc(eng_sem[ename], nsig)
            dk = {}
            for op in grp:
                if op.dma is not None:
                    dk.setdefault(op.dma, []).append(op)
            for kd, lst2 in dk.items():
                before = lst2[0].val - 16 * lst2[0].ndma
                if before > 0:
                    eng.wait_ge(dma_sem[kd], before)
                eng.sem_inc(dma_sem[kd], 16 * sum(o.ndma for o in lst2))

        def emit_region(eng, ename, grp, known, rc):
            e, r = grp[0].region
            eng.reg_load(rc, S.CNTI[0:1, e:e + 1])
            with eng.If_lt(rc, r * BLK + 1):
                emit_comp(eng, ename, grp)
            with eng.Else():
                k2 = dict(known)
                for op in grp:
                    emit_op(eng, op, k2)

        def emit_run(eng, ename, run_ops, known, rc):
            Dp = ops[cnti_op]
            key = id(Dp.sem)
            if known.get(key, 0) < Dp.val:
                eng.wait_ge(Dp.sem, Dp.val)
                known[key] = Dp.val
            eng.reg_load(rc, S.CMAXI[0:1, 0:1])
            with eng.If_lt(rc, BLK + 1):
                emit_comp(eng, ename, run_ops)
            with eng.Else():
                i = 0
                while i < len(run_ops):
                    j = i
                    while j < len(run_ops) and run_ops[j].region == run_ops[i].region:
                        j += 1
                    emit_region(eng, ename, run_ops[i:j], known, rc)
                    i = j

        def run(eng, lst, ename):
            known = {}
            with eng.register("rc_" + ename) as rc:
                i = 0
                while i < len(lst):
                    op = lst[i]
                    if op.region is None:
                        emit_op(eng, op, known)
                        i += 1
                        continue
                    j = i
                    while j < len(lst) and lst[j].region is not None:
                        j += 1
                    emit_run(eng, ename, lst[i:j], known, rc)
                    i = j

        @block.tensor
        def _(eng):
            run(eng, by["pe"], "pe")

        @block.scalar
        def _(eng):
            run(eng, by["act"], "act")

        @block.vector
        def _(eng):
            run(eng, by["dve"], "dve")

        @block.gpsimd
        def _(eng):
            with eng.register("rb_rows") as rb:
                eng.reg_mov(rb, NE * CAP - 1)
                S.rb_pool = rb
                run(eng, by["pool"], "pool")

        @block.sync
        def _(eng):
            run(eng, by["sp"], "sp")
        es.close()


def pack_params(inp, core):
    P = np.zeros((128, NPRM), np.float32)

    def vec(v):
        return np.ascontiguousarray(v.reshape(NCH, 128).T)
    for l in range(2):
        b = l * P_LAYER
        P[:, b + P_NMG:b + P_NMG + 8] = vec(inp["norm_mix_g"][l])
        P[:, b + P_SCW:b + P_SCW + 24] = inp["sc_conv_w"][l].reshape(3, NCH, 128).transpose(2, 1, 0).reshape(128, 24)
        P[:, b + P_CFW:b + P_CFW + 248] = inp["cf_conv_w"][l].reshape(31, NCH, 128).transpose(2, 1, 0).reshape(128, 248)
        P[:, b + P_CFB:b + P_CFB + 8] = vec(inp["cf_conv_b"][l])
        P[:, b + P_LNG:b + P_LNG + 8] = vec(inp["cf_ln_g"][l])
        P[:, b + P_LNB:b + P_LNB + 8] = vec(inp["cf_ln_b"][l])
        P[:, b + P_PSC:b + P_PSC + 8] = vec(inp["pool_scale"][l])
        P[:, b + P_NFG:b + P_NFG + 8] = vec(inp["norm_ffn_g"][l])
    P[:, P_FIN:P_FIN + 8] = vec(inp["norm_final_g"])
    P[:, P_ROUT:P_ROUT + 64] = inp["moe_router"][0].reshape(NCH, 128, NE).transpose(1, 0, 2).reshape(128, 64)
    first = (core % 4 == 0)
    P[:, P_MASK:P_MASK + 64] = 0.0 if first else 1.0
    for g, w in enumerate(WINS):
        for i in range(16):
            cntv = min(i + 1, w) if first else w
            P[:, P_INVC + g * 16 + i] = 1.0 / cntv
    P[:, P_ECAP:P_ECAP + NE] = (np.arange(NE, dtype=np.float32) * CAP)[None, :]
    P[:, P_UT:P_UT + 128] = np.triu(np.ones((128, 128), np.float32), 1)
    return P


_CACHE = {}


def run(inputs, stop="full"):
    inp = {k: np.asarray(v, dtype=np.float32) for k, v in inputs.items()}
    if stop not in _CACHE:
        _CACHE[stop] = Builder(stop).build()
    nc = _CACHE[stop]
    x = inp["x"]
    shared = {k: np.ascontiguousarray(inp[k]) for k in
              ("w_in", "w_sc_out", "w_cf_out", "w_pool_out", "w_o", "pool_w", "dense_w1", "dense_w3", "dense_w2",
               "moe_w1", "moe_w3", "moe_w2")}
    in_maps = []
    for core in range(8):
        b, q = divmod(core, 4)
        t0 = q * TOK
        xs = np.zeros((TT, D), np.float32)
        if q > 0:
            xs[:HALO] = x[b, t0 - HALO:t0]
        xs[HALO:] = x[b, t0:t0 + TOK]
        m = dict(shared)
        m["xT"] = np.ascontiguousarray(xs.T)
        m["prm"] = pack_params(inp, core)
        in_maps.append(m)
    res = run_bass_kernel_spmd(nc, in_maps, core_ids=list(range(8)))
    out = np.zeros((2, SEQ, D), np.float32)
    for core in range(8):
        b, q = divmod(core, 4)
        out[b, q * TOK:(q + 1) * TOK] = res.results[core]["outT"].T
    return out


def kernel(**inputs):
    return run(inputs, "full")
```

```python
import numpy as np
import concourse.bass as bass
import concourse.mybir as mybir
from concourse.bass_utils import run_bass_kernel_spmd
from contextlib import ExitStack

F32 = mybir.dt.float32
BF16 = mybir.dt.bfloat16
I32 = mybir.dt.int32
AF = mybir.ActivationFunctionType
ALU = mybir.AluOpType
AX = mybir.AxisListType

D = 1024
NCH = 8
SEQ = 8192
TOK = 2048
HALO = 64
TT = TOK + HALO
TH = 1088
DFF = 3584
NFG = 7
NE = 8
NS = 5
NTF = 5
NTB = 2
NSLT = 5
BLK = 576
STL = [(sl, min(128, BLK - sl * 128)) for sl in range(NSLT)]
assert (NSLT - 1) * 128 < BLK <= NSLT * 128 and BLK % 32 == 0
NRND = (TOK + BLK - 1) // BLK
CAP = NRND * BLK
CB = [(c, min(512, BLK - c)) for c in range(0, BLK, 512)]
NTT = TOK // 128
WINS = (2, 4, 8, 16)
RMS_EPS = 1e-6
LN_EPS = 1e-5

C_SCB, C_SCC, C_SCX, C_CFV, C_CFG, C_PU, C_GA, C_GB, C_GC = [i * 1024 for i in range(9)]

P_LAYER = 320
P_NMG, P_SCW, P_CFW, P_CFB, P_LNG, P_LNB, P_PSC, P_NFG = 0, 8, 32, 280, 288, 296, 304, 312
P_FIN = 2 * P_LAYER
P_ROUT = P_FIN + 8
P_MASK = P_ROUT + 64
P_INVC = P_MASK + 64
P_ECAP = P_INVC + 64
P_UT = P_ECAP + 8
NPRM = P_UT + 128


class Op:
    __slots__ = ("idx", "eng", "fn", "deps", "signals", "val", "sem", "dma", "writes", "ndma", "region")

    def __init__(self, idx, eng, fn):
        self.idx = idx
        self.eng = eng
        self.fn = fn
        self.deps = []
        self.signals = False
        self.val = 0
        self.sem = None
        self.dma = None
        self.writes = ()
        self.ndma = 0
        self.region = None


class Sched:
    COMPUTE = ("pe", "act", "dve", "pool")

    def __init__(self):
        self.ops = []
        self.last_w = {}
        self.readers = {}
        self.dma_count = {}
        self.dma_last = {}
        self.bar = None
        self.bar_done = set()
        self.last_on = {}

    def barrier(self):
        self.bar = [self.last_on[e] for e in self.COMPUTE if e in self.last_on]
        self.bar_done = set()

    def add(self, eng, fn, reads=(), writes=(), dma=None, ndma=1, region=None):
        idx = len(self.ops)
        op = Op(idx, eng, fn)
        op.region = region
        deps = {}

        def need(d, raw):
            Dp = self.ops[d]
            if Dp.dma is not None:
                key = ("dma", Dp.dma)
            else:
                if Dp.eng == eng and eng == "pe":
                    return
                key = ("eng", Dp.eng)
            if key not in deps or deps[key] < d:
                deps[key] = d

        for r in reads:
            d = self.last_w.get(r)
            if d is not None:
                need(d, True)
        for w in writes:
            d = self.last_w.get(w)
            if d is not None:
                need(d, False)
            rd = self.readers.get(w)
            if rd:
                for d in rd.values():
                    need(d, False)
        if dma is not None:
            d = self.dma_last.get(dma)
            if d is not None:
                need(d, False)
        if self.bar is not None and eng in self.COMPUTE and eng not in self.bar_done:
            self.bar_done.add(eng)
            for d in self.bar:
                if self.ops[d].eng != eng:
                    need(d, False)
        for d in deps.values():
            self.ops[d].signals = True
        op.deps = sorted(deps.values())
        op.writes = frozenset(writes)
        if dma is not None:
            op.dma = dma
            op.ndma = ndma
            self.dma_count[dma] = self.dma_count.get(dma, 0) + ndma
            op.val = 16 * self.dma_count[dma]
            self.dma_last[dma] = idx
        for r in reads:
            self.readers.setdefault(r, {})[eng if dma is None else ("dma", dma)] = idx
        for w in writes:
            self.last_w[w] = idx
            self.readers[w] = {}
        self.ops.append(op)
        if dma is None:
            self.last_on[eng] = idx
        return idx


def U(name, ch, c0, c1):
    return [(name, ch, u) for u in range(c0 // 32, (c1 + 31) // 32)]


class Builder:
    def __init__(self, stop="full", debug=False):
        self.stop = stop
        self.debug = debug
        self.nc = bass.Bass("TRN2", target_bir_lowering=False)
        self.S = Sched()
        self.dry = False
        self.plan = []
        self.es = ExitStack()

    def declare(self):
        nc = self.nc

        def din(name, shape):
            return nc.dram_tensor(name, list(shape), F32, kind="ExternalInput").ap()

        self.xT = din("xT", [D, TT])
        self.prm_d = din("prm", [128, NPRM])
        self.w_in = din("w_in", [2, D, 9216])
        self.w_sc_out = din("w_sc_out", [2, D, D])
        self.w_cf_out = din("w_cf_out", [2, D, D])
        self.w_pool_out = din("w_pool_out", [2, D, D])
        self.w_o = din("w_o", [2, D, D])
        self.pool_w = din("pool_w", [2, 4, 256, 256])
        self.dense_w1 = din("dense_w1", [1, D, DFF])
        self.dense_w3 = din("dense_w3", [1, D, DFF])
        self.dense_w2 = din("dense_w2", [1, DFF, D])
        self.moe_w1 = din("moe_w1", [1, NE, D, DFF])
        self.moe_w3 = din("moe_w3", [1, NE, D, DFF])
        self.moe_w2 = din("moe_w2", [1, NE, DFF, D])
        self.outT = nc.dram_tensor("outT", [D, TOK], F32, kind="ExternalOutput").ap()

        def sb(name, shape, dt):
            return self.es.enter_context(nc.sbuf_tensor(name, list(shape), dt))

        self.X = sb("X", [128, NCH, TT], F32)
        self.WORK = sb("WORK", [128, 13056], F32)
        hw = 4352
        self.H = self.WORK[:, 0:hw].bitcast(BF16).rearrange("p (c t) -> p c t", c=NCH)
        self.T1 = self.WORK[:, hw:2 * hw].bitcast(BF16).rearrange("p (c t) -> p c t", c=NCH)
        self.T2 = self.WORK[:, 2 * hw:3 * hw].bitcast(BF16).rearrange("p (c t) -> p c t", c=NCH)
        self.H2 = self.WORK[:, 0:8448].bitcast(BF16).rearrange("p (c t) -> p c t", c=NCH)
        self.GS = self.WORK[:, 8448:8448 + 4224].bitcast(BF16).rearrange("p (c t) -> p c t", c=4)
        self.MGB = sb("mgb", [128, NCH * TH], BF16)
        self.MG = self.MGB[:, :].rearrange("p (c t) -> p c t", c=NCH)
        self.WS = [sb(f"ws{i}", [128, 4096], BF16)[:, :] for i in range(NS - 1)] + [self.MGB[:, 0:4096]]
        self.TF = [sb(f"tf{i}", [128, 512], F32) for i in range(NTF)]
        self.TB = [sb(f"tb{i}", [128, 512], BF16) for i in range(NTB)]
        self.MEAN = sb("mean", [128, 512], F32)
        self.RSTD = sb("rstd", [128, 512], F32)
        self.DG = sb("diag", [128, 3968], F32)
        dgb = self.DG[:, 0:3968].bitcast(BF16)
        self.DIAG = [dgb[:, 0:3968].rearrange("p (k m) -> p k m", k=31),
                     dgb[:, 3968:7936].rearrange("p (k m) -> p k m", k=31)]
        dga = self.DG[:, :].bitcast(BF16)
        self.HT = [dga[:, i * 1024:(i + 1) * 1024] for i in range(4)]
        self.HGS = dga[:, 0:NSLT * 1024].rearrange("p (s d) -> p s d", s=NSLT)
        self.GSR = dga[:, NSLT * 1024:NSLT * 1024 + 4 * BLK].rearrange("p (j t) -> p j t", j=4)
        hgw = NCH * BLK // 2
        self.HG = self.WORK[:, 0:hgw].bitcast(BF16).rearrange("p (c t) -> p c t", c=NCH)
        yw = NSLT * 1024
        self.YACC = [self.WORK[:, hgw + i * yw:hgw + (i + 1) * yw].rearrange("p (s d) -> p s d", s=NSLT)
                     for i in range(2)]
        assert hgw + 2 * yw <= 13056 and NSLT * 1024 + 4 * BLK <= 7936
        self.GT = [self.WORK[:, i * 1024:(i + 1) * 1024] for i in range(8)]
        if self.debug:
            self.HD = nc.dram_tensor("hd_scr", [NE * CAP, D], BF16, kind="ExternalOutput").ap()
            self.YD = nc.dram_tensor("yd_scr", [NE * CAP, D], F32, kind="ExternalOutput").ap()
            self.DBG = nc.dram_tensor("dbg_i", [128, 40], I32, kind="ExternalOutput").ap()
        else:
            self.HD = nc.dram_tensor("hd_scr", [NE * CAP, D], BF16).ap()
            self.YD = nc.dram_tensor("yd_scr", [NE * CAP, D], F32).ap()
        self.RT = sb("rt", [128, 4 * NTT * NE + NTT * NE // 2], F32)
        self.ZT = self.RT[:, 0:512].bitcast(BF16)
        self.UTB = sb("utb", [128, 128], BF16)
        nte = NTT * NE

        def r3(lo):
            return self.RT[:, lo:lo + nte].rearrange("p (t e) -> p t e", e=NE)
        self.CNT, self.OFF, self.POS, self.TMP = r3(0), r3(nte), r3(2 * nte), r3(3 * nte)
        self.MSKB = self.RT[:, 4 * nte:4 * nte + nte // 2].bitcast(BF16).rearrange("p (t e) -> p t e", e=NE)
        self.D1 = sb("d1", [128, NTT], F32)
        self.D2 = sb("d2", [128, NTT], F32)
        self.DI1 = sb("di1", [128, NTT], I32)
        self.DI2 = sb("di2", [128, NTT], I32)
        self.CNTI = sb("cnti", [128, NE], I32)
        self.CMAXF = sb("cmaxf", [128, 1], F32)
        self.CMAXI = sb("cmaxi", [128, 1], I32)
        self.IDF = sb("idf", [128, 128], F32)
        self.IDB = sb("idb", [128, 128], BF16)
        self.ONESB = sb("onesb", [128, 128], BF16)
        self.PRM = sb("prmsb", [128, NPRM], F32)
        self.LG = sb("lg", [128, 16, NE], F32)
        self.L2 = sb("l2", [128, 16, NE], F32)
        self.EQ1 = sb("eq1", [128, 16, NE], F32)
        self.EQ2 = sb("eq2", [128, 16, NE], F32)
        self.M1 = sb("m1", [128, 16], F32)
        self.M2 = sb("m2", [128, 16], F32)
        self.P1 = sb("p1", [128, 16], F32)
        self.P2 = sb("p2", [128, 16], F32)
        self.PS = [self.es.enter_context(nc.psum_tensor(f"ps{i}", [128, 512], F32)) for i in range(8)]
        self.sems = {}

    def reset_rot(self):
        self.ib = 0
        self.iht = 0
        self.igt = 0
        self.mg_live = False
        self.cur_region = None
        self.itf = 0
        self.itb = 0
        self.wk = 0
        self.w_issued = 0
        self.w_rel = []
        self.idg = 0

    def bank(self):
        i = self.ib % 8
        self.ib += 1
        return i

    def tf(self):
        i = self.itf % NTF
        self.itf += 1
        return i

    def tb(self):
        i = self.itb % NTB
        self.itb += 1
        return i

    def ht(self):
        i = self.iht % 4
        self.iht += 1
        return i

    def gt(self):
        i = self.igt % 8
        self.igt += 1
        return i

    def add(self, eng, fn, reads=(), writes=(), dma=None, ndma=1):
        if self.dry:
            return
        if self.cur_region is not None:
            reads = list(reads) + [("cnti",)]
        return self.S.add(eng, fn, reads, writes, dma, ndma, region=self.cur_region)

    def wget(self, issue_fn):
        k = self.wk
        self.wk += 1
        if self.dry:
            slot = self.rr % self.ring_n
            self.rr = slot + 1
            self.plan.append((issue_fn, self.cur_region))
            self.slot_plan.append(slot)
            return k, self.WS[slot]
        self.pump()
        assert self.w_issued > k, "weight ring deadlock: too many slots held"
        return k, self.WS[self.slot_plan[k]]

    def wrel(self, k):
        if self.dry:
            return
        self.w_rel.append(k)
        self.pump()

    def pump(self):
        while self.w_issued < len(self.plan):
            j = self.w_issued
            if self.prev_same[j] is not None and self.prev_same[j] not in self.w_rel:
                break
            slot = self.slot_plan[j]
            if slot == NS - 1 and self.mg_live:
                break
            fn, reg = self.plan[j]
            ws = self.WS[slot]

            def f(eng, sem, fn=fn, ws=ws):
                return fn(eng, ws, sem)
            self.S.add("pool", f, reads=([("cnti",)] if reg is not None else ()), writes=[("w", slot)],
                       dma=("w", slot), ndma=fn.ndma, region=reg)
            self.w_issued += 1

    def ld_cols(self, src2d, c0, ncols=512):
        def fn(eng, ws, sem):
            dst = ws[:, 0:8 * ncols].rearrange("p (k c) -> p k c", k=8)
            src = src2d[:, c0:c0 + ncols].rearrange("(k p) c -> p k c", p=128)
            return eng.dma_start(out=dst, in_=src).then_inc(sem, 16)
        fn.ndma = 1
        return fn

    def ld_rows(self, src2d, r0):
        def fn(eng, ws, sem):
            dst = ws[:, :].rearrange("p (f d) -> p f d", f=4)
            src = src2d[r0:r0 + 512, :].rearrange("(f p) d -> p f d", p=128)
            return eng.dma_start(out=dst, in_=src).then_inc(sem, 16)
        fn.ndma = 1
        return fn

    def ld_poolw(self, src3d):
        def fn(eng, ws, sem):
            dst = ws[:, 0:2048].rearrange("p (k c) -> p k c", k=8)
            src = src3d.rearrange("g (k p) e -> p (g k) e", p=128)
            return eng.dma_start(out=dst, in_=src).then_inc(sem, 16)
        fn.ndma = 1
        return fn

    def mm_group(self, bank, n, pairs, reads):
        ps = self.PS[bank]
        nc = self.nc

        def fn(eng, pairs=pairs, ps=ps, n=n):
            last = None
            m = pairs[0][0].shape[-1]
            for i, (l, r) in enumerate(pairs):
                last = eng.matmul(ps[0:m, 0:n], lhsT=l, rhs=r, start=(i == 0), stop=(i == len(pairs) - 1))
            return last
        self.add("pe", fn, reads=reads, writes=[("ps", bank)])

    def proj(self, wsl, k, j, rhs_buf, rname, c0, n, bank, extra=()):
        wv = wsl[:, 0:4096].rearrange("p (k c) -> p k c", k=8)
        pairs = [(wv[:, kc, j * 128:(j + 1) * 128], rhs_buf[:, kc, c0:c0 + n]) for kc in range(NCH)]
        reads = [("w", self.slot_plan[k])] + list(extra)
        for kc in range(NCH):
            reads += U(rname, kc, c0, c0 + n)
        self.mm_group(bank, n, pairs, reads)

    def st_init(self):
        nc = self.nc
        S = self
        for (lo, hi) in ((1024, TT), (0, 1024)):
            for c in range(NCH):
                def f(eng, sem, c=c, lo=lo, hi=hi):
                    return eng.dma_start(out=S.X[:, c, lo:hi], in_=S.xT[c * 128:(c + 1) * 128, lo:hi]).then_inc(sem, 16)
                self.add("sp", f, writes=U("X", c, lo, hi), dma=("xl", c, lo))

        def f(eng, sem):
            return eng.dma_start(out=S.PRM[:, :], in_=S.prm_d[:, :]).then_inc(sem, 16)
        self.add("sp", f, writes=[("prm",)], dma=("prm",))

        def f(eng):
            eng.memset(S.ONESB[:, :], 1.0)
            return eng.memset(S.IDF[:, :], 0.0)
        self.add("pool", f, writes=[("idf0",), ("ones",)])

        def f(eng):
            return eng.affine_select(out=S.IDF[:, :], in_=S.IDF[:, :], compare_op=ALU.not_equal, fill=1.0,
                                     base=0, pattern=[[-1, 128]], channel_multiplier=1)
        self.add("pool", f, reads=[("idf0",)], writes=[("idf",)])

        def f(eng):
            return eng.tensor_copy(out=S.IDB[:, :], in_=S.IDF[:, :])
        self.add("pool", f, reads=[("idf",)], writes=[("idb",)])

        if self.stop != "full":
            return

        def f(eng):
            eng.memset(S.ZT[:, :], 0.0)
            return eng.tensor_copy(out=S.UTB[:, :], in_=S.PRM[:, P_UT:P_UT + 128])
        self.add("pool", f, reads=[("prm",)], writes=[("zt",), ("utb",)])
        assert (NE * CAP) % 128 == 0
        hdv = S.HD.rearrange("(q p) d -> q p d", p=128)
        nq = NE * CAP // 128
        per = 20
        for q0 in range(0, nq, per):
            def f(eng, sem, q0=q0):
                last = None
                for q in range(q0, min(nq, q0 + per)):
                    last = eng.dma_start(out=hdv[q], in_=S.ZT[:, :]).then_inc(sem, 16)
                return last
            self.add("sp", f, reads=[("zt",)], writes=[("hdz",)], dma=("zi",), ndma=min(nq, q0 + per) - q0)

    def st_norm(self, blocks, xoff, hbuf, hname, gcol, mask=False, router=False):
        S = self
        for (c0, n) in blocks:
            g0 = xoff + c0
            bk = self.bank()
            for c in range(NCH):
                t = self.tb()

                def f(eng, c=c, t=t, g0=g0, n=n):
                    return eng.activation(out=S.TB[t][:, 0:n], in_=S.X[:, c, g0:g0 + n], func=AF.Square)
                self.add("act", f, reads=U("X", c, g0, g0 + n), writes=[("tb", t)])

                def f(eng, c=c, t=t, n=n, bk=bk):
                    return eng.matmul(S.PS[bk][:, 0:n], lhsT=S.ONESB[:, :], rhs=S.TB[t][:, 0:n],
                                      start=(c == 0), stop=(c == NCH - 1))
                self.add("pe", f, reads=[("tb", t), ("ones",)], writes=[("ps", bk)])
            t1 = self.tf()

            def f(eng, t1=t1, n=n, bk=bk):
                return eng.activation(out=S.TF[t1][:, 0:n], in_=S.PS[bk][:, 0:n], func=AF.Sqrt,
                                      scale=1.0 / D, bias=S.EPSR[:, 0:1])
            self.add("act", f, reads=[("ps", bk), ("eps",)], writes=[("tf", t1)])

            def f(eng, t1=t1, n=n):
                return eng.reciprocal(out=S.RSTD[:, 0:n], in_=S.TF[t1][:, 0:n])
            self.add("dve", f, reads=[("tf", t1)], writes=[("rstd",)])
            if mask and c0 < HALO:
                def f(eng, c0=c0, n=n):
                    return eng.tensor_tensor(out=S.RSTD[:, 0:n], in0=S.RSTD[:, 0:n],
                                             in1=S.PRM[:, P_MASK + c0:P_MASK + c0 + n], op=ALU.mult)
                self.add("dve", f, reads=[("rstd",), ("prm",)], writes=[("rstd",)])
            if router:
                rb = self.bank()
            for c in range(NCH):
                if not router:
                    def f(eng, c=c, c0=c0, g0=g0, n=n):
                        return eng.scalar_tensor_tensor(out=hbuf[:, c, c0:c0 + n], in0=S.X[:, c, g0:g0 + n],
                                                        scalar=S.PRM[:, gcol + c:gcol + c + 1],
                                                        in1=S.RSTD[:, 0:n], op0=ALU.mult, op1=ALU.mult)
                    self.add("dve", f, reads=U("X", c, g0, g0 + n) + [("rstd",), ("prm",)],
                             writes=U(hname, c, c0, c0 + n))
                else:
                    t2 = self.tf()

                    def f(eng, c=c, g0=g0, n=n, t2=t2):
                        return eng.scalar_tensor_tensor(out=S.TF[t2][:, 0:n], in0=S.X[:, c, g0:g0 + n],
                                                        scalar=S.PRM[:, gcol + c:gcol + c + 1],
                                                        in1=S.RSTD[:, 0:n], op0=ALU.mult, op1=ALU.mult)
                    self.add("dve", f, reads=U("X", c, g0, g0 + n) + [("rstd",), ("prm",)], writes=[("tf", t2)])

                    def f(eng, c=c, c0=c0, n=n, t2=t2):
                        return eng.activation(out=hbuf[:, c, c0:c0 + n], in_=S.TF[t2][:, 0:n], func=AF.Copy)
                    self.add("act", f, reads=[("tf", t2)], writes=U(hname, c, c0, c0 + n))

                    def f(eng, c=c, n=n, t2=t2, rb=rb):
                        return eng.matmul(S.PS[rb][0:NE, 0:n], lhsT=S.PRM[:, P_ROUT + c * NE:P_ROUT + (c + 1) * NE],
                                          rhs=S.TF[t2][:, 0:n], start=(c == 0), stop=(c == NCH - 1))
                    self.add("pe", f, reads=[("tf", t2), ("prm",)], writes=[("ps", rb)])
            if router:
                t3 = self.tf()

                def f(eng, n=n, t3=t3, rb=rb):
                    return eng.activation(out=S.TF[t3][0:NE, 0:n], in_=S.PS[rb][0:NE, 0:n], func=AF.Copy)
                self.add("act", f, reads=[("ps", rb)], writes=[("tf", t3)])
                tb_ = self.bank()
                nt = n // 128

                def f(eng, t3=t3, tb_=tb_, nt=nt):
                    last = None
                    for j in range(nt):
                        last = eng.transpose(out=S.PS[tb_][:, j * NE:(j + 1) * NE],
                                             in_=S.TF[t3][0:NE, j * 128:(j + 1) * 128],
                                             identity=S.IDF[0:NE, 0:NE])
                    return last
                self.add("pe", f, reads=[("tf", t3), ("idf",)], writes=[("ps", tb_)])
                j0 = (g0 - HALO) // 128

                def f(eng, tb_=tb_, nt=nt, j0=j0):
                    return eng.tensor_copy(out=S.LG[:, j0:j0 + nt, :],
                                           in_=S.PS[tb_][:, 0:nt * NE].rearrange("p (j e) -> p j e", e=NE))
                self.add("dve", f, reads=[("ps", tb_)], writes=[("lg", j0)])

    def blocks_of(self, first):
        b = []
        if first < HALO:
            b.append((first, HALO - first))
        b += [(HALO, 512), (HALO + 512, 512)]
        return b

    def st_outproj(self, l, w_out, gbase, post, xoff, first, last):
        S = self
        mgres = [("w", NS - 1)]
        pending = None
        for grp in range(2):
            kw, wsl = self.wget(self.ld_cols(w_out[l], grp * 512))
            kg, gsl = self.wget(self.ld_cols(self.w_in[l], gbase + grp * 512))
            for (c0, n) in post:
                for j in range(4):
                    d = grp * 4 + j
                    by = self.bank()
                    self.proj(wsl, kw, j, S.T1, "T1", c0, n, by)
                    bg = self.bank()
                    self.proj(gsl, kg, j, S.H, "H", c0, n, bg)
                    t = self.tf()

                    def f(eng, t=t, n=n, bg=bg):
                        return eng.activation(out=S.TF[t][:, 0:n], in_=S.PS[bg][:, 0:n], func=AF.Sigmoid)
                    self.add("act", f, reads=[("ps", bg)], writes=[("tf", t)])
                    if first:
                        def f(eng, t=t, n=n, by=by, d=d, c0=c0):
                            return eng.tensor_tensor(out=S.MG[:, d, c0:c0 + n], in0=S.PS[by][:, 0:n],
                                                     in1=S.TF[t][:, 0:n], op=ALU.mult)
                        self.add("dve", f, reads=[("ps", by), ("tf", t)], writes=U("MG", d, c0, c0 + n) + mgres)
                    else:
                        def f(eng, t=t, n=n, by=by):
                            return eng.tensor_tensor(out=S.TF[t][:, 0:n], in0=S.PS[by][:, 0:n],
                                                     in1=S.TF[t][:, 0:n], op=ALU.mult)
                        self.add("dve", f, reads=[("ps", by), ("tf", t)], writes=[("tf", t)])
                        if pending is not None:
                            pending()

                        def pend(t=t, n=n, d=d, c0=c0):
                            def f(eng):
                                return eng.tensor_tensor(out=S.MG[:, d, c0:c0 + n], in0=S.MG[:, d, c0:c0 + n],
                                                         in1=S.TF[t][:, 0:n], op=ALU.add)
                            self.add("dve", f, reads=[("tf", t)] + U("MG", d, c0, c0 + n),
                                     writes=U("MG", d, c0, c0 + n) + mgres)
                        pending = pend
            self.wrel(kw)
            self.wrel(kg)
        if pending is not None:
            pending()
        if not last:
            return
        for grp in range(2):
            ko, osl = self.wget(self.ld_cols(self.w_o[l], grp * 512))
            for j in range(4):
                e = grp * 4 + j
                for (c0, n) in post:
                    b = self.bank()
                    self.proj(osl, ko, j, S.MG, "MG", c0, n, b, extra=mgres)
                    g0 = xoff + c0

                    def f(eng, e=e, g0=g0, n=n, b=b):
                        return eng.tensor_tensor(out=S.X[:, e, g0:g0 + n], in0=S.X[:, e, g0:g0 + n],
                                                 in1=S.PS[b][:, 0:n], op=ALU.add)
                    self.add("dve", f, reads=[("ps", b)] + U("X", e, g0, g0 + n), writes=U("X", e, g0, g0 + n))
            self.wrel(ko)

    def build_diag(self, col0, ntap):
        S = self
        db = self.idg % 2
        self.idg += 1

        def f(eng, db=db, col0=col0, ntap=ntap):
            return eng.tensor_tensor(out=S.DIAG[db][:, 0:ntap, :],
                                     in0=S.IDB[:, :].unsqueeze(1).to_broadcast([128, ntap, 128]),
                                     in1=S.PRM[:, col0:col0 + ntap].unsqueeze(2).to_broadcast([128, ntap, 128]),
                                     op=ALU.mult)
        self.add("dve", f, reads=[("idb",), ("prm",)], writes=[("diag", db, 0)])
        return db

    def st_mixer_half(self, l, half, ci0, po0):
        S = self
        xoff = 0 if half == 0 else 1024
        ci = self.blocks_of(ci0)
        post = self.blocks_of(po0)
        pl = l * P_LAYER
        win = self.w_in[l]
        self.st_norm(ci, xoff, S.H, "H", pl + P_NMG, mask=(half == 0))

        for grp in range(2):
            k, sl = self.wget(self.ld_cols(win, C_PU + grp * 512))
            for j in range(4):
                c = grp * 4 + j
                for (c0, n) in ci:
                    b = self.bank()
                    self.proj(sl, k, j, S.H, "H", c0, n, b)

                    def f(eng, c=c, c0=c0, n=n, b=b):
                        return eng.activation(out=S.T2[:, c, c0:c0 + n], in_=S.PS[b][:, 0:n], func=AF.Copy)
                    self.add("act", f, reads=[("ps", b)], writes=U("T2", c, c0, c0 + n))
            self.wrel(k)
        kp, psl = self.wget(self.ld_poolw(self.pool_w[l]))
        pwv = psl[:, 0:2048].rearrange("p (k c) -> p k c", k=8)
        for g in range(4):
            w = WINS[g]
            for cc in range(2):
                c = 2 * g + cc
                for (c0, n) in post:
                    b = self.bank()
                    pairs = [(S.IDB[:, :], S.T2[:, c, c0 - jj:c0 - jj + n]) for jj in range(w)]
                    self.mm_group(b, n, pairs, [("idb",)] + U("T2", c, c0 - w + 1, c0 + n))

                    def f(eng, c=c, c0=c0, n=n, b=b, w=w):
                        return eng.scalar_tensor_tensor(out=S.T1[:, c, c0:c0 + n], in0=S.PS[b][:, 0:n],
                                                        scalar=1.0 / w, in1=S.T2[:, c, c0:c0 + n],
                                                        op0=ALU.mult, op1=ALU.subtract)
                    self.add("dve", f, reads=[("ps", b)] + U("T2", c, c0, c0 + n), writes=U("T1", c, c0, c0 + n))
                    if half == 0 and c0 == HALO:
                        t = self.tf()

                        def f(eng, t=t, b=b, g=g):
                            return eng.tensor_tensor(out=S.TF[t][:, 0:16], in0=S.PS[b][:, 0:16],
                                                     in1=S.PRM[:, P_INVC + g * 16:P_INVC + (g + 1) * 16], op=ALU.mult)
                        self.add("dve", f, reads=[("ps", b), ("prm",)], writes=[("tf", t)])

                        def f(eng, t=t, c=c):
                            return eng.tensor_tensor(out=S.T1[:, c, HALO:HALO + 16], in0=S.TF[t][:, 0:16],
                                                     in1=S.T2[:, c, HALO:HALO + 16], op=ALU.subtract)
                        self.add("dve", f, reads=[("tf", t)] + U("T2", c, HALO, HALO + 16),
                                 writes=U("T1", c, HALO, HALO + 16))
            for (c0, n) in post:
                bs = []
                for e2 in range(2):
                    b = self.bank()
                    bs.append(b)
                    pairs = [(pwv[:, g * 2 + kc, e2 * 128:(e2 + 1) * 128], S.T1[:, 2 * g + kc, c0:c0 + n])
                             for kc in range(2)]
                    rd = [("w", self.slot_plan[kp])] + U("T1", 2 * g, c0, c0 + n) + U("T1", 2 * g + 1, c0, c0 + n)
                    self.mm_group(b, n, pairs, rd)
                for e2 in range(2):
                    ch = 2 * g + e2

                    def f(eng, ch=ch, c0=c0, n=n, b=bs[e2]):
                        return eng.activation(out=S.T1[:, ch, c0:c0 + n], in_=S.PS[b][:, 0:n], func=AF.Copy,
                                              scale=S.PRM[:, pl + P_PSC + ch:pl + P_PSC + ch + 1])
                    self.add("act", f, reads=[("ps", bs[e2]), ("prm",)], writes=U("T1", ch, c0, c0 + n))
        self.wrel(kp)
        self.st_outproj(l, self.w_pool_out, C_GC, post, xoff, True, False)

        for grp in range(2):
            kc_, csl = self.wget(self.ld_cols(win, C_SCC + grp * 512))
            kx_, xsl = self.wget(self.ld_cols(win, C_SCX + grp * 512))
            for j in range(4):
                c = grp * 4 + j
                for (c0, n) in ci:
                    b1 = self.bank()
                    self.proj(csl, kc_, j, S.H, "H", c0, n, b1)
                    b2 = self.bank()
                    self.proj(xsl, kx_, j, S.H, "H", c0, n, b2)
                    t = self.tf()

                    def f(eng, t=t, n=n, b1=b1):
                        return eng.activation(out=S.TF[t][:, 0:n], in_=S.PS[b1][:, 0:n], func=AF.Copy)
                    self.add("act", f, reads=[("ps", b1)], writes=[("tf", t)])

                    def f(eng, t=t, n=n, b2=b2, c=c, c0=c0):
                        return eng.tensor_tensor(out=S.T2[:, c, c0:c0 + n], in0=S.PS[b2][:, 0:n],
                                                 in1=S.TF[t][:, 0:n], op=ALU.mult)
                    self.add("dve", f, reads=[("ps", b2), ("tf", t)], writes=U("T2", c, c0, c0 + n))
            self.wrel(kc_)
            self.wrel(kx_)
        for grp in range(2):
            kb_, bsl = self.wget(self.ld_cols(win, C_SCB + grp * 512))
            for j in range(4):
                c = grp * 4 + j
                db = self.build_diag(pl + P_SCW + c * 3, 3)
                for (c0, n) in post:
                    b1 = self.bank()
                    pairs = [(S.DIAG[db][:, kk, :], S.T2[:, c, c0 - 2 + kk:c0 - 2 + kk + n]) for kk in range(3)]
                    self.mm_group(b1, n, pairs, [("diag", db, 0)] + U("T2", c, c0 - 2, c0 + n))
                    b2 = self.bank()
                    self.proj(bsl, kb_, j, S.H, "H", c0, n, b2)
                    t = self.tf()

                    def f(eng, t=t, n=n, b2=b2):
                        return eng.activation(out=S.TF[t][:, 0:n], in_=S.PS[b2][:, 0:n], func=AF.Copy)
                    self.add("act", f, reads=[("ps", b2)], writes=[("tf", t)])

                    def f(eng, t=t, n=n, b1=b1, c=c, c0=c0):
                        return eng.tensor_tensor(out=S.T1[:, c, c0:c0 + n], in0=S.PS[b1][:, 0:n],
                                                 in1=S.TF[t][:, 0:n], op=ALU.mult)
                    self.add("dve", f, reads=[("ps", b1), ("tf", t)], writes=U("T1", c, c0, c0 + n))
            self.wrel(kb_)
        self.st_outproj(l, self.w_sc_out, C_GA, post, xoff, False, False)

        for grp in range(2):
            kg_, gsl = self.wget(self.ld_cols(win, C_CFG + grp * 512))
            kv_, vsl = self.wget(self.ld_cols(win, C_CFV + grp * 512))
            for j in range(4):
                c = grp * 4 + j
                for (c0, n) in ci:
                    b1 = self.bank()
                    self.proj(gsl, kg_, j, S.H, "H", c0, n, b1)
                    b2 = self.bank()
                    self.proj(vsl, kv_, j, S.H, "H", c0, n, b2)
                    t = self.tf()

                    def f(eng, t=t, n=n, b1=b1):
                        return eng.activation(out=S.TF[t][:, 0:n], in_=S.PS[b1][:, 0:n], func=AF.Sigmoid)
                    self.add("act", f, reads=[("ps", b1)], writes=[("tf", t)])

                    def f(eng, t=t, n=n, b2=b2, c=c, c0=c0):
                        return eng.tensor_tensor(out=S.T2[:, c, c0:c0 + n], in0=S.PS[b2][:, 0:n],
                                                 in1=S.TF[t][:, 0:n], op=ALU.mult)
                    self.add("dve", f, reads=[("ps", b2), ("tf", t)], writes=U("T2", c, c0, c0 + n))
            self.wrel(kg_)
            self.wrel(kv_)
        for c in range(NCH):
            db = self.build_diag(pl + P_CFW + c * 31, 31)
            for (c0, n) in post:
                b = self.bank()
                pairs = [(S.DIAG[db][:, kk, :], S.T2[:, c, c0 - 30 + kk:c0 - 30 + kk + n]) for kk in range(31)]
                self.mm_group(b, n, pairs, [("diag", db, 0)] + U("T2", c, c0 - 30, c0 + n))

                def f(eng, c=c, c0=c0, n=n, b=b):
                    return eng.activation(out=S.T1[:, c, c0:c0 + n], in_=S.PS[b][:, 0:n], func=AF.Identity,
                                          bias=S.PRM[:, pl + P_CFB + c:pl + P_CFB + c + 1])
                self.add("act", f, reads=[("ps", b), ("prm",)], writes=U("T1", c, c0, c0 + n))
        for (c0, n) in post:
            b1 = self.bank()
            b2 = self.bank()
            for c in range(NCH):
                t = self.tb()

                def f(eng, c=c, t=t, c0=c0, n=n):
                    return eng.activation(out=S.TB[t][:, 0:n], in_=S.T1[:, c, c0:c0 + n], func=AF.Square)
                self.add("act", f, reads=U("T1", c, c0, c0 + n), writes=[("tb", t)])

                def f(eng, c=c, t=t, c0=c0, n=n, b1=b1, b2=b2):
                    eng.matmul(S.PS[b1][:, 0:n], lhsT=S.ONESB[:, :], rhs=S.T1[:, c, c0:c0 + n],
                               start=(c == 0), stop=(c == NCH - 1))
                    return eng.matmul(S.PS[b2][:, 0:n], lhsT=S.ONESB[:, :], rhs=S.TB[t][:, 0:n],
                                      start=(c == 0), stop=(c == NCH - 1))
                self.add("pe", f, reads=[("tb", t), ("ones",)] + U("T1", c, c0, c0 + n),
                         writes=[("ps", b1), ("ps", b2)])

            def f(eng, n=n, b1=b1):
                return eng.activation(out=S.MEAN[:, 0:n], in_=S.PS[b1][:, 0:n], func=AF.Copy, scale=1.0 / D)
            self.add("act", f, reads=[("ps", b1)], writes=[("mean",)])
            ta = self.tf()

            def f(eng, n=n, ta=ta):
                return eng.tensor_tensor(out=S.TF[ta][:, 0:n], in0=S.MEAN[:, 0:n], in1=S.MEAN[:, 0:n], op=ALU.mult)
            self.add("dve", f, reads=[("mean",)], writes=[("tf", ta)])
            tv = self.tf()

            def f(eng, n=n, ta=ta, tv=tv, b2=b2):
                return eng.scalar_tensor_tensor(out=S.TF[tv][:, 0:n], in0=S.PS[b2][:, 0:n], scalar=1.0 / D,
                                                in1=S.TF[ta][:, 0:n], op0=ALU.mult, op1=ALU.subtract)
            self.add("dve", f, reads=[("ps", b2), ("tf", ta)], writes=[("tf", tv)])
            tr = self.tf()

            def f(eng, n=n, tv=tv, tr=tr):
                return eng.activation(out=S.TF[tr][:, 0:n], in_=S.TF[tv][:, 0:n], func=AF.Sqrt,
                                      bias=S.EPSL[:, 0:1])
            self.add("act", f, reads=[("tf", tv), ("eps",)], writes=[("tf", tr)])

            def f(eng, n=n, tr=tr):
                return eng.reciprocal(out=S.RSTD[:, 0:n], in_=S.TF[tr][:, 0:n])
            self.add("dve", f, reads=[("tf", tr)], writes=[("rstd",)])
            for c in range(NCH):
                t1 = self.tf()

                def f(eng, c=c, c0=c0, n=n, t1=t1):
                    return eng.tensor_tensor(out=S.TF[t1][:, 0:n], in0=S.T1[:, c, c0:c0 + n], in1=S.MEAN[:, 0:n],
                                             op=ALU.subtract)
                self.add("dve", f, reads=U("T1", c, c0, c0 + n) + [("mean",)], writes=[("tf", t1)])
                t2 = self.tf()

                def f(eng, n=n, t1=t1, t2=t2):
                    return eng.tensor_tensor(out=S.TF[t2][:, 0:n], in0=S.TF[t1][:, 0:n], in1=S.RSTD[:, 0:n],
                                             op=ALU.mult)
                self.add("dve", f, reads=[("tf", t1), ("rstd",)], writes=[("tf", t2)])

                def f(eng, c=c, c0=c0, n=n, t2=t2):
                    return eng.activation(out=S.T1[:, c, c0:c0 + n], in_=S.TF[t2][:, 0:n], func=AF.Silu,
                                          scale=S.PRM[:, pl + P_LNG + c:pl + P_LNG + c + 1],
                                          bias=S.PRM[:, pl + P_LNB + c:pl + P_LNB + c + 1])
                self.add("act", f, reads=[("tf", t2), ("prm",)], writes=U("T1", c, c0, c0 + n))
        self.st_outproj(l, self.w_cf_out, C_GB, post, xoff, False, True)

    def st_ffn(self, l, blocks, experts):
        S = self
        for (w1, w3, w2) in experts:
            for fg in range(NFG):
                k1, s1 = self.wget(self.ld_cols(w1, fg * 512))
                k3, s3 = self.wget(self.ld_cols(w3, fg * 512))
                k2, s2 = self.wget(self.ld_rows(w2, fg * 512))
                for (c0, n) in blocks:
                    for j in range(4):
                        ba = self.bank()
                        self.proj(s1, k1, j, S.H2, "H2", c0, n, ba)
                        bc = self.bank()
                        self.proj(s3, k3, j, S.H2, "H2", c0, n, bc)
                        t = self.tf()

                        def f(eng, t=t, n=n, ba=ba):
                            return eng.activation(out=S.TF[t][:, 0:n], in_=S.PS[ba][:, 0:n], func=AF.Silu)
                        self.add("act", f, reads=[("ps", ba)], writes=[("tf", t)])

                        def f(eng, t=t, n=n, bc=bc, j=j, c0=c0):
                            return eng.tensor_tensor(out=S.GS[:, j, c0:c0 + n], in0=S.PS[bc][:, 0:n],
                                                     in1=S.TF[t][:, 0:n], op=ALU.mult)
                        self.add("dve", f, reads=[("ps", bc), ("tf", t)], writes=U("GS", j, c0, c0 + n))
                self.wrel(k1)
                self.wrel(k3)
                w2v = s2[:, :].rearrange("p (f d) -> p f d", f=4)
                for (c0, n) in blocks:
                    for d in range(NCH):
                        b = self.bank()
                        pairs = [(w2v[:, j, d * 128:(d + 1) * 128], S.GS[:, j, c0:c0 + n]) for j in range(4)]
                        rd = [("w", self.slot_plan[k2])]
                        for j in range(4):
                            rd += U("GS", j, c0, c0 + n)
                        self.mm_group(b, n, pairs, rd)

                        def f(eng, d=d, c0=c0, n=n, b=b):
                            return eng.tensor_tensor(out=S.X[:, d, c0:c0 + n], in0=S.X[:, d, c0:c0 + n],
                                                     in1=S.PS[b][:, 0:n], op=ALU.add)
                        self.add("dve", f, reads=[("ps", b)] + U("X", d, c0, c0 + n), writes=U("X", d, c0, c0 + n))
                self.wrel(k2)

    def st_route(self):
        S = self
        allg = [("lg", j0) for j0 in range(0, 16, 4)]

        def bc(ap):
            return ap.unsqueeze(2).to_broadcast([128, NTT, NE])

        def f(eng):
            return eng.tensor_reduce(out=S.M1[:, :], in_=S.LG[:, :, :], axis=AX.X, op=ALU.max)
        self.add("dve", f, reads=allg, writes=[("m1",)])

        def f(eng):
            return eng.tensor_tensor(out=S.EQ1[:, :, :], in0=S.LG[:, :, :], in1=bc(S.M1[:, :]), op=ALU.is_equal)
        self.add("dve", f, reads=allg + [("m1",)], writes=[("eq1",)])

        def f(eng):
            return eng.scalar_tensor_tensor(out=S.L2[:, :, :], in0=S.EQ1[:, :, :], scalar=-1e30, in1=S.LG[:, :, :],
                                            op0=ALU.mult, op1=ALU.add)
        self.add("dve", f, reads=allg + [("eq1",)], writes=[("l2",)])

        def f(eng):
            return eng.tensor_reduce(out=S.M2[:, :], in_=S.L2[:, :, :], axis=AX.X, op=ALU.max)
        self.add("dve", f, reads=[("l2",)], writes=[("m2",)])

        def f(eng):
            return eng.tensor_tensor(out=S.EQ2[:, :, :], in0=S.L2[:, :, :], in1=bc(S.M2[:, :]), op=ALU.is_equal)
        self.add("dve", f, reads=[("l2",), ("m2",)], writes=[("eq2",)])

        def f(eng):
            return eng.tensor_tensor(out=S.M2[:, :], in0=S.M1[:, :], in1=S.M2[:, :], op=ALU.subtract)
        self.add("dve", f, reads=[("m1",), ("m2",), ("eq2",)], writes=[("dd",)])

        def f(eng):
            eng.activation(out=S.P1[:, :], in_=S.M2[:, :], func=AF.Sigmoid)
            return eng.activation(out=S.P2[:, :], in_=S.M2[:, :], func=AF.Sigmoid, scale=-1.0)
        self.add("act", f, reads=[("dd",)], writes=[("p12",)])

        def f(eng):
            return eng.tensor_tensor(out=S.MSKB[:, :, :], in0=S.EQ1[:, :, :], in1=S.EQ2[:, :, :], op=ALU.add)
        self.add("dve", f, reads=[("eq1",), ("eq2",), ("hdz",)], writes=[("mskb",)])
        ba = self.bank()
        bc_ = self.bank()
        mflat = S.MSKB[:, :, :].rearrange("p t e -> p (t e)")

        def f(eng, ba=ba, bc_=bc_):
            eng.matmul(S.PS[ba][:, 0:NTT * NE], lhsT=S.UTB[:, :], rhs=mflat, start=True, stop=True)
            return eng.matmul(S.PS[bc_][:, 0:NTT * NE], lhsT=S.ONESB[:, :], rhs=mflat, start=True, stop=True)
        self.add("pe", f, reads=[("mskb",), ("utb",), ("ones",)], writes=[("ps", ba), ("ps", bc_)])

        def v3(ap):
            return ap.rearrange("p (t e) -> p t e", e=NE)

        def f(eng, bc_=bc_):
            return eng.tensor_copy(out=S.CNT[:, :, :], in_=v3(S.PS[bc_][:, 0:NTT * NE]))
        self.add("dve", f, reads=[("ps", bc_)], writes=[("cntb",)])
        bufs = [(S.CNT, "cntb"), (S.OFF, "offb"), (S.TMP, "tmpb"), (S.OFF, "offb"), (S.TMP, "tmpb")]
        for k, sh in enumerate((1, 2, 4, 8)):
            (src, sn), (dst, dn) = bufs[k], bufs[k + 1]

            def f(eng, src=src, dst=dst, sh=sh):
                eng.tensor_copy(out=dst[:, 0:sh, :], in_=src[:, 0:sh, :])
                return eng.tensor_tensor(out=dst[:, sh:NTT, :], in0=src[:, sh:NTT, :], in1=src[:, 0:NTT - sh, :], op=ALU.add)
            self.add("dve", f, reads=[(sn,)], writes=[(dn,)])

        def f(eng):
            return eng.tensor_copy(out=S.CNTI[:, :], in_=S.TMP[:, NTT - 1, :])
        self.add("dve", f, reads=[("tmpb",)], writes=[("cnti0",)])

        def f(eng):
            return eng.tensor_reduce(out=S.CMAXF[:, :], in_=S.TMP[:, NTT - 1, :], axis=AX.X, op=ALU.max)
        self.add("dve", f, reads=[("tmpb",)], writes=[("cmaxf",)])

        def f(eng):
            return eng.tensor_copy(out=S.CMAXI[:, :], in_=S.CMAXF[:, :])
        self.cnti_op = self.add("dve", f, reads=[("cmaxf",), ("cnti0",)], writes=[("cnti",)])

        def f(eng):
            return eng.tensor_tensor(out=S.OFF[:, :, :], in0=S.TMP[:, :, :], in1=S.CNT[:, :, :], op=ALU.subtract)
        self.add("dve", f, reads=[("tmpb",), ("cntb",)], writes=[("offb",)])

        def f(eng):
            return eng.tensor_tensor(out=S.CNT[:, :, :], in0=S.OFF[:, :, :],
                                     in1=S.PRM[:, P_ECAP:P_ECAP + NE].unsqueeze(1).to_broadcast([128, NTT, NE]),
                                     op=ALU.add)
        self.add("dve", f, reads=[("offb",), ("prm",)], writes=[("cntb",)])

        def f(eng, ba=ba):
            return eng.tensor_tensor(out=S.POS[:, :, :], in0=v3(S.PS[ba][:, 0:NTT * NE]), in1=S.CNT[:, :, :], op=ALU.add)
        self.add("dve", f, reads=[("ps", ba), ("cntb",)], writes=[("pos",)])

        def f(eng):
            return eng.tensor_tensor(out=S.TMP[:, :, :], in0=S.EQ1[:, :, :], in1=S.POS[:, :, :], op=ALU.mult)
        self.add("dve", f, reads=[("pos",), ("eq1",), ("cnti",)], writes=[("tmpb",)])

        def f(eng):
            return eng.tensor_reduce(out=S.D1[:, :], in_=S.TMP[:, :, :], axis=AX.X, op=ALU.add)
        self.add("dve", f, reads=[("tmpb",)], writes=[("d1",)])

        def f(eng):
            return eng.tensor_tensor(out=S.OFF[:, :, :], in0=S.EQ2[:, :, :], in1=S.POS[:, :, :], op=ALU.mult)
        self.add("dve", f, reads=[("pos",), ("eq2",)], writes=[("offb",)])

        def f(eng):
            return eng.tensor_reduce(out=S.D2[:, :], in_=S.OFF[:, :, :], axis=AX.X, op=ALU.add)
        self.add("dve", f, reads=[("offb",)], writes=[("d2",)])

        def f(eng):
            return eng.tensor_copy(out=S.DI1[:, :], in_=S.D1[:, :])
        self.add("dve", f, reads=[("d1",)], writes=[("di1",)])

        def f(eng):
            return eng.tensor_copy(out=S.DI2[:, :], in_=S.D2[:, :])
        self.add("dve", f, reads=[("d2",)], writes=[("di",)])
        if self.debug:
            def f(eng, sem):
                eng.dma_start(out=S.DBG[:, 0:16], in_=S.DI1[:, :]).then_inc(sem, 16)
                eng.dma_start(out=S.DBG[:, 16:32], in_=S.DI2[:, :]).then_inc(sem, 16)
                return eng.dma_start(out=S.DBG[:, 32:40], in_=S.CNTI[:, :]).then_inc(sem, 16)
            self.add("sp", f, reads=[("di",), ("di1",), ("cnti",)], writes=[("dbg",)], dma=("dbg",), ndma=3)

    def st_scatter(self):
        S = self
        for tt in range(NTT):
            t0 = HALO + tt * 128
            bs = [self.bank(), self.bank()]

            def f(eng, t0=t0, bs=bs):
                last = None
                for kc in range(NCH):
                    last = eng.matmul(S.PS[bs[kc // 4]][:, (kc % 4) * 128:(kc % 4 + 1) * 128],
                                      lhsT=S.H2[:, kc, t0:t0 + 128], rhs=S.IDB[:, :], start=True, stop=True)
                return last
            rd = [("idb",)]
            for kc in range(NCH):
                rd += U("H2", kc, t0, t0 + 128)
            self.add("pe", f, reads=rd, writes=[("ps", bs[0]), ("ps", bs[1])])
            i = self.ht()

            def f(eng, i=i, b=bs[0]):
                return eng.activation(out=S.HT[i][:, 0:512], in_=S.PS[b][:, 0:512], func=AF.Copy)
            self.add("act", f, reads=[("ps", bs[0])], writes=[("ht", i, 0)])

            def f(eng, i=i, b=bs[1]):
                return eng.tensor_copy(out=S.HT[i][:, 512:1024], in_=S.PS[b][:, 0:512])
            self.add("dve", f, reads=[("ps", bs[1])], writes=[("ht", i, 1)])

            def f(eng, sem, i=i, tt=tt):
                eng.indirect_dma_start(out=S.HD[:, :], out_offset=bass.IndirectOffsetOnAxis(ap=S.DI1[:, tt:tt + 1], axis=0),
                                       in_=S.HT[i], in_offset=None, bounds_check=S.rb_pool,
                                       oob_is_err=False).then_inc(sem, 16)
                return eng.indirect_dma_start(out=S.HD[:, :],
                                              out_offset=bass.IndirectOffsetOnAxis(ap=S.DI2[:, tt:tt + 1], axis=0),
                                              in_=S.HT[i], in_offset=None, bounds_check=S.rb_pool,
                                              oob_is_err=False).then_inc(sem, 16)
            self.add("pool", f, reads=[("ht", i, 0), ("ht", i, 1), ("di",), ("di1",), ("hdz",)], writes=[("hd", tt)],
                     dma=("scat", tt % 4), ndma=2)

    def pass_load(self, e, r):
        S = self
        row0 = e * CAP + r * BLK

        nfull = BLK // 128
        rem = BLK - nfull * 128

        def f(eng, sem, row0=row0):
            last = eng.dma_start(out=S.HGS[:, 0:nfull, :],
                                 in_=S.HD[row0:row0 + nfull * 128, :].rearrange("(s p) d -> p s d", p=128)).then_inc(sem, 16)
            if rem:
                last = eng.dma_start(out=S.HGS[0:rem, nfull, :],
                                     in_=S.HD[row0 + nfull * 128:row0 + BLK, :]).then_inc(sem, 16)
            return last
        self.add("sp", f, reads=[("hdz",)] + [("hd", tt) for tt in range(NTT)], writes=[("hgs",)], dma=("hgl",),
                 ndma=2 if rem else 1)

    def st_pass(self, e, r, yi, prefetch=None):
        S = self
        row0 = e * CAP + r * BLK
        if prefetch is None or not prefetch[0]:
            self.pass_load(e, r)
        alt = 0
        for kc in range(NCH):
            for (c0, n) in CB:
                b = self.bank()

                def f(eng, kc=kc, c0=c0, n=n, b=b):
                    last = None
                    for (sl, rows) in STL:
                        if not (c0 <= sl * 128 < c0 + n):
                            continue
                        o = sl * 128 - c0
                        last = eng.matmul(S.PS[b][:, o:o + rows], lhsT=S.HGS[0:rows, sl, kc * 128:(kc + 1) * 128],
                                          rhs=S.IDB[0:rows, 0:rows], start=True, stop=True)
                    return last
                self.add("pe", f, reads=[("hgs",), ("idb",)], writes=[("ps", b)])
                if alt % 2 == 0:
                    def f(eng, kc=kc, c0=c0, n=n, b=b):
                        return eng.activation(out=S.HG[:, kc, c0:c0 + n], in_=S.PS[b][:, 0:n], func=AF.Copy)
                    self.add("act", f, reads=[("ps", b)], writes=U("HG", kc, c0, c0 + n))
                else:
                    def f(eng, kc=kc, c0=c0, n=n, b=b):
                        return eng.tensor_copy(out=S.HG[:, kc, c0:c0 + n], in_=S.PS[b][:, 0:n])
                    self.add("dve", f, reads=[("ps", b)], writes=U("HG", kc, c0, c0 + n))
                alt += 1
        if prefetch is not None and prefetch[1] is not None:
            self.pass_load(prefetch[1], 0)
        w1, w3, w2 = self.moe_w1[0, e], self.moe_w3[0, e], self.moe_w2[0, e]
        Y = S.YACC[yi]
        for fg in range(NFG):
            k1, s1 = self.wget(self.ld_cols(w1, fg * 512))
            k3, s3 = self.wget(self.ld_cols(w3, fg * 512))
            k2, s2 = self.wget(self.ld_rows(w2, fg * 512))
            for (c0, n) in CB:
                for j in range(4):
                    ba = self.bank()
                    self.proj(s1, k1, j, S.HG, "HG", c0, n, ba)
                    bc = self.bank()
                    self.proj(s3, k3, j, S.HG, "HG", c0, n, bc)
                    t = self.tf()

                    def f(eng, t=t, n=n, ba=ba):
                        return eng.activation(out=S.TF[t][:, 0:n], in_=S.PS[ba][:, 0:n], func=AF.Silu)
                    self.add("act", f, reads=[("ps", ba)], writes=[("tf", t)])

                    def f(eng, t=t, n=n, bc=bc, j=j, c0=c0):
                        return eng.tensor_tensor(out=S.GSR[:, j, c0:c0 + n], in0=S.PS[bc][:, 0:n],
                                                 in1=S.TF[t][:, 0:n], op=ALU.mult)
                    self.add("dve", f, reads=[("ps", bc), ("tf", t)], writes=U("GSR", j, c0, c0 + n))
            self.wrel(k1)
            self.wrel(k3)
            w2v = s2[:, :].rearrange("p (f d) -> p f d", f=4)
            for (sl, rows) in STL:
                for hf in range(2):
                    b = self.bank()
                    pairs = [(S.GSR[:, j, sl * 128:sl * 128 + rows], w2v[:, j, hf * 512:(hf + 1) * 512]) for j in range(4)]
                    rd = [("w", self.slot_plan[k2])]
                    for j in range(4):
                        rd += U("GSR", j, sl * 128, sl * 128 + rows)
                    self.mm_group(b, 512, pairs, rd)
                    yres = ("yacc", yi, sl, hf)
                    if fg == 0:
                        def f(eng, sl=sl, hf=hf, b=b, Y=Y, rows=rows):
                            return eng.activation(out=Y[0:rows, sl, hf * 512:(hf + 1) * 512], in_=S.PS[b][0:rows, 0:512],
                                                  func=AF.Copy)
                        self.add("act", f, reads=[("ps", b)], writes=[yres])
                    else:
                        def f(eng, sl=sl, hf=hf, b=b, Y=Y, rows=rows):
                            return eng.tensor_tensor(out=Y[0:rows, sl, hf * 512:(hf + 1) * 512],
                                                     in0=Y[0:rows, sl, hf * 512:(hf + 1) * 512], in1=S.PS[b][0:rows, 0:512],
                                                     op=ALU.add)
                        self.add("dve", f, reads=[("ps", b), yres], writes=[yres])
            self.wrel(k2)

        nfull = BLK // 128
        rem = BLK - nfull * 128

        def f(eng, sem, row0=row0, Y=Y):
            last = eng.dma_start(out=S.YD[row0:row0 + nfull * 128, :].rearrange("(s p) d -> p s d", p=128),
                                 in_=Y[:, 0:nfull, :]).then_inc(sem, 16)
            if rem:
                last = eng.dma_start(out=S.YD[row0 + nfull * 128:row0 + BLK, :], in_=Y[0:rem, nfull, :]).then_inc(sem, 16)
            return last
        self.add("sp", f, reads=[("yacc", yi, sl, hf) for sl in range(NSLT) for hf in range(2)], writes=[("yd",)],
                 dma=("yst",), ndma=2 if rem else 1)

    def st_combine(self):
        S = self
        for tt in range(NTT):
            t0 = HALO + tt * 128
            g1 = self.gt()
            g2 = self.gt()

            def f(eng, sem, g1=g1, g2=g2, tt=tt):
                eng.indirect_dma_start(out=S.GT[g1], out_offset=None, in_=S.YD[:, :],
                                       in_offset=bass.IndirectOffsetOnAxis(ap=S.DI1[:, tt:tt + 1], axis=0),
                                       bounds_check=S.rb_pool, oob_is_err=False).then_inc(sem, 16)
                return eng.indirect_dma_start(out=S.GT[g2], out_offset=None, in_=S.YD[:, :],
                                              in_offset=bass.IndirectOffsetOnAxis(ap=S.DI2[:, tt:tt + 1], axis=0),
                                              bounds_check=S.rb_pool, oob_is_err=False).then_inc(sem, 16)
            self.add("pool", f, reads=[("yd",), ("di",), ("di1",)], writes=[("gt", g1), ("gt", g2)], dma=("gath", tt % 4),
                     ndma=2)

            def f(eng, g1=g1, tt=tt):
                return eng.activation(out=S.GT[g1], in_=S.GT[g1], func=AF.Copy, scale=S.P1[:, tt:tt + 1])
            self.add("act", f, reads=[("gt", g1), ("p12",)], writes=[("gt", g1)])

            def f(eng, g1=g1, g2=g2, tt=tt):
                return eng.scalar_tensor_tensor(out=S.GT[g1], in0=S.GT[g2], scalar=S.P2[:, tt:tt + 1], in1=S.GT[g1],
                                                op0=ALU.mult, op1=ALU.add)
            self.add("dve", f, reads=[("gt", g1), ("gt", g2), ("p12",)], writes=[("gt", g1)])
            for hf in range(2):
                b = self.bank()

                def f(eng, g1=g1, hf=hf, b=b):
                    last = None
                    for q in range(4):
                        kc = hf * 4 + q
                        last = eng.transpose(out=S.PS[b][:, q * 128:(q + 1) * 128], in_=S.GT[g1][:, kc * 128:(kc + 1) * 128],
                                             identity=S.IDF[:, :])
                    return last
                self.add("pe", f, reads=[("gt", g1), ("idf",)], writes=[("ps", b)])

                def f(eng, hf=hf, b=b, t0=t0):
                    return eng.tensor_tensor(out=S.X[:, hf * 4:(hf + 1) * 4, t0:t0 + 128],
                                             in0=S.X[:, hf * 4:(hf + 1) * 4, t0:t0 + 128],
                                             in1=S.PS[b][:, 0:512].rearrange("p (q t) -> p q t", q=4), op=ALU.add)
                xr = []
                for kc in range(hf * 4, hf * 4 + 4):
                    xr += U("X", kc, t0, t0 + 128)
                self.add("dve", f, reads=[("ps", b)] + xr, writes=xr)
            if tt % 4 == 3:
                self.st_final(True, only=tt // 4, finish=(tt == NTT - 1))

    def st_final(self, norm, only=None, finish=True):
        S = self
        mains = [(HALO + i * 512, 512) for i in range(4)]
        ov = self.outT.rearrange("(c p) t -> p c t", p=128)
        for bi, (c0, n) in enumerate(mains):
            if only is not None and bi != only:
                continue
            if norm:
                bk = self.bank()
                for c in range(NCH):
                    t = self.tb()

                    def f(eng, c=c, t=t, c0=c0, n=n):
                        return eng.activation(out=S.TB[t][:, 0:n], in_=S.X[:, c, c0:c0 + n], func=AF.Square)
                    self.add("act", f, reads=U("X", c, c0, c0 + n), writes=[("tb", t)])

                    def f(eng, c=c, t=t, n=n, bk=bk):
                        return eng.matmul(S.PS[bk][:, 0:n], lhsT=S.ONESB[:, :], rhs=S.TB[t][:, 0:n],
                                          start=(c == 0), stop=(c == NCH - 1))
                    self.add("pe", f, reads=[("tb", t), ("ones",)], writes=[("ps", bk)])
                t1 = self.tf()

                def f(eng, t1=t1, n=n, bk=bk):
                    return eng.activation(out=S.TF[t1][:, 0:n], in_=S.PS[bk][:, 0:n], func=AF.Sqrt,
                                          scale=1.0 / D, bias=S.EPSR[:, 0:1])
                self.add("act", f, reads=[("ps", bk), ("eps",)], writes=[("tf", t1)])

                def f(eng, t1=t1, n=n):
                    return eng.reciprocal(out=S.RSTD[:, 0:n], in_=S.TF[t1][:, 0:n])
                self.add("dve", f, reads=[("tf", t1)], writes=[("rstd",)])
                for c in range(NCH):
                    def f(eng, c=c, c0=c0, n=n):
                        return eng.scalar_tensor_tensor(out=S.X[:, c, c0:c0 + n], in0=S.X[:, c, c0:c0 + n],
                                                        scalar=S.PRM[:, P_FIN + c:P_FIN + c + 1],
                                                        in1=S.RSTD[:, 0:n], op0=ALU.mult, op1=ALU.mult)
                    self.add("dve", f, reads=U("X", c, c0, c0 + n) + [("rstd",), ("prm",)],
                             writes=U("X", c, c0, c0 + n))
            rd = []
            for c in range(NCH):
                rd += U("X", c, c0, c0 + n)

            def f(eng, sem, c0=c0, n=n):
                return eng.dma_start(out=ov[:, :, c0 - HALO:c0 - HALO + n], in_=S.X[:, :, c0:c0 + n]).then_inc(sem, 16)
            self.add("sp", f, reads=rd, writes=[("out", bi)], dma=("out", bi))
        if not finish:
            return

        def f(eng):
            return None
        self.add("sp", f, reads=[("out", i) for i in range(4)], writes=[("done",)])

    def program(self):
        S = self
        mains = [(HALO + i * 512, 512) for i in range(4)]
        self.st_init()
        order = ["mix0", "l0", "mix1", "full"]
        lim = order.index(self.stop)
        self.ring_n = NS - 1
        self.mg_live = True
        self.st_mixer_half(0, 1, 32, 64)
        self.st_mixer_half(0, 0, 0, 32)
        self.mg_live = False
        if lim >= 1:
            if not self.dry:
                self.S.barrier()
            fb = [(32, 32)] + mains
            self.ring_n = NS
            self.st_norm(fb, 0, S.H2, "H2", P_NFG)
            self.st_ffn(0, fb, [(self.dense_w1[0], self.dense_w3[0], self.dense_w2[0])])
        if lim >= 2:
            if not self.dry:
                self.S.barrier()
            self.ring_n = NS - 1
            self.mg_live = True
            self.st_mixer_half(1, 1, 32, 64)
            self.st_mixer_half(1, 0, 32, 64)
            self.mg_live = False
        if lim >= 3:
            if not self.dry:
                self.S.barrier()
            self.ring_n = NS
            self.st_norm(mains, 0, S.H2, "H2", P_LAYER + P_NFG, router=True)
            self.st_route()
            self.st_scatter()
            if not self.dry:
                self.S.barrier()
            for e in range(NE):
                self.st_pass(e, 0, e % 2, prefetch=(e > 0, e + 1 if e + 1 < NE else None))
            yi = 0
            for e in range(NE):
                for r in range(1, NRND):
                    self.cur_region = (e, r)
                    self.st_pass(e, r, yi)
                    yi ^= 1
                    self.cur_region = None
            if not self.dry:
                self.S.barrier()
            self.st_combine()
        else:
            self.st_final(norm=False)

    def build(self):
        nc = self.nc
        self.declare()
        self.EPSR = self.es.enter_context(nc.sbuf_tensor("epsr", [128, 1], F32))
        self.EPSL = self.es.enter_context(nc.sbuf_tensor("epsl", [128, 1], F32))
        self.dry = True
        self.slot_plan = []
        self.rr = 0
        self.ring_n = NS
        self.reset_rot()
        self.program()
        last = {}
        self.prev_same = []
        for j, sl in enumerate(self.slot_plan):
            self.prev_same.append(last.get(sl))
            last[sl] = j
        self.dry = False
        self.reset_rot()
        S = self

        def f(eng):
            eng.memset(S.EPSR[:, :], RMS_EPS)
            return eng.memset(S.EPSL[:, :], LN_EPS)
        self.add("dve", f, writes=[("eps",)])
        self.program()
        assert self.wk == len(self.plan)
        self.emit()
        return nc

    def emit(self):
        nc = self.nc
        ops = self.S.ops
        es = self.es
        eng_sem = {}
        for e in Sched.COMPUTE:
            eng_sem[e] = es.enter_context(nc.semaphore(f"s_{e}"))
        dma_sem = {}
        for op in ops:
            if op.dma is not None and op.dma not in dma_sem:
                dma_sem[op.dma] = es.enter_context(nc.semaphore("d_" + "_".join(str(x) for x in op.dma)))
        if getattr(self, "cnti_op", None) is not None:
            ops[self.cnti_op].signals = True
        cnt = {e: 0 for e in Sched.COMPUTE}
        for op in ops:
            if op.dma is not None:
                op.sem = dma_sem[op.dma]
            elif op.eng in cnt:
                if op.signals:
                    cnt[op.eng] += 1
                    op.val = cnt[op.eng]
                op.sem = eng_sem[op.eng]
            else:
                assert not op.signals, "queue-engine non-dma op cannot signal"
        by = {e: [] for e in ("pe", "act", "dve", "pool", "sp")}
        for op in ops:
            by[op.eng].append(op)
        block = es.enter_context(nc.Block())

        cnti_op = getattr(self, "cnti_op", None)
        S = self

        def emit_op(eng, op, known):
            for d in op.deps:
                Dp = ops[d]
                key = id(Dp.sem)
                if known.get(key, 0) >= Dp.val:
                    continue
                eng.wait_ge(Dp.sem, Dp.val)
                known[key] = Dp.val
            if op.dma is not None:
                op.fn(eng, op.sem)
            else:
                inst = op.fn(eng)
                if op.signals:
                    inst.then_inc(op.sem, 1)

        def emit_comp(eng, ename, grp):
            nsig = sum(1 for op in grp if op.dma is None and op.signals)
            if nsig:
                eng.drain().then_inc(eng_sem[ename], nsig)
            dk = {}
            for op in grp:
                if op.dma is not None:
                    dk.setdefault(op.dma, []).append(op)
            for kd, lst2 in dk.items():
                before = lst2[0].val - 16 * lst2[0].ndma
                if before > 0:
                    eng.wait_ge(dma_sem[kd], before)
                eng.sem_inc(dma_sem[kd], 16 * sum(o.ndma for o in lst2))

        def emit_region(eng, ename, grp, known, rc):
            e, r = grp[0].region
            eng.reg_load(rc, S.CNTI[0:1, e:e + 1])
            with eng.If_lt(rc, r * BLK + 1):
                emit_comp(eng, ename, grp)
            with eng.Else():
                k2 = dict(known)
                for op in grp:
                    emit_op(eng, op, k2)

        def emit_run(eng, ename, run_ops, known, rc):
            Dp = ops[cnti_op]
            key = id(Dp.sem)
            if known.get(key, 0) < Dp.val:
                eng.wait_ge(Dp.sem, Dp.val)
                known[key] = Dp.val
            eng.reg_load(rc, S.CMAXI[0:1, 0:1])
            with eng.If_lt(rc, BLK + 1):
                emit_comp(eng, ename, run_ops)
            with eng.Else():
                i = 0
                while i < len(run_ops):
                    j = i
                    while j < len(run_ops) and run_ops[j].region == run_ops[i].region:
                        j += 1
                    emit_region(eng, ename, run_ops[i:j], known, rc)
                    i = j

        def run(eng, lst, ename):
            known = {}
            with eng.register("rc_" + ename) as rc:
                i = 0
                while i < len(lst):
                    op = lst[i]
                    if op.region is None:
                        emit_op(eng, op, known)
                        i += 1
                        continue
                    j = i
                    while j < len(lst) and lst[j].region is not None:
                        j += 1
                    emit_run(eng, ename, lst[i:j], known, rc)
                    i = j

        @block.tensor
        def _(eng):
            run(eng, by["pe"], "pe")

        @block.scalar
        def _(eng):
            run(eng, by["act"], "act")

        @block.vector
        def _(eng):
            run(eng, by["dve"], "dve")

        @block.gpsimd
        def _(eng):
            with eng.register("rb_rows") as rb:
                eng.reg_mov(rb, NE * CAP - 1)
                S.rb_pool = rb
                run(eng, by["pool"], "pool")

        @block.sync
        def _(eng):
            run(eng, by["sp"], "sp")
        es.close()


def pack_params(inp, core):
    P = np.zeros((128, NPRM), np.float32)

    def vec(v):
        return np.ascontiguousarray(v.reshape(NCH, 128).T)
    for l in range(2):
        b = l * P_LAYER
        P[:, b + P_NMG:b + P_NMG + 8] = vec(inp["norm_mix_g"][l])
        P[:, b + P_SCW:b + P_SCW + 24] = inp["sc_conv_w"][l].reshape(3, NCH, 128).transpose(2, 1, 0).reshape(128, 24)
        P[:, b + P_CFW:b + P_CFW + 248] = inp["cf_conv_w"][l].reshape(31, NCH, 128).transpose(2, 1, 0).reshape(128, 248)
        P[:, b + P_CFB:b + P_CFB + 8] = vec(inp["cf_conv_b"][l])
        P[:, b + P_LNG:b + P_LNG + 8] = vec(inp["cf_ln_g"][l])
        P[:, b + P_LNB:b + P_LNB + 8] = vec(inp["cf_ln_b"][l])
        P[:, b + P_PSC:b + P_PSC + 8] = vec(inp["pool_scale"][l])
        P[:, b + P_NFG:b + P_NFG + 8] = vec(inp["norm_ffn_g"][l])
    P[:, P_FIN:P_FIN + 8] = vec(inp["norm_final_g"])
    P[:, P_ROUT:P_ROUT + 64] = inp["moe_router"][0].reshape(NCH, 128, NE).transpose(1, 0, 2).reshape(128, 64)
    first = (core % 4 == 0)
    P[:, P_MASK:P_MASK + 64] = 0.0 if first else 1.0
    for g, w in enumerate(WINS):
        for i in range(16):
            cntv = min(i + 1, w) if first else w
            P[:, P_INVC + g * 16 + i] = 1.0 / cntv
    P[:, P_ECAP:P_ECAP + NE] = (np.arange(NE, dtype=np.float32) * CAP)[None, :]
    P[:, P_UT:P_UT + 128] = np.triu(np.ones((128, 128), np.float32), 1)
    return P


_CACHE = {}


def run(inputs, stop="full"):
    inp = {k: np.asarray(v, dtype=np.float32) for k, v in inputs.items()}
    if stop not in _CACHE:
        _CACHE[stop] = Builder(stop).build()
    nc = _CACHE[stop]
    x = inp["x"]
    shared = {k: np.ascontiguousarray(inp[k]) for k in
              ("w_in", "w_sc_out", "w_cf_out", "w_pool_out", "w_o", "pool_w", "dense_w1", "dense_w3", "dense_w2",
               "moe_w1", "moe_w3", "moe_w2")}
    in_maps = []
    for core in range(8):
        b, q = divmod(core, 4)
        t0 = q * TOK
        xs = np.zeros((TT, D), np.float32)
        if q > 0:
            xs[:HALO] = x[b, t0 - HALO:t0]
        xs[HALO:] = x[b, t0:t0 + TOK]
        m = dict(shared)
        m["xT"] = np.ascontiguousarray(xs.T)
        m["prm"] = pack_params(inp, core)
        in_maps.append(m)
    res = run_bass_kernel_spmd(nc, in_maps, core_ids=list(range(8)))
    out = np.zeros((2, SEQ, D), np.float32)
    for core in range(8):
        b, q = divmod(core, 4)
        out[b, q * TOK:(q + 1) * TOK] = res.results[core]["outT"].T
    return out


def kernel(**inputs):
    return run(inputs, "full")
```

```python
import numpy as np
import concourse.bass as bass
import concourse.mybir as mybir
from concourse.bass_utils import run_bass_kernel_spmd
from contextlib import ExitStack

F32 = mybir.dt.float32
BF16 = mybir.dt.bfloat16
I32 = mybir.dt.int32
AF = mybir.ActivationFunctionType
ALU = mybir.AluOpType
AX = mybir.AxisListType

D = 1024
NCH = 8
SEQ = 8192
TOK = 2048
HALO = 64
TT = TOK + HALO
TH = 1088
DFF = 3584
NFG = 7
NE = 8
NS = 5
NTF = 5
NTB = 2
NSLT = 5
BLK = 576
STL = [(sl, min(128, BLK - sl * 128)) for sl in range(NSLT)]
assert (NSLT - 1) * 128 < BLK <= NSLT * 128 and BLK % 32 == 0
NRND = (TOK + BLK - 1) // BLK
CAP = NRND * BLK
CB = [(c, min(512, BLK - c)) for c in range(0, BLK, 512)]
NTT = TOK // 128
WINS = (2, 4, 8, 16)
RMS_EPS = 1e-6
LN_EPS = 1e-5

C_SCB, C_SCC, C_SCX, C_CFV, C_CFG, C_PU, C_GA, C_GB, C_GC = [i * 1024 for i in range(9)]

P_LAYER = 320
P_NMG, P_SCW, P_CFW, P_CFB, P_LNG, P_LNB, P_PSC, P_NFG = 0, 8, 32, 280, 288, 296, 304, 312
P_FIN = 2 * P_LAYER
P_ROUT = P_FIN + 8
P_MASK = P_ROUT + 64
P_INVC = P_MASK + 64
P_ECAP = P_INVC + 64
P_UT = P_ECAP + 8
NPRM = P_UT + 128


class Op:
    __slots__ = ("idx", "eng", "fn", "deps", "signals", "val", "sem", "dma", "writes", "ndma", "region")

    def __init__(self, idx, eng, fn):
        self.idx = idx
        self.eng = eng
        self.fn = fn
        self.deps = []
        self.signals = False
        self.val = 0
        self.sem = None
        self.dma = None
        self.writes = ()
        self.ndma = 0
        self.region = None


class Sched:
    COMPUTE = ("pe", "act", "dve", "pool")

    def __init__(self):
        self.ops = []
        self.last_w = {}
        self.readers = {}
        self.dma_count = {}
        self.dma_last = {}
        self.bar = None
        self.bar_done = set()
        self.last_on = {}

    def barrier(self):
        self.bar = [self.last_on[e] for e in self.COMPUTE if e in self.last_on]
        self.bar_done = set()

    def add(self, eng, fn, reads=(), writes=(), dma=None, ndma=1, region=None):
        idx = len(self.ops)
        op = Op(idx, eng, fn)
        op.region = region
        deps = {}

        def need(d, raw):
            Dp = self.ops[d]
            if Dp.dma is not None:
                key = ("dma", Dp.dma)
            else:
                if Dp.eng == eng and eng == "pe":
                    return
                key = ("eng", Dp.eng)
            if key not in deps or deps[key] < d:
                deps[key] = d

        for r in reads:
            d = self.last_w.get(r)
            if d is not None:
                need(d, True)
        for w in writes:
            d = self.last_w.get(w)
            if d is not None:
                need(d, False)
            rd = self.readers.get(w)
            if rd:
                for d in rd.values():
                    need(d, False)
        if dma is not None:
            d = self.dma_last.get(dma)
            if d is not None:
                need(d, False)
        if self.bar is not None and eng in self.COMPUTE and eng not in self.bar_done:
            self.bar_done.add(eng)
            for d in self.bar:
                if self.ops[d].eng != eng:
                    need(d, False)
        for d in deps.values():
            self.ops[d].signals = True
        op.deps = sorted(deps.values())
        op.writes = frozenset(writes)
        if dma is not None:
            op.dma = dma
            op.ndma = ndma
            self.dma_count[dma] = self.dma_count.get(dma, 0) + ndma
            op.val = 16 * self.dma_count[dma]
            self.dma_last[dma] = idx
        for r in reads:
            self.readers.setdefault(r, {})[eng if dma is None else ("dma", dma)] = idx
        for w in writes:
            self.last_w[w] = idx
            self.readers[w] = {}
        self.ops.append(op)
        if dma is None:
            self.last_on[eng] = idx
        return idx


def U(name, ch, c0, c1):
    return [(name, ch, u) for u in range(c0 // 32, (c1 + 31) // 32)]


class Builder:
    def __init__(self, stop="full", debug=False):
        self.stop = stop
        self.debug = debug
        self.nc = bass.Bass("TRN2", target_bir_lowering=False)
        self.S = Sched()
        self.dry = False
        self.plan = []
        self.es = ExitStack()

    def declare(self):
        nc = self.nc

        def din(name, shape):
            return nc.dram_tensor(name, list(shape), F32, kind="ExternalInput").ap()

        self.xT = din("xT", [D, TT])
        self.prm_d = din("prm", [128, NPRM])
        self.w_in = din("w_in", [2, D, 9216])
        self.w_sc_out = din("w_sc_out", [2, D, D])
        self.w_cf_out = din("w_cf_out", [2, D, D])
        self.w_pool_out = din("w_pool_out", [2, D, D])
        self.w_o = din("w_o", [2, D, D])
        self.pool_w = din("pool_w", [2, 4, 256, 256])
        self.dense_w1 = din("dense_w1", [1, D, DFF])
        self.dense_w3 = din("dense_w3", [1, D, DFF])
        self.dense_w2 = din("dense_w2", [1, DFF, D])
        self.moe_w1 = din("moe_w1", [1, NE, D, DFF])
        self.moe_w3 = din("moe_w3", [1, NE, D, DFF])
        self.moe_w2 = din("moe_w2", [1, NE, DFF, D])
        self.outT = nc.dram_tensor("outT", [D, TOK], F32, kind="ExternalOutput").ap()

        def sb(name, shape, dt):
            return self.es.enter_context(nc.sbuf_tensor(name, list(shape), dt))

        self.X = sb("X", [128, NCH, TT], F32)
        self.WORK = sb("WORK", [128, 13056], F32)
        hw = 4352
        self.H = self.WORK[:, 0:hw].bitcast(BF16).rearrange("p (c t) -> p c t", c=NCH)
        self.T1 = self.WORK[:, hw:2 * hw].bitcast(BF16).rearrange("p (c t) -> p c t", c=NCH)
        self.T2 = self.WORK[:, 2 * hw:3 * hw].bitcast(BF16).rearrange("p (c t) -> p c t", c=NCH)
        self.H2 = self.WORK[:, 0:8448].bitcast(BF16).rearrange("p (c t) -> p c t", c=NCH)
        self.GS = self.WORK[:, 8448:8448 + 4224].bitcast(BF16).rearrange("p (c t) -> p c t", c=4)
        self.MGB = sb("mgb", [128, NCH * TH], BF16)
        self.MG = self.MGB[:, :].rearrange("p (c t) -> p c t", c=NCH)
        self.WS = [sb(f"ws{i}", [128, 4096], BF16)[:, :] for i in range(NS - 1)] + [self.MGB[:, 0:4096]]
        self.TF = [sb(f"tf{i}", [128, 512], F32) for i in range(NTF)]
        self.TB = [sb(f"tb{i}", [128, 512], BF16) for i in range(NTB)]
        self.MEAN = sb("mean", [128, 512], F32)
        self.RSTD = sb("rstd", [128, 512], F32)
        self.DG = sb("diag", [128, 3968], F32)
        dgb = self.DG[:, 0:3968].bitcast(BF16)
        self.DIAG = [dgb[:, 0:3968].rearrange("p (k m) -> p k m", k=31),
                     dgb[:, 3968:7936].rearrange("p (k m) -> p k m", k=31)]
        dga = self.DG[:, :].bitcast(BF16)
        self.HT = [dga[:, i * 1024:(i + 1) * 1024] for i in range(4)]
        self.HGS = dga[:, 0:NSLT * 1024].rearrange("p (s d) -> p s d", s=NSLT)
        self.GSR = dga[:, NSLT * 1024:NSLT * 1024 + 4 * BLK].rearrange("p (j t) -> p j t", j=4)
        hgw = NCH * BLK // 2
        self.HG = self.WORK[:, 0:hgw].bitcast(BF16).rearrange("p (c t) -> p c t", c=NCH)
        yw = NSLT * 1024
        self.YACC = [self.WORK[:, hgw + i * yw:hgw + (i + 1) * yw].rearrange("p (s d) -> p s d", s=NSLT)
                     for i in range(2)]
        assert hgw + 2 * yw <= 13056 and NSLT * 1024 + 4 * BLK <= 7936
        self.GT = [self.WORK[:, i * 1024:(i + 1) * 1024] for i in range(8)]
        if self.debug:
            self.HD = nc.dram_tensor("hd_scr", [NE * CAP, D], BF16, kind="ExternalOutput").ap()
            self.YD = nc.dram_tensor("yd_scr", [NE * CAP, D], F32, kind="ExternalOutput").ap()
            self.DBG = nc.dram_tensor("dbg_i", [128, 40], I32, kind="ExternalOutput").ap()
        else:
            self.HD = nc.dram_tensor("hd_scr", [NE * CAP, D], BF16).ap()
            self.YD = nc.dram_tensor("yd_scr", [NE * CAP, D], F32).ap()
        self.RT = sb("rt", [128, 4 * NTT * NE + NTT * NE // 2], F32)
        self.ZT = self.RT[:, 0:512].bitcast(BF16)
        self.UTB = sb("utb", [128, 128], BF16)
        nte = NTT * NE

        def r3(lo):
            return self.RT[:, lo:lo + nte].rearrange("p (t e) -> p t e", e=NE)
        self.CNT, self.OFF, self.POS, self.TMP = r3(0), r3(nte), r3(2 * nte), r3(3 * nte)
        self.MSKB = self.RT[:, 4 * nte:4 * nte + nte // 2].bitcast(BF16).rearrange("p (t e) -> p t e", e=NE)
        self.D1 = sb("d1", [128, NTT], F32)
        self.D2 = sb("d2", [128, NTT], F32)
        self.DI1 = sb("di1", [128, NTT], I32)
        self.DI2 = sb("di2", [128, NTT], I32)
        self.CNTI = sb("cnti", [128, NE], I32)
        self.CMAXF = sb("cmaxf", [128, 1], F32)
        self.CMAXI = sb("cmaxi", [128, 1], I32)
        self.IDF = sb("idf", [128, 128], F32)
        self.IDB = sb("idb", [128, 128], BF16)
        self.ONESB = sb("onesb", [128, 128], BF16)
        self.PRM = sb("prmsb", [128, NPRM], F32)
        self.LG = sb("lg", [128, 16, NE], F32)
        self.L2 = sb("l2", [128, 16, NE], F32)
        self.EQ1 = sb("eq1", [128, 16, NE], F32)
        self.EQ2 = sb("eq2", [128, 16, NE], F32)
        self.M1 = sb("m1", [128, 16], F32)
        self.M2 = sb("m2", [128, 16], F32)
        self.P1 = sb("p1", [128, 16], F32)
        self.P2 = sb("p2", [128, 16], F32)
        self.PS = [self.es.enter_context(nc.psum_tensor(f"ps{i}", [128, 512], F32)) for i in range(8)]
        self.sems = {}

    def reset_rot(self):
        self.ib = 0
        self.iht = 0
        self.igt = 0
        self.mg_live = False
        self.cur_region = None
        self.itf = 0
        self.itb = 0
        self.wk = 0
        self.w_issued = 0
        self.w_rel = []
        self.idg = 0

    def bank(self):
        i = self.ib % 8
        self.ib += 1
        return i

    def tf(self):
        i = self.itf % NTF
        self.itf += 1
        return i

    def tb(self):
        i = self.itb % NTB
        self.itb += 1
        return i

    def ht(self):
        i = self.iht % 4
        self.iht += 1
        return i

    def gt(self):
        i = self.igt % 8
        self.igt += 1
        return i

    def add(self, eng, fn, reads=(), writes=(), dma=None, ndma=1):
        if self.dry:
            return
        if self.cur_region is not None:
            reads = list(reads) + [("cnti",)]
        return self.S.add(eng, fn, reads, writes, dma, ndma, region=self.cur_region)

    def wget(self, issue_fn):
        k = self.wk
        self.wk += 1
        if self.dry:
            slot = self.rr % self.ring_n
            self.rr = slot + 1
            self.plan.append((issue_fn, self.cur_region))
            self.slot_plan.append(slot)
            return k, self.WS[slot]
        self.pump()
        assert self.w_issued > k, "weight ring deadlock: too many slots held"
        return k, self.WS[self.slot_plan[k]]

    def wrel(self, k):
        if self.dry:
            return
        self.w_rel.append(k)
        self.pump()

    def pump(self):
        while self.w_issued < len(self.plan):
            j = self.w_issued
            if self.prev_same[j] is not None and self.prev_same[j] not in self.w_rel:
                break
            slot = self.slot_plan[j]
            if slot == NS - 1 and self.mg_live:
                break
            fn, reg = self.plan[j]
            ws = self.WS[slot]

            def f(eng, sem, fn=fn, ws=ws):
                return fn(eng, ws, sem)
            self.S.add("pool", f, reads=([("cnti",)] if reg is not None else ()), writes=[("w", slot)],
                       dma=("w", slot), ndma=fn.ndma, region=reg)
            self.w_issued += 1

    def ld_cols(self, src2d, c0, ncols=512):
        def fn(eng, ws, sem):
            dst = ws[:, 0:8 * ncols].rearrange("p (k c) -> p k c", k=8)
            src = src2d[:, c0:c0 + ncols].rearrange("(k p) c -> p k c", p=128)
            return eng.dma_start(out=dst, in_=src).then_inc(sem, 16)
        fn.ndma = 1
        return fn

    def ld_rows(self, src2d, r0):
        def fn(eng, ws, sem):
            dst = ws[:, :].rearrange("p (f d) -> p f d", f=4)
            src = src2d[r0:r0 + 512, :].rearrange("(f p) d -> p f d", p=128)
            return eng.dma_start(out=dst, in_=src).then_inc(sem, 16)
        fn.ndma = 1
        return fn

    def ld_poolw(self, src3d):
        def fn(eng, ws, sem):
            dst = ws[:, 0:2048].rearrange("p (k c) -> p k c", k=8)
            src = src3d.rearrange("g (k p) e -> p (g k) e", p=128)
            return eng.dma_start(out=dst, in_=src).then_inc(sem, 16)
        fn.ndma = 1
        return fn

    def mm_group(self, bank, n, pairs, reads):
        ps = self.PS[bank]
        nc = self.nc

        def fn(eng, pairs=pairs, ps=ps, n=n):
            last = None
            m = pairs[0][0].shape[-1]
            for i, (l, r) in enumerate(pairs):
                last = eng.matmul(ps[0:m, 0:n], lhsT=l, rhs=r, start=(i == 0), stop=(i == len(pairs) - 1))
            return last
        self.add("pe", fn, reads=reads, writes=[("ps", bank)])

    def proj(self, wsl, k, j, rhs_buf, rname, c0, n, bank, extra=()):
        wv = wsl[:, 0:4096].rearrange("p (k c) -> p k c", k=8)
        pairs = [(wv[:, kc, j * 128:(j + 1) * 128], rhs_buf[:, kc, c0:c0 + n]) for kc in range(NCH)]
        reads = [("w", self.slot_plan[k])] + list(extra)
        for kc in range(NCH):
            reads += U(rname, kc, c0, c0 + n)
        self.mm_group(bank, n, pairs, reads)

    def st_init(self):
        nc = self.nc
        S = self
        for (lo, hi) in ((1024, TT), (0, 1024)):
            for c in range(NCH):
                def f(eng, sem, c=c, lo=lo, hi=hi):
                    return eng.dma_start(out=S.X[:, c, lo:hi], in_=S.xT[c * 128:(c + 1) * 128, lo:hi]).then_inc(sem, 16)
                self.add("sp", f, writes=U("X", c, lo, hi), dma=("xl", c, lo))

        def f(eng, sem):
            return eng.dma_start(out=S.PRM[:, :], in_=S.prm_d[:, :]).then_inc(sem, 16)
        self.add("sp", f, writes=[("prm",)], dma=("prm",))

        def f(eng):
            eng.memset(S.ONESB[:, :], 1.0)
            return eng.memset(S.IDF[:, :], 0.0)
        self.add("pool", f, writes=[("idf0",), ("ones",)])

        def f(eng):
            return eng.affine_select(out=S.IDF[:, :], in_=S.IDF[:, :], compare_op=ALU.not_equal, fill=1.0,
                                     base=0, pattern=[[-1, 128]], channel_multiplier=1)
        self.add("pool", f, reads=[("idf0",)], writes=[("idf",)])

        def f(eng):
            return eng.tensor_copy(out=S.IDB[:, :], in_=S.IDF[:, :])
        self.add("pool", f, reads=[("idf",)], writes=[("idb",)])

        if self.stop != "full":
            return

        def f(eng):
            eng.memset(S.ZT[:, :], 0.0)
            return eng.tensor_copy(out=S.UTB[:, :], in_=S.PRM[:, P_UT:P_UT + 128])
        self.add("pool", f, reads=[("prm",)], writes=[("zt",), ("utb",)])
        assert (NE * CAP) % 128 == 0
        hdv = S.HD.rearrange("(q p) d -> q p d", p=128)
        nq = NE * CAP // 128
        per = 20
        for q0 in range(0, nq, per):
            def f(eng, sem, q0=q0):
                last = None
                for q in range(q0, min(nq, q0 + per)):
                    last = eng.dma_start(out=hdv[q], in_=S.ZT[:, :]).then_inc(sem, 16)
                return last
            self.add("sp", f, reads=[("zt",)], writes=[("hdz",)], dma=("zi",), ndma=min(nq, q0 + per) - q0)

    def st_norm(self, blocks, xoff, hbuf, hname, gcol, mask=False, router=False):
        S = self
        for (c0, n) in blocks:
            g0 = xoff + c0
            bk = self.bank()
            for c in range(NCH):
                t = self.tb()

                def f(eng, c=c, t=t, g0=g0, n=n):
                    return eng.activation(out=S.TB[t][:, 0:n], in_=S.X[:, c, g0:g0 + n], func=AF.Square)
                self.add("act", f, reads=U("X", c, g0, g0 + n), writes=[("tb", t)])

                def f(eng, c=c, t=t, n=n, bk=bk):
                    return eng.matmul(S.PS[bk][:, 0:n], lhsT=S.ONESB[:, :], rhs=S.TB[t][:, 0:n],
                                      start=(c == 0), stop=(c == NCH - 1))
                self.add("pe", f, reads=[("tb", t), ("ones",)], writes=[("ps", bk)])
            t1 = self.tf()

            def f(eng, t1=t1, n=n, bk=bk):
                return eng.activation(out=S.TF[t1][:, 0:n], in_=S.PS[bk][:, 0:n], func=AF.Sqrt,
                                      scale=1.0 / D, bias=S.EPSR[:, 0:1])
            self.add("act", f, reads=[("ps", bk), ("eps",)], writes=[("tf", t1)])

            def f(eng, t1=t1, n=n):
                return eng.reciprocal(out=S.RSTD[:, 0:n], in_=S.TF[t1][:, 0:n])
            self.add("dve", f, reads=[("tf", t1)], writes=[("rstd",)])
            if mask and c0 < HALO:
                def f(eng, c0=c0, n=n):
                    return eng.tensor_tensor(out=S.RSTD[:, 0:n], in0=S.RSTD[:, 0:n],
                                             in1=S.PRM[:, P_MASK + c0:P_MASK + c0 + n], op=ALU.mult)
                self.add("dve", f, reads=[("rstd",), ("prm",)], writes=[("rstd",)])
            if router:
                rb = self.bank()
            for c in range(NCH):
                if not router:
                    def f(eng, c=c, c0=c0, g0=g0, n=n):
                        return eng.scalar_tensor_tensor(out=hbuf[:, c, c0:c0 + n], in0=S.X[:, c, g0:g0 + n],
                                                        scalar=S.PRM[:, gcol + c:gcol + c + 1],
                                                        in1=S.RSTD[:, 0:n], op0=ALU.mult, op1=ALU.mult)
                    self.add("dve", f, reads=U("X", c, g0, g0 + n) + [("rstd",), ("prm",)],
                             writes=U(hname, c, c0, c0 + n))
                else:
                    t2 = self.tf()

                    def f(eng, c=c, g0=g0, n=n, t2=t2):
                        return eng.scalar_tensor_tensor(out=S.TF[t2][:, 0:n], in0=S.X[:, c, g0:g0 + n],
                                                        scalar=S.PRM[:, gcol + c:gcol + c + 1],
                                                        in1=S.RSTD[:, 0:n], op0=ALU.mult, op1=ALU.mult)
                    self.add("dve", f, reads=U("X", c, g0, g0 + n) + [("rstd",), ("prm",)], writes=[("tf", t2)])

                    def f(eng, c=c, c0=c0, n=n, t2=t2):
                        return eng.activation(out=hbuf[:, c, c0:c0 + n], in_=S.TF[t2][:, 0:n], func=AF.Copy)
                    self.add("act", f, reads=[("tf", t2)], writes=U(hname, c, c0, c0 + n))

                    def f(eng, c=c, n=n, t2=t2, rb=rb):
                        return eng.matmul(S.PS[rb][0:NE, 0:n], lhsT=S.PRM[:, P_ROUT + c * NE:P_ROUT + (c + 1) * NE],
                                          rhs=S.TF[t2][:, 0:n], start=(c == 0), stop=(c == NCH - 1))
                    self.add("pe", f, reads=[("tf", t2), ("prm",)], writes=[("ps", rb)])
            if router:
                t3 = self.tf()

                def f(eng, n=n, t3=t3, rb=rb):
                    return eng.activation(out=S.TF[t3][0:NE, 0:n], in_=S.PS[rb][0:NE, 0:n], func=AF.Copy)
                self.add("act", f, reads=[("ps", rb)], writes=[("tf", t3)])
                tb_ = self.bank()
                nt = n // 128

                def f(eng, t3=t3, tb_=tb_, nt=nt):
                    last = None
                    for j in range(nt):
                        last = eng.transpose(out=S.PS[tb_][:, j * NE:(j + 1) * NE],
                                             in_=S.TF[t3][0:NE, j * 128:(j + 1) * 128],
                                             identity=S.IDF[0:NE, 0:NE])
                    return last
                self.add("pe", f, reads=[("tf", t3), ("idf",)], writes=[("ps", tb_)])
                j0 = (g0 - HALO) // 128

                def f(eng, tb_=tb_, nt=nt, j0=j0):
                    return eng.tensor_copy(out=S.LG[:, j0:j0 + nt, :],
                                           in_=S.PS[tb_][:, 0:nt * NE].rearrange("p (j e) -> p j e", e=NE))
                self.add("dve", f, reads=[("ps", tb_)], writes=[("lg", j0)])

    def blocks_of(self, first):
        b = []
        if first < HALO:
            b.append((first, HALO - first))
        b += [(HALO, 512), (HALO + 512, 512)]
        return b

    def st_outproj(self, l, w_out, gbase, post, xoff, first, last):
        S = self
        mgres = [("w", NS - 1)]
        pending = None
        for grp in range(2):
            kw, wsl = self.wget(self.ld_cols(w_out[l], grp * 512))
            kg, gsl = self.wget(self.ld_cols(self.w_in[l], gbase + grp * 512))
            for (c0, n) in post:
                for j in range(4):
                    d = grp * 4 + j
                    by = self.bank()
                    self.proj(wsl, kw, j, S.T1, "T1", c0, n, by)
                    bg = self.bank()
                    self.proj(gsl, kg, j, S.H, "H", c0, n, bg)
                    t = self.tf()

                    def f(eng, t=t, n=n, bg=bg):
                        return eng.activation(out=S.TF[t][:, 0:n], in_=S.PS[bg][:, 0:n], func=AF.Sigmoid)
                    self.add("act", f, reads=[("ps", bg)], writes=[("tf", t)])
                    if first:
                        def f(eng, t=t, n=n, by=by, d=d, c0=c0):
                            return eng.tensor_tensor(out=S.MG[:, d, c0:c0 + n], in0=S.PS[by][:, 0:n],
                                                     in1=S.TF[t][:, 0:n], op=ALU.mult)
                        self.add("dve", f, reads=[("ps", by), ("tf", t)], writes=U("MG", d, c0, c0 + n) + mgres)
                    else:
                        def f(eng, t=t, n=n, by=by):
                            return eng.tensor_tensor(out=S.TF[t][:, 0:n], in0=S.PS[by][:, 0:n],
                                                     in1=S.TF[t][:, 0:n], op=ALU.mult)
                        self.add("dve", f, reads=[("ps", by), ("tf", t)], writes=[("tf", t)])
                        if pending is not None:
                            pending()

                        def pend(t=t, n=n, d=d, c0=c0):
                            def f(eng):
                                return eng.tensor_tensor(out=S.MG[:, d, c0:c0 + n], in0=S.MG[:, d, c0:c0 + n],
                                                         in1=S.TF[t][:, 0:n], op=ALU.add)
                            self.add("dve", f, reads=[("tf", t)] + U("MG", d, c0, c0 + n),
                                     writes=U("MG", d, c0, c0 + n) + mgres)
                        pending = pend
            self.wrel(kw)
            self.wrel(kg)
        if pending is not None:
            pending()
        if not last:
            return
        for grp in range(2):
            ko, osl = self.wget(self.ld_cols(self.w_o[l], grp * 512))
            for (c0, n) in post:
                for j in range(4):
                    e = grp * 4 + j
                    b = self.bank()
                    self.proj(osl, ko, j, S.MG, "MG", c0, n, b, extra=mgres)
                    g0 = xoff + c0

                    def f(eng, e=e, g0=g0, n=n, b=b):
                        return eng.tensor_tensor(out=S.X[:, e, g0:g0 + n], in0=S.X[:, e, g0:g0 + n],
                                                 in1=S.PS[b][:, 0:n], op=ALU.add)
                    self.add("dve", f, reads=[("ps", b)] + U("X", e, g0, g0 + n), writes=U("X", e, g0, g0 + n))
            self.wrel(ko)

    def build_diag(self, col0, ntap):
        S = self
        db = self.idg % 2
        self.idg += 1

        def f(eng, db=db, col0=col0, ntap=ntap):
            return eng.tensor_tensor(out=S.DIAG[db][:, 0:ntap, :],
                                     in0=S.IDB[:, :].unsqueeze(1).to_broadcast([128, ntap, 128]),
                                     in1=S.PRM[:, col0:col0 + ntap].unsqueeze(2).to_broadcast([128, ntap, 128]),
                                     op=ALU.mult)
        self.add("dve", f, reads=[("idb",), ("prm",)], writes=[("diag", db, 0)])
        return db

    def st_mixer_half(self, l, half, ci0, po0):
        S = self
        xoff = 0 if half == 0 else 1024
        ci = self.blocks_of(ci0)
        post = self.blocks_of(po0)
        pl = l * P_LAYER
        win = self.w_in[l]
        self.st_norm(ci, xoff, S.H, "H", pl + P_NMG, mask=(half == 0))

        for grp in range(2):
            k, sl = self.wget(self.ld_cols(win, C_PU + grp * 512))
            for (c0, n) in ci:
                for j in range(4):
                    c = grp * 4 + j
                    b = self.bank()
                    self.proj(sl, k, j, S.H, "H", c0, n, b)

                    def f(eng, c=c, c0=c0, n=n, b=b):
                        return eng.activation(out=S.T2[:, c, c0:c0 + n], in_=S.PS[b][:, 0:n], func=AF.Copy)
                    self.add("act", f, reads=[("ps", b)], writes=U("T2", c, c0, c0 + n))
            self.wrel(k)
        kp, psl = self.wget(self.ld_poolw(self.pool_w[l]))
        pwv = psl[:, 0:2048].rearrange("p (k c) -> p k c", k=8)
        for g in range(4):
            w = WINS[g]
            for cc in range(2):
                c = 2 * g + cc
                for (c0, n) in post:
                    b = self.bank()
                    pairs = [(S.IDB[:, :], S.T2[:, c, c0 - jj:c0 - jj + n]) for jj in range(w)]
                    self.mm_group(b, n, pairs, [("idb",)] + U("T2", c, c0 - w + 1, c0 + n))

                    def f(eng, c=c, c0=c0, n=n, b=b, w=w):
                        return eng.scalar_tensor_tensor(out=S.T1[:, c, c0:c0 + n], in0=S.PS[b][:, 0:n],
                                                        scalar=1.0 / w, in1=S.T2[:, c, c0:c0 + n],
                                                        op0=ALU.mult, op1=ALU.subtract)
                    self.add("dve", f, reads=[("ps", b)] + U("T2", c, c0, c0 + n), writes=U("T1", c, c0, c0 + n))
                    if half == 0 and c0 == HALO:
                        t = self.tf()

                        def f(eng, t=t, b=b, g=g):
                            return eng.tensor_tensor(out=S.TF[t][:, 0:16], in0=S.PS[b][:, 0:16],
                                                     in1=S.PRM[:, P_INVC + g * 16:P_INVC + (g + 1) * 16], op=ALU.mult)
                        self.add("dve", f, reads=[("ps", b), ("prm",)], writes=[("tf", t)])

                        def f(eng, t=t, c=c):
                            return eng.tensor_tensor(out=S.T1[:, c, HALO:HALO + 16], in0=S.TF[t][:, 0:16],
                                                     in1=S.T2[:, c, HALO:HALO + 16], op=ALU.subtract)
                        self.add("dve", f, reads=[("tf", t)] + U("T2", c, HALO, HALO + 16),
                                 writes=U("T1", c, HALO, HALO + 16))
            for (c0, n) in post:
                bs = []
                for e2 in range(2):
                    b = self.bank()
                    bs.append(b)
                    pairs = [(pwv[:, g * 2 + kc, e2 * 128:(e2 + 1) * 128], S.T1[:, 2 * g + kc, c0:c0 + n])
                             for kc in range(2)]
                    rd = [("w", self.slot_plan[kp])] + U("T1", 2 * g, c0, c0 + n) + U("T1", 2 * g + 1, c0, c0 + n)
                    self.mm_group(b, n, pairs, rd)
                for e2 in range(2):
                    ch = 2 * g + e2

                    def f(eng, ch=ch, c0=c0, n=n, b=bs[e2]):
                        return eng.activation(out=S.T1[:, ch, c0:c0 + n], in_=S.PS[b][:, 0:n], func=AF.Copy,
                                              scale=S.PRM[:, pl + P_PSC + ch:pl + P_PSC + ch + 1])
                    self.add("act", f, reads=[("ps", bs[e2]), ("prm",)], writes=U("T1", ch, c0, c0 + n))
        self.wrel(kp)
        self.st_outproj(l, self.w_pool_out, C_GC, post, xoff, True, False)

        for grp in range(2):
            kc_, csl = self.wget(self.ld_cols(win, C_SCC + grp * 512))
            kx_, xsl = self.wget(self.ld_cols(win, C_SCX + grp * 512))
            for j in range(4):
                c = grp * 4 + j
                for (c0, n) in ci:
                    b1 = self.bank()
                    self.proj(csl, kc_, j, S.H, "H", c0, n, b1)
                    b2 = self.bank()
                    self.proj(xsl, kx_, j, S.H, "H", c0, n, b2)
                    t = self.tf()

                    def f(eng, t=t, n=n, b1=b1):
                        return eng.activation(out=S.TF[t][:, 0:n], in_=S.PS[b1][:, 0:n], func=AF.Copy)
                    self.add("act", f, reads=[("ps", b1)], writes=[("tf", t)])

                    def f(eng, t=t, n=n, b2=b2, c=c, c0=c0):
                        return eng.tensor_tensor(out=S.T2[:, c, c0:c0 + n], in0=S.PS[b2][:, 0:n],
                                                 in1=S.TF[t][:, 0:n], op=ALU.mult)
                    self.add("dve", f, reads=[("ps", b2), ("tf", t)], writes=U("T2", c, c0, c0 + n))
            self.wrel(kc_)
            self.wrel(kx_)
        for grp in range(2):
            kb_, bsl = self.wget(self.ld_cols(win, C_SCB + grp * 512))
            for j in range(4):
                c = grp * 4 + j
                db = self.build_diag(pl + P_SCW + c * 3, 3)
                for (c0, n) in post:
                    b1 = self.bank()
                    pairs = [(S.DIAG[db][:, kk, :], S.T2[:, c, c0 - 2 + kk:c0 - 2 + kk + n]) for kk in range(3)]
                    self.mm_group(b1, n, pairs, [("diag", db, 0)] + U("T2", c, c0 - 2, c0 + n))
                    b2 = self.bank()
                    self.proj(bsl, kb_, j, S.H, "H", c0, n, b2)
                    t = self.tf()

                    def f(eng, t=t, n=n, b2=b2):
                        return eng.activation(out=S.TF[t][:, 0:n], in_=S.PS[b2][:, 0:n], func=AF.Copy)
                    self.add("act", f, reads=[("ps", b2)], writes=[("tf", t)])

                    def f(eng, t=t, n=n, b1=b1, c=c, c0=c0):
                        return eng.tensor_tensor(out=S.T1[:, c, c0:c0 + n], in0=S.PS[b1][:, 0:n],
                                                 in1=S.TF[t][:, 0:n], op=ALU.mult)
                    self.add("dve", f, reads=[("ps", b1), ("tf", t)], writes=U("T1", c, c0, c0 + n))
            self.wrel(kb_)
        self.st_outproj(l, self.w_sc_out, C_GA, post, xoff, False, False)

        for grp in range(2):
            kg_, gsl = self.wget(self.ld_cols(win, C_CFG + grp * 512))
            kv_, vsl = self.wget(self.ld_cols(win, C_CFV + grp * 512))
            for j in range(4):
                c = grp * 4 + j
                for (c0, n) in ci:
                    b1 = self.bank()
                    self.proj(gsl, kg_, j, S.H, "H", c0, n, b1)
                    b2 = self.bank()
                    self.proj(vsl, kv_, j, S.H, "H", c0, n, b2)
                    t = self.tf()

                    def f(eng, t=t, n=n, b1=b1):
                        return eng.activation(out=S.TF[t][:, 0:n], in_=S.PS[b1][:, 0:n], func=AF.Sigmoid)
                    self.add("act", f, reads=[("ps", b1)], writes=[("tf", t)])

                    def f(eng, t=t, n=n, b2=b2, c=c, c0=c0):
                        return eng.tensor_tensor(out=S.T2[:, c, c0:c0 + n], in0=S.PS[b2][:, 0:n],
                                                 in1=S.TF[t][:, 0:n], op=ALU.mult)
                    self.add("dve", f, reads=[("ps", b2), ("tf", t)], writes=U("T2", c, c0, c0 + n))
            self.wrel(kg_)
            self.wrel(kv_)
        for c in range(NCH):
            db = self.build_diag(pl + P_CFW + c * 31, 31)
            for (c0, n) in post:
                b = self.bank()
                pairs = [(S.DIAG[db][:, kk, :], S.T2[:, c, c0 - 30 + kk:c0 - 30 + kk + n]) for kk in range(31)]
                self.mm_group(b, n, pairs, [("diag", db, 0)] + U("T2", c, c0 - 30, c0 + n))

                def f(eng, c=c, c0=c0, n=n, b=b):
                    return eng.activation(out=S.T1[:, c, c0:c0 + n], in_=S.PS[b][:, 0:n], func=AF.Identity,
                                          bias=S.PRM[:, pl + P_CFB + c:pl + P_CFB + c + 1])
                self.add("act", f, reads=[("ps", b), ("prm",)], writes=U("T1", c, c0, c0 + n))
        for (c0, n) in post:
            b1 = self.bank()
            b2 = self.bank()
            for c in range(NCH):
                t = self.tb()

                def f(eng, c=c, t=t, c0=c0, n=n):
                    return eng.activation(out=S.TB[t][:, 0:n], in_=S.T1[:, c, c0:c0 + n], func=AF.Square)
                self.add("act", f, reads=U("T1", c, c0, c0 + n), writes=[("tb", t)])

                def f(eng, c=c, t=t, c0=c0, n=n, b1=b1, b2=b2):
                    eng.matmul(S.PS[b1][:, 0:n], lhsT=S.ONESB[:, :], rhs=S.T1[:, c, c0:c0 + n],
                               start=(c == 0), stop=(c == NCH - 1))
                    return eng.matmul(S.PS[b2][:, 0:n], lhsT=S.ONESB[:, :], rhs=S.TB[t][:, 0:n],
                                      start=(c == 0), stop=(c == NCH - 1))
                self.add("pe", f, reads=[("tb", t), ("ones",)] + U("T1", c, c0, c0 + n),
                         writes=[("ps", b1), ("ps", b2)])

            def f(eng, n=n, b1=b1):
                return eng.activation(out=S.MEAN[:, 0:n], in_=S.PS[b1][:, 0:n], func=AF.Copy, scale=1.0 / D)
            self.add("act", f, reads=[("ps", b1)], writes=[("mean",)])
            ta = self.tf()

            def f(eng, n=n, ta=ta):
                return eng.tensor_tensor(out=S.TF[ta][:, 0:n], in0=S.MEAN[:, 0:n], in1=S.MEAN[:, 0:n], op=ALU.mult)
            self.add("dve", f, reads=[("mean",)], writes=[("tf", ta)])
            tv = self.tf()

            def f(eng, n=n, ta=ta, tv=tv, b2=b2):
                return eng.scalar_tensor_tensor(out=S.TF[tv][:, 0:n], in0=S.PS[b2][:, 0:n], scalar=1.0 / D,
                                                in1=S.TF[ta][:, 0:n], op0=ALU.mult, op1=ALU.subtract)
            self.add("dve", f, reads=[("ps", b2), ("tf", ta)], writes=[("tf", tv)])
            tr = self.tf()

            def f(eng, n=n, tv=tv, tr=tr):
                return eng.activation(out=S.TF[tr][:, 0:n], in_=S.TF[tv][:, 0:n], func=AF.Sqrt,
                                      bias=S.EPSL[:, 0:1])
            self.add("act", f, reads=[("tf", tv), ("eps",)], writes=[("tf", tr)])

            def f(eng, n=n, tr=tr):
                return eng.reciprocal(out=S.RSTD[:, 0:n], in_=S.TF[tr][:, 0:n])
            self.add("dve", f, reads=[("tf", tr)], writes=[("rstd",)])
            for c in range(NCH):
                t1 = self.tf()

                def f(eng, c=c, c0=c0, n=n, t1=t1):
                    return eng.tensor_tensor(out=S.TF[t1][:, 0:n], in0=S.T1[:, c, c0:c0 + n], in1=S.MEAN[:, 0:n],
                                             op=ALU.subtract)
                self.add("dve", f, reads=U("T1", c, c0, c0 + n) + [("mean",)], writes=[("tf", t1)])
                t2 = self.tf()

                def f(eng, n=n, t1=t1, t2=t2):
                    return eng.tensor_tensor(out=S.TF[t2][:, 0:n], in0=S.TF[t1][:, 0:n], in1=S.RSTD[:, 0:n],
                                             op=ALU.mult)
                self.add("dve", f, reads=[("tf", t1), ("rstd",)], writes=[("tf", t2)])

                def f(eng, c=c, c0=c0, n=n, t2=t2):
                    return eng.activation(out=S.T1[:, c, c0:c0 + n], in_=S.TF[t2][:, 0:n], func=AF.Silu,
                                          scale=S.PRM[:, pl + P_LNG + c:pl + P_LNG + c + 1],
                                          bias=S.PRM[:, pl + P_LNB + c:pl + P_LNB + c + 1])
                self.add("act", f, reads=[("tf", t2), ("prm",)], writes=U("T1", c, c0, c0 + n))
        self.st_outproj(l, self.w_cf_out, C_GB, post, xoff, False, True)

    def st_ffn(self, l, blocks, experts):
        S = self
        for (w1, w3, w2) in experts:
            for fg in range(NFG):
                k1, s1 = self.wget(self.ld_cols(w1, fg * 512))
                k3, s3 = self.wget(self.ld_cols(w3, fg * 512))
                k2, s2 = self.wget(self.ld_rows(w2, fg * 512))
                for (c0, n) in blocks:
                    for j in range(4):
                        ba = self.bank()
                        self.proj(s1, k1, j, S.H2, "H2", c0, n, ba)
                        bc = self.bank()
                        self.proj(s3, k3, j, S.H2, "H2", c0, n, bc)
                        t = self.tf()

                        def f(eng, t=t, n=n, ba=ba):
                            return eng.activation(out=S.TF[t][:, 0:n], in_=S.PS[ba][:, 0:n], func=AF.Silu)
                        self.add("act", f, reads=[("ps", ba)], writes=[("tf", t)])

                        def f(eng, t=t, n=n, bc=bc, j=j, c0=c0):
                            return eng.tensor_tensor(out=S.GS[:, j, c0:c0 + n], in0=S.PS[bc][:, 0:n],
                                                     in1=S.TF[t][:, 0:n], op=ALU.mult)
                        self.add("dve", f, reads=[("ps", bc), ("tf", t)], writes=U("GS", j, c0, c0 + n))
                self.wrel(k1)
                self.wrel(k3)
                w2v = s2[:, :].rearrange("p (f d) -> p f d", f=4)
                for (c0, n) in blocks:
                    for d in range(NCH):
                        b = self.bank()
                        pairs = [(w2v[:, j, d * 128:(d + 1) * 128], S.GS[:, j, c0:c0 + n]) for j in range(4)]
                        rd = [("w", self.slot_plan[k2])]
                        for j in range(4):
                            rd += U("GS", j, c0, c0 + n)
                        self.mm_group(b, n, pairs, rd)

                        def f(eng, d=d, c0=c0, n=n, b=b):
                            return eng.tensor_tensor(out=S.X[:, d, c0:c0 + n], in0=S.X[:, d, c0:c0 + n],
                                                     in1=S.PS[b][:, 0:n], op=ALU.add)
                        self.add("dve", f, reads=[("ps", b)] + U("X", d, c0, c0 + n), writes=U("X", d, c0, c0 + n))
                self.wrel(k2)

    def st_route(self):
        S = self
        allg = [("lg", j0) for j0 in range(0, 16, 4)]

        def bc(ap):
            return ap.unsqueeze(2).to_broadcast([128, NTT, NE])

        def f(eng):
            return eng.tensor_reduce(out=S.M1[:, :], in_=S.LG[:, :, :], axis=AX.X, op=ALU.max)
        self.add("dve", f, reads=allg, writes=[("m1",)])

        def f(eng):
            return eng.tensor_tensor(out=S.EQ1[:, :, :], in0=S.LG[:, :, :], in1=bc(S.M1[:, :]), op=ALU.is_equal)
        self.add("dve", f, reads=allg + [("m1",)], writes=[("eq1",)])

        def f(eng):
            return eng.scalar_tensor_tensor(out=S.L2[:, :, :], in0=S.EQ1[:, :, :], scalar=-1e30, in1=S.LG[:, :, :],
                                            op0=ALU.mult, op1=ALU.add)
        self.add("dve", f, reads=allg + [("eq1",)], writes=[("l2",)])

        def f(eng):
            return eng.tensor_reduce(out=S.M2[:, :], in_=S.L2[:, :, :], axis=AX.X, op=ALU.max)
        self.add("dve", f, reads=[("l2",)], writes=[("m2",)])

        def f(eng):
            return eng.tensor_tensor(out=S.EQ2[:, :, :], in0=S.L2[:, :, :], in1=bc(S.M2[:, :]), op=ALU.is_equal)
        self.add("dve", f, reads=[("l2",), ("m2",)], writes=[("eq2",)])

        def f(eng):
            return eng.tensor_tensor(out=S.M2[:, :], in0=S.M1[:, :], in1=S.M2[:, :], op=ALU.subtract)
        self.add("dve", f, reads=[("m1",), ("m2",), ("eq2",)], writes=[("dd",)])

        def f(eng):
            eng.activation(out=S.P1[:, :], in_=S.M2[:, :], func=AF.Sigmoid)
            return eng.activation(out=S.P2[:, :], in_=S.M2[:, :], func=AF.Sigmoid, scale=-1.0)
        self.add("act", f, reads=[("dd",)], writes=[("p12",)])

        def f(eng):
            return eng.tensor_tensor(out=S.MSKB[:, :, :], in0=S.EQ1[:, :, :], in1=S.EQ2[:, :, :], op=ALU.add)
        self.add("dve", f, reads=[("eq1",), ("eq2",), ("hdz",)], writes=[("mskb",)])
        ba = self.bank()
        bc_ = self.bank()
        mflat = S.MSKB[:, :, :].rearrange("p t e -> p (t e)")

        def f(eng, ba=ba, bc_=bc_):
            eng.matmul(S.PS[ba][:, 0:NTT * NE], lhsT=S.UTB[:, :], rhs=mflat, start=True, stop=True)
            return eng.matmul(S.PS[bc_][:, 0:NTT * NE], lhsT=S.ONESB[:, :], rhs=mflat, start=True, stop=True)
        self.add("pe", f, reads=[("mskb",), ("utb",), ("ones",)], writes=[("ps", ba), ("ps", bc_)])

        def v3(ap):
            return ap.rearrange("p (t e) -> p t e", e=NE)

        def f(eng, bc_=bc_):
            return eng.tensor_copy(out=S.CNT[:, :, :], in_=v3(S.PS[bc_][:, 0:NTT * NE]))
        self.add("dve", f, reads=[("ps", bc_)], writes=[("cntb",)])
        bufs = [(S.CNT, "cntb"), (S.OFF, "offb"), (S.TMP, "tmpb"), (S.OFF, "offb"), (S.TMP, "tmpb")]
        for k, sh in enumerate((1, 2, 4, 8)):
            (src, sn), (dst, dn) = bufs[k], bufs[k + 1]

            def f(eng, src=src, dst=dst, sh=sh):
                eng.tensor_copy(out=dst[:, 0:sh, :], in_=src[:, 0:sh, :])
                return eng.tensor_tensor(out=dst[:, sh:NTT, :], in0=src[:, sh:NTT, :], in1=src[:, 0:NTT - sh, :], op=ALU.add)
            self.add("dve", f, reads=[(sn,)], writes=[(dn,)])

        def f(eng):
            return eng.tensor_copy(out=S.CNTI[:, :], in_=S.TMP[:, NTT - 1, :])
        self.add("dve", f, reads=[("tmpb",)], writes=[("cnti0",)])

        def f(eng):
            return eng.tensor_reduce(out=S.CMAXF[:, :], in_=S.TMP[:, NTT - 1, :], axis=AX.X, op=ALU.max)
        self.add("dve", f, reads=[("tmpb",)], writes=[("cmaxf",)])

        def f(eng):
            return eng.tensor_copy(out=S.CMAXI[:, :], in_=S.CMAXF[:, :])
        self.cnti_op = self.add("dve", f, reads=[("cmaxf",), ("cnti0",)], writes=[("cnti",)])

        def f(eng):
            return eng.tensor_tensor(out=S.OFF[:, :, :], in0=S.TMP[:, :, :], in1=S.CNT[:, :, :], op=ALU.subtract)
        self.add("dve", f, reads=[("tmpb",), ("cntb",)], writes=[("offb",)])

        def f(eng):
            return eng.tensor_tensor(out=S.CNT[:, :, :], in0=S.OFF[:, :, :],
                                     in1=S.PRM[:, P_ECAP:P_ECAP + NE].unsqueeze(1).to_broadcast([128, NTT, NE]),
                                     op=ALU.add)
        self.add("dve", f, reads=[("offb",), ("prm",)], writes=[("cntb",)])

        def f(eng, ba=ba):
            return eng.tensor_tensor(out=S.POS[:, :, :], in0=v3(S.PS[ba][:, 0:NTT * NE]), in1=S.CNT[:, :, :], op=ALU.add)
        self.add("dve", f, reads=[("ps", ba), ("cntb",)], writes=[("pos",)])

        def f(eng):
            return eng.tensor_tensor(out=S.TMP[:, :, :], in0=S.EQ1[:, :, :], in1=S.POS[:, :, :], op=ALU.mult)
        self.add("dve", f, reads=[("pos",), ("eq1",), ("cnti",)], writes=[("tmpb",)])

        def f(eng):
            return eng.tensor_reduce(out=S.D1[:, :], in_=S.TMP[:, :, :], axis=AX.X, op=ALU.add)
        self.add("dve", f, reads=[("tmpb",)], writes=[("d1",)])

        def f(eng):
            return eng.tensor_tensor(out=S.OFF[:, :, :], in0=S.EQ2[:, :, :], in1=S.POS[:, :, :], op=ALU.mult)
        self.add("dve", f, reads=[("pos",), ("eq2",)], writes=[("offb",)])

        def f(eng):
            return eng.tensor_reduce(out=S.D2[:, :], in_=S.OFF[:, :, :], axis=AX.X, op=ALU.add)
        self.add("dve", f, reads=[("offb",)], writes=[("d2",)])

        def f(eng):
            return eng.tensor_copy(out=S.DI1[:, :], in_=S.D1[:, :])
        self.add("dve", f, reads=[("d1",)], writes=[("di1",)])

        def f(eng):
            return eng.tensor_copy(out=S.DI2[:, :], in_=S.D2[:, :])
        self.add("dve", f, reads=[("d2",)], writes=[("di",)])
        if self.debug:
            def f(eng, sem):
                eng.dma_start(out=S.DBG[:, 0:16], in_=S.DI1[:, :]).then_inc(sem, 16)
                eng.dma_start(out=S.DBG[:, 16:32], in_=S.DI2[:, :]).then_inc(sem, 16)
                return eng.dma_start(out=S.DBG[:, 32:40], in_=S.CNTI[:, :]).then_inc(sem, 16)
            self.add("sp", f, reads=[("di",), ("di1",), ("cnti",)], writes=[("dbg",)], dma=("dbg",), ndma=3)

    def st_scatter(self):
        S = self
        for tt in range(NTT):
            t0 = HALO + tt * 128
            bs = [self.bank(), self.bank()]

            def f(eng, t0=t0, bs=bs):
                last = None
                for kc in range(NCH):
                    last = eng.matmul(S.PS[bs[kc // 4]][:, (kc % 4) * 128:(kc % 4 + 1) * 128],
                                      lhsT=S.H2[:, kc, t0:t0 + 128], rhs=S.IDB[:, :], start=True, stop=True)
                return last
            rd = [("idb",)]
            for kc in range(NCH):
                rd += U("H2", kc, t0, t0 + 128)
            self.add("pe", f, reads=rd, writes=[("ps", bs[0]), ("ps", bs[1])])
            i = self.ht()

            def f(eng, i=i, b=bs[0]):
                return eng.activation(out=S.HT[i][:, 0:512], in_=S.PS[b][:, 0:512], func=AF.Copy)
            self.add("act", f, reads=[("ps", bs[0])], writes=[("ht", i, 0)])

            def f(eng, i=i, b=bs[1]):
                return eng.tensor_copy(out=S.HT[i][:, 512:1024], in_=S.PS[b][:, 0:512])
            self.add("dve", f, reads=[("ps", bs[1])], writes=[("ht", i, 1)])

            def f(eng, sem, i=i, tt=tt):
                eng.indirect_dma_start(out=S.HD[:, :], out_offset=bass.IndirectOffsetOnAxis(ap=S.DI1[:, tt:tt + 1], axis=0),
                                       in_=S.HT[i], in_offset=None, bounds_check=S.rb_pool,
                                       oob_is_err=False).then_inc(sem, 16)
                return eng.indirect_dma_start(out=S.HD[:, :],
                                              out_offset=bass.IndirectOffsetOnAxis(ap=S.DI2[:, tt:tt + 1], axis=0),
                                              in_=S.HT[i], in_offset=None, bounds_check=S.rb_pool,
                                              oob_is_err=False).then_inc(sem, 16)
            self.add("pool", f, reads=[("ht", i, 0), ("ht", i, 1), ("di",), ("di1",), ("hdz",)], writes=[("hd", tt)],
                     dma=("scat", tt % 4), ndma=2)

    def pass_load(self, e, r):
        S = self
        row0 = e * CAP + r * BLK

        nfull = BLK // 128
        rem = BLK - nfull * 128

        def f(eng, sem, row0=row0):
            last = eng.dma_start(out=S.HGS[:, 0:nfull, :],
                                 in_=S.HD[row0:row0 + nfull * 128, :].rearrange("(s p) d -> p s d", p=128)).then_inc(sem, 16)
            if rem:
                last = eng.dma_start(out=S.HGS[0:rem, nfull, :],
                                     in_=S.HD[row0 + nfull * 128:row0 + BLK, :]).then_inc(sem, 16)
            return last
        self.add("sp", f, reads=[("hdz",)] + [("hd", tt) for tt in range(NTT)], writes=[("hgs",)], dma=("hgl",),
                 ndma=2 if rem else 1)

    def st_pass(self, e, r, yi, prefetch=None):
        S = self
        row0 = e * CAP + r * BLK
        if prefetch is None or not prefetch[0]:
            self.pass_load(e, r)
        alt = 0
        for kc in range(NCH):
            for (c0, n) in CB:
                b = self.bank()

                def f(eng, kc=kc, c0=c0, n=n, b=b):
                    last = None
                    for (sl, rows) in STL:
                        if not (c0 <= sl * 128 < c0 + n):
                            continue
                        o = sl * 128 - c0
                        last = eng.matmul(S.PS[b][:, o:o + rows], lhsT=S.HGS[0:rows, sl, kc * 128:(kc + 1) * 128],
                                          rhs=S.IDB[0:rows, 0:rows], start=True, stop=True)
                    return last
                self.add("pe", f, reads=[("hgs",), ("idb",)], writes=[("ps", b)])
                if alt % 2 == 0:
                    def f(eng, kc=kc, c0=c0, n=n, b=b):
                        return eng.activation(out=S.HG[:, kc, c0:c0 + n], in_=S.PS[b][:, 0:n], func=AF.Copy)
                    self.add("act", f, reads=[("ps", b)], writes=U("HG", kc, c0, c0 + n))
                else:
                    def f(eng, kc=kc, c0=c0, n=n, b=b):
                        return eng.tensor_copy(out=S.HG[:, kc, c0:c0 + n], in_=S.PS[b][:, 0:n])
                    self.add("dve", f, reads=[("ps", b)], writes=U("HG", kc, c0, c0 + n))
                alt += 1
        if prefetch is not None and prefetch[1] is not None:
            self.pass_load(prefetch[1], 0)
        w1, w3, w2 = self.moe_w1[0, e], self.moe_w3[0, e], self.moe_w2[0, e]
        Y = S.YACC[yi]
        for fg in range(NFG):
            k1, s1 = self.wget(self.ld_cols(w1, fg * 512))
            k3, s3 = self.wget(self.ld_cols(w3, fg * 512))
            k2, s2 = self.wget(self.ld_rows(w2, fg * 512))
            for (c0, n) in CB:
                for j in range(4):
                    ba = self.bank()
                    self.proj(s1, k1, j, S.HG, "HG", c0, n, ba)
                    bc = self.bank()
                    self.proj(s3, k3, j, S.HG, "HG", c0, n, bc)
                    t = self.tf()

                    def f(eng, t=t, n=n, ba=ba):
                        return eng.activation(out=S.TF[t][:, 0:n], in_=S.PS[ba][:, 0:n], func=AF.Silu)
                    self.add("act", f, reads=[("ps", ba)], writes=[("tf", t)])

                    def f(eng, t=t, n=n, bc=bc, j=j, c0=c0):
                        return eng.tensor_tensor(out=S.GSR[:, j, c0:c0 + n], in0=S.PS[bc][:, 0:n],
                                                 in1=S.TF[t][:, 0:n], op=ALU.mult)
                    self.add("dve", f, reads=[("ps", bc), ("tf", t)], writes=U("GSR", j, c0, c0 + n))
            self.wrel(k1)
            self.wrel(k3)
            w2v = s2[:, :].rearrange("p (f d) -> p f d", f=4)
            for (sl, rows) in STL:
                for hf in range(2):
                    b = self.bank()
                    pairs = [(S.GSR[:, j, sl * 128:sl * 128 + rows], w2v[:, j, hf * 512:(hf + 1) * 512]) for j in range(4)]
                    rd = [("w", self.slot_plan[k2])]
                    for j in range(4):
                        rd += U("GSR", j, sl * 128, sl * 128 + rows)
                    self.mm_group(b, 512, pairs, rd)
                    yres = ("yacc", yi, sl, hf)
                    if fg == 0:
                        def f(eng, sl=sl, hf=hf, b=b, Y=Y, rows=rows):
                            return eng.activation(out=Y[0:rows, sl, hf * 512:(hf + 1) * 512], in_=S.PS[b][0:rows, 0:512],
                                                  func=AF.Copy)
                        self.add("act", f, reads=[("ps", b)], writes=[yres])
                    else:
                        def f(eng, sl=sl, hf=hf, b=b, Y=Y, rows=rows):
                            return eng.tensor_tensor(out=Y[0:rows, sl, hf * 512:(hf + 1) * 512],
                                                     in0=Y[0:rows, sl, hf * 512:(hf + 1) * 512], in1=S.PS[b][0:rows, 0:512],
                                                     op=ALU.add)
                        self.add("dve", f, reads=[("ps", b), yres], writes=[yres])
            self.wrel(k2)

        nfull = BLK // 128
        rem = BLK - nfull * 128

        def f(eng, sem, row0=row0, Y=Y):
            last = eng.dma_start(out=S.YD[row0:row0 + nfull * 128, :].rearrange("(s p) d -> p s d", p=128),
                                 in_=Y[:, 0:nfull, :]).then_inc(sem, 16)
            if rem:
                last = eng.dma_start(out=S.YD[row0 + nfull * 128:row0 + BLK, :], in_=Y[0:rem, nfull, :]).then_inc(sem, 16)
            return last
        self.add("sp", f, reads=[("yacc", yi, sl, hf) for sl in range(NSLT) for hf in range(2)], writes=[("yd",)],
                 dma=("yst",), ndma=2 if rem else 1)

    def st_combine(self):
        S = self
        for tt in range(NTT):
            t0 = HALO + tt * 128
            g1 = self.gt()
            g2 = self.gt()

            def f(eng, sem, g1=g1, g2=g2, tt=tt):
                eng.indirect_dma_start(out=S.GT[g1], out_offset=None, in_=S.YD[:, :],
                                       in_offset=bass.IndirectOffsetOnAxis(ap=S.DI1[:, tt:tt + 1], axis=0),
                                       bounds_check=S.rb_pool, oob_is_err=False).then_inc(sem, 16)
                return eng.indirect_dma_start(out=S.GT[g2], out_offset=None, in_=S.YD[:, :],
                                              in_offset=bass.IndirectOffsetOnAxis(ap=S.DI2[:, tt:tt + 1], axis=0),
                                              bounds_check=S.rb_pool, oob_is_err=False).then_inc(sem, 16)
            self.add("pool", f, reads=[("yd",), ("di",), ("di1",)], writes=[("gt", g1), ("gt", g2)], dma=("gath", tt % 4),
                     ndma=2)

            def f(eng, g1=g1, tt=tt):
                return eng.activation(out=S.GT[g1], in_=S.GT[g1], func=AF.Copy, scale=S.P1[:, tt:tt + 1])
            self.add("act", f, reads=[("gt", g1), ("p12",)], writes=[("gt", g1)])

            def f(eng, g1=g1, g2=g2, tt=tt):
                return eng.scalar_tensor_tensor(out=S.GT[g1], in0=S.GT[g2], scalar=S.P2[:, tt:tt + 1], in1=S.GT[g1],
                                                op0=ALU.mult, op1=ALU.add)
            self.add("dve", f, reads=[("gt", g1), ("gt", g2), ("p12",)], writes=[("gt", g1)])
            for hf in range(2):
                b = self.bank()

                def f(eng, g1=g1, hf=hf, b=b):
                    last = None
                    for q in range(4):
                        kc = hf * 4 + q
                        last = eng.transpose(out=S.PS[b][:, q * 128:(q + 1) * 128], in_=S.GT[g1][:, kc * 128:(kc + 1) * 128],
                                             identity=S.IDF[:, :])
                    return last
                self.add("pe", f, reads=[("gt", g1), ("idf",)], writes=[("ps", b)])

                def f(eng, hf=hf, b=b, t0=t0):
                    return eng.tensor_tensor(out=S.X[:, hf * 4:(hf + 1) * 4, t0:t0 + 128],
                                             in0=S.X[:, hf * 4:(hf + 1) * 4, t0:t0 + 128],
                                             in1=S.PS[b][:, 0:512].rearrange("p (q t) -> p q t", q=4), op=ALU.add)
                xr = []
                for kc in range(hf * 4, hf * 4 + 4):
                    xr += U("X", kc, t0, t0 + 128)
                self.add("dve", f, reads=[("ps", b)] + xr, writes=xr)
            if tt % 4 == 3:
                self.st_final(True, only=tt // 4, finish=(tt == NTT - 1))

    def st_final(self, norm, only=None, finish=True):
        S = self
        mains = [(HALO + i * 512, 512) for i in range(4)]
        ov = self.outT.rearrange("(c p) t -> p c t", p=128)
        for bi, (c0, n) in enumerate(mains):
            if only is not None and bi != only:
                continue
            if norm:
                bk = self.bank()
                for c in range(NCH):
                    t = self.tb()

                    def f(eng, c=c, t=t, c0=c0, n=n):
                        return eng.activation(out=S.TB[t][:, 0:n], in_=S.X[:, c, c0:c0 + n], func=AF.Square)
                    self.add("act", f, reads=U("X", c, c0, c0 + n), writes=[("tb", t)])

                    def f(eng, c=c, t=t, n=n, bk=bk):
                        return eng.matmul(S.PS[bk][:, 0:n], lhsT=S.ONESB[:, :], rhs=S.TB[t][:, 0:n],
                                          start=(c == 0), stop=(c == NCH - 1))
                    self.add("pe", f, reads=[("tb", t), ("ones",)], writes=[("ps", bk)])
                t1 = self.tf()

                def f(eng, t1=t1, n=n, bk=bk):
                    return eng.activation(out=S.TF[t1][:, 0:n], in_=S.PS[bk][:, 0:n], func=AF.Sqrt,
                                          scale=1.0 / D, bias=S.EPSR[:, 0:1])
                self.add("act", f, reads=[("ps", bk), ("eps",)], writes=[("tf", t1)])

                def f(eng, t1=t1, n=n):
                    return eng.reciprocal(out=S.RSTD[:, 0:n], in_=S.TF[t1][:, 0:n])
                self.add("dve", f, reads=[("tf", t1)], writes=[("rstd",)])
                for c in range(NCH):
                    def f(eng, c=c, c0=c0, n=n):
                        return eng.scalar_tensor_tensor(out=S.X[:, c, c0:c0 + n], in0=S.X[:, c, c0:c0 + n],
                                                        scalar=S.PRM[:, P_FIN + c:P_FIN + c + 1],
                                                        in1=S.RSTD[:, 0:n], op0=ALU.mult, op1=ALU.mult)
                    self.add("dve", f, reads=U("X", c, c0, c0 + n) + [("rstd",), ("prm",)],
                             writes=U("X", c, c0, c0 + n))
            rd = []
            for c in range(NCH):
                rd += U("X", c, c0, c0 + n)

            def f(eng, sem, c0=c0, n=n):
                return eng.dma_start(out=ov[:, :, c0 - HALO:c0 - HALO + n], in_=S.X[:, :, c0:c0 + n]).then_inc(sem, 16)
            self.add("sp", f, reads=rd, writes=[("out", bi)], dma=("out", bi))
        if not finish:
            return

        def f(eng):
            return None
        self.add("sp", f, reads=[("out", i) for i in range(4)], writes=[("done",)])

    def program(self):
        S = self
        mains = [(HALO + i * 512, 512) for i in range(4)]
        self.st_init()
        order = ["mix0", "l0", "mix1", "full"]
        lim = order.index(self.stop)
        self.ring_n = NS - 1
        self.mg_live = True
        self.st_mixer_half(0, 1, 32, 64)
        self.st_mixer_half(0, 0, 0, 32)
        self.mg_live = False
        if lim >= 1:
            if not self.dry:
                self.S.barrier()
            fb = [(32, 32)] + mains
            self.ring_n = NS
            self.st_norm(fb, 0, S.H2, "H2", P_NFG)
            self.st_ffn(0, fb, [(self.dense_w1[0], self.dense_w3[0], self.dense_w2[0])])
        if lim >= 2:
            if not self.dry:
                self.S.barrier()
            self.ring_n = NS - 1
            self.mg_live = True
            self.st_mixer_half(1, 1, 32, 64)
            self.st_mixer_half(1, 0, 32, 64)
            self.mg_live = False
        if lim >= 3:
            if not self.dry:
                self.S.barrier()
            self.ring_n = NS
            self.st_norm(mains, 0, S.H2, "H2", P_LAYER + P_NFG, router=True)
            self.st_route()
            self.st_scatter()
            if not self.dry:
                self.S.barrier()
            for e in range(NE):
                self.st_pass(e, 0, e % 2, prefetch=(e > 0, e + 1 if e + 1 < NE else None))
            yi = 0
            for e in range(NE):
                for r in range(1, NRND):
                    self.cur_region = (e, r)
                    self.st_pass(e, r, yi)
                    yi ^= 1
                    self.cur_region = None
            if not self.dry:
                self.S.barrier()
            self.st_combine()
        else:
            self.st_final(norm=False)

    def build(self):
        nc = self.nc
        self.declare()
        self.EPSR = self.es.enter_context(nc.sbuf_tensor("epsr", [128, 1], F32))
        self.EPSL = self.es.enter_context(nc.sbuf_tensor("epsl", [128, 1], F32))
        self.dry = True
        self.slot_plan = []
        self.rr = 0
        self.ring_n = NS
        self.reset_rot()
        self.program()
        last = {}
        self.prev_same = []
        for j, sl in enumerate(self.slot_plan):
            self.prev_same.append(last.get(sl))
            last[sl] = j
        self.dry = False
        self.reset_rot()
        S = self

        def f(eng):
            eng.memset(S.EPSR[:, :], RMS_EPS)
            return eng.memset(S.EPSL[:, :], LN_EPS)
        self.add("dve", f, writes=[("eps",)])
        self.program()
        assert self.wk == len(self.plan)
        self.emit()
        return nc

    def emit(self):
        nc = self.nc
        ops = self.S.ops
        es = self.es
        eng_sem = {}
        for e in Sched.COMPUTE:
            eng_sem[e] = es.enter_context(nc.semaphore(f"s_{e}"))
        dma_sem = {}
        for op in ops:
            if op.dma is not None and op.dma not in dma_sem:
                dma_sem[op.dma] = es.enter_context(nc.semaphore("d_" + "_".join(str(x) for x in op.dma)))
        if getattr(self, "cnti_op", None) is not None:
            ops[self.cnti_op].signals = True
        cnt = {e: 0 for e in Sched.COMPUTE}
        for op in ops:
            if op.dma is not None:
                op.sem = dma_sem[op.dma]
            elif op.eng in cnt:
                if op.signals:
                    cnt[op.eng] += 1
                    op.val = cnt[op.eng]
                op.sem = eng_sem[op.eng]
            else:
                assert not op.signals, "queue-engine non-dma op cannot signal"
        by = {e: [] for e in ("pe", "act", "dve", "pool", "sp")}
        for op in ops:
            by[op.eng].append(op)
        block = es.enter_context(nc.Block())

        cnti_op = getattr(self, "cnti_op", None)
        S = self

        def emit_op(eng, op, known):
            for d in op.deps:
                Dp = ops[d]
                key = id(Dp.sem)
                if known.get(key, 0) >= Dp.val:
                    continue
                eng.wait_ge(Dp.sem, Dp.val)
                known[key] = Dp.val
            if op.dma is not None:
                op.fn(eng, op.sem)
            else:
                inst = op.fn(eng)
                if op.signals:
                    inst.then_inc(op.sem, 1)

        def emit_comp(eng, ename, grp):
            nsig = sum(1 for op in grp if op.dma is None and op.signals)
            if nsig:
                eng.drain().then_inc(eng_sem[ename], nsig)
            dk = {}
            for op in grp:
                if op.dma is not None:
                    dk.setdefault(op.dma, []).append(op)
            for kd, lst2 in dk.items():
                before = lst2[0].val - 16 * lst2[0].ndma
                if before > 0:
                    eng.wait_ge(dma_sem[kd], before)
                eng.sem_inc(dma_sem[kd], 16 * sum(o.ndma for o in lst2))

        def emit_region(eng, ename, grp, known, rc):
            e, r = grp[0].region
            eng.reg_load(rc, S.CNTI[0:1, e:e + 1])
            with eng.If_lt(rc, r * BLK + 1):
                emit_comp(eng, ename, grp)
            with eng.Else():
                k2 = dict(known)
                for op in grp:
                    emit_op(eng, op, k2)

        def emit_run(eng, ename, run_ops, known, rc):
            Dp = ops[cnti_op]
            key = id(Dp.sem)
            if known.get(key, 0) < Dp.val:
                eng.wait_ge(Dp.sem, Dp.val)
                known[key] = Dp.val
            eng.reg_load(rc, S.CMAXI[0:1, 0:1])
            with eng.If_lt(rc, BLK + 1):
                emit_comp(eng, ename, run_ops)
            with eng.Else():
                i = 0
                while i < len(run_ops):
                    j = i
                    while j < len(run_ops) and run_ops[j].region == run_ops[i].region:
                        j += 1
                    emit_region(eng, ename, run_ops[i:j], known, rc)
                    i = j

        def run(eng, lst, ename):
            known = {}
            with eng.register("rc_" + ename) as rc:
                i = 0
                while i < len(lst):
                    op = lst[i]
                    if op.region is None:
                        emit_op(eng, op, known)
                        i += 1
                        continue
                    j = i
                    while j < len(lst) and lst[j].region is not None:
                        j += 1
                    emit_run(eng, ename, lst[i:j], known, rc)
                    i = j

        @block.tensor
        def _(eng):
            run(eng, by["pe"], "pe")

        @block.scalar
        def _(eng):
            run(eng, by["act"], "act")

        @block.vector
        def _(eng):
            run(eng, by["dve"], "dve")

        @block.gpsimd
        def _(eng):
            with eng.register("rb_rows") as rb:
                eng.reg_mov(rb, NE * CAP - 1)
                S.rb_pool = rb
                run(eng, by["pool"], "pool")

        @block.sync
        def _(eng):
            run(eng, by["sp"], "sp")
        es.close()


def pack_params(inp, core):
    P = np.zeros((128, NPRM), np.float32)

    def vec(v):
        return np.ascontiguousarray(v.reshape(NCH, 128).T)
    for l in range(2):
        b = l * P_LAYER
        P[:, b + P_NMG:b + P_NMG + 8] = vec(inp["norm_mix_g"][l])
        P[:, b + P_SCW:b + P_SCW + 24] = inp["sc_conv_w"][l].reshape(3, NCH, 128).transpose(2, 1, 0).reshape(128, 24)
        P[:, b + P_CFW:b + P_CFW + 248] = inp["cf_conv_w"][l].reshape(31, NCH, 128).transpose(2, 1, 0).reshape(128, 248)
        P[:, b + P_CFB:b + P_CFB + 8] = vec(inp["cf_conv_b"][l])
        P[:, b + P_LNG:b + P_LNG + 8] = vec(inp["cf_ln_g"][l])
        P[:, b + P_LNB:b + P_LNB + 8] = vec(inp["cf_ln_b"][l])
        P[:, b + P_PSC:b + P_PSC + 8] = vec(inp["pool_scale"][l])
        P[:, b + P_NFG:b + P_NFG + 8] = vec(inp["norm_ffn_g"][l])
    P[:, P_FIN:P_FIN + 8] = vec(inp["norm_final_g"])
    P[:, P_ROUT:P_ROUT + 64] = inp["moe_router"][0].reshape(NCH, 128, NE).transpose(1, 0, 2).reshape(128, 64)
    first = (core % 4 == 0)
    P[:, P_MASK:P_MASK + 64] = 0.0 if first else 1.0
    for g, w in enumerate(WINS):
        for i in range(16):
            cntv = min(i + 1, w) if first else w
            P[:, P_INVC + g * 16 + i] = 1.0 / cntv
    P[:, P_ECAP:P_ECAP + NE] = (np.arange(NE, dtype=np.float32) * CAP)[None, :]
    P[:, P_UT:P_UT + 128] = np.triu(np.ones((128, 128), np.float32), 1)
    return P


_CACHE = {}


def run(inputs, stop="full"):
    inp = {k: np.asarray(v, dtype=np.float32) for k, v in inputs.items()}
    if stop not in _CACHE:
        _CACHE[stop] = Builder(stop).build()
    nc = _CACHE[stop]
    x = inp["x"]
    shared = {k: np.ascontiguousarray(inp[k]) for k in
              ("w_in", "w_sc_out", "w_cf_out", "w_pool_out", "w_o", "pool_w", "dense_w1", "dense_w3", "dense_w2",
               "moe_w1", "moe_w3", "moe_w2")}
    in_maps = []
    for core in range(8):
        b, q = divmod(core, 4)
        t0 = q * TOK
        xs = np.zeros((TT, D), np.float32)
        if q > 0:
            xs[:HALO] = x[b, t0 - HALO:t0]
        xs[HALO:] = x[b, t0:t0 + TOK]
        m = dict(shared)
        m["xT"] = np.ascontiguousarray(xs.T)
        m["prm"] = pack_params(inp, core)
        in_maps.append(m)
    res = run_bass_kernel_spmd(nc, in_maps, core_ids=list(range(8)))
    out = np.zeros((2, SEQ, D), np.float32)
    for core in range(8):
        b, q = divmod(core, 4)
        out[b, q * TOK:(q + 1) * TOK] = res.results[core]["outT"].T
    return out


def kernel(**inputs):
    return run(inputs, "full")
```

```python
import numpy as np
import concourse.bass as bass
import concourse.mybir as mybir
from concourse.bass_utils import run_bass_kernel_spmd
from contextlib import ExitStack

F32 = mybir.dt.float32
BF16 = mybir.dt.bfloat16
I32 = mybir.dt.int32
AF = mybir.ActivationFunctionType
ALU = mybir.AluOpType
AX = mybir.AxisListType

D = 1024
NCH = 8
SEQ = 8192
TOK = 2048
HALO = 64
TT = TOK + HALO
TH = 1088
DFF = 3584
NFG = 7
NE = 8
NS = 5
NTF = 5
NTB = 2
NSLT = 5
BLK = 576
STL = [(sl, min(128, BLK - sl * 128)) for sl in range(NSLT)]
assert (NSLT - 1) * 128 < BLK <= NSLT * 128 and BLK % 32 == 0
NRND = (TOK + BLK - 1) // BLK
CAP = NRND * BLK
CB = [(c, min(512, BLK - c)) for c in range(0, BLK, 512)]
NTT = TOK // 128
WINS = (2, 4, 8, 16)
RMS_EPS = 1e-6
LN_EPS = 1e-5

C_SCB, C_SCC, C_SCX, C_CFV, C_CFG, C_PU, C_GA, C_GB, C_GC = [i * 1024 for i in range(9)]

P_LAYER = 320
P_NMG, P_SCW, P_CFW, P_CFB, P_LNG, P_LNB, P_PSC, P_NFG = 0, 8, 32, 280, 288, 296, 304, 312
P_FIN = 2 * P_LAYER
P_ROUT = P_FIN + 8
P_MASK = P_ROUT + 64
P_INVC = P_MASK + 64
P_ECAP = P_INVC + 64
P_UT = P_ECAP + 8
NPRM = P_UT + 128


class Op:
    __slots__ = ("idx", "eng", "fn", "deps", "signals", "val", "sem", "dma", "writes", "ndma", "region")

    def __init__(self, idx, eng, fn):
        self.idx = idx
        self.eng = eng
        self.fn = fn
        self.deps = []
        self.signals = False
        self.val = 0
        self.sem = None
        self.dma = None
        self.writes = ()
        self.ndma = 0
        self.region = None


class Sched:
    COMPUTE = ("pe", "act", "dve", "pool")

    def __init__(self):
        self.ops = []
        self.last_w = {}
        self.readers = {}
        self.dma_count = {}
        self.dma_last = {}
        self.bar = None
        self.bar_done = set()
        self.last_on = {}

    def barrier(self):
        self.bar = [self.last_on[e] for e in self.COMPUTE if e in self.last_on]
        self.bar_done = set()

    def add(self, eng, fn, reads=(), writes=(), dma=None, ndma=1, region=None):
        idx = len(self.ops)
        op = Op(idx, eng, fn)
        op.region = region
        deps = {}

        def need(d, raw):
            Dp = self.ops[d]
            if Dp.dma is not None:
                key = ("dma", Dp.dma)
            else:
                if Dp.eng == eng and eng == "pe":
                    return
                key = ("eng", Dp.eng)
            if key not in deps or deps[key] < d:
                deps[key] = d

        for r in reads:
            d = self.last_w.get(r)
            if d is not None:
                need(d, True)
        for w in writes:
            d = self.last_w.get(w)
            if d is not None:
                need(d, False)
            rd = self.readers.get(w)
            if rd:
                for d in rd.values():
                    need(d, False)
        if dma is not None:
            d = self.dma_last.get(dma)
            if d is not None:
                need(d, False)
        if self.bar is not None and eng in self.COMPUTE and eng not in self.bar_done:
            self.bar_done.add(eng)
            for d in self.bar:
                if self.ops[d].eng != eng:
                    need(d, False)
        for d in deps.values():
            self.ops[d].signals = True
        op.deps = sorted(deps.values())
        op.writes = frozenset(writes)
        if dma is not None:
            op.dma = dma
            op.ndma = ndma
            self.dma_count[dma] = self.dma_count.get(dma, 0) + ndma
            op.val = 16 * self.dma_count[dma]
            self.dma_last[dma] = idx
        for r in reads:
            self.readers.setdefault(r, {})[eng if dma is None else ("dma", dma)] = idx
        for w in writes:
            self.last_w[w] = idx
            self.readers[w] = {}
        self.ops.append(op)
        if dma is None:
            self.last_on[eng] = idx
        return idx


def U(name, ch, c0, c1):
    return [(name, ch, u) for u in range(c0 // 32, (c1 + 31) // 32)]


class Builder:
    def __init__(self, stop="full", debug=False):
        self.stop = stop
        self.debug = debug
        self.nc = bass.Bass("TRN2", target_bir_lowering=False)
        self.S = Sched()
        self.dry = False
        self.plan = []
        self.es = ExitStack()

    def declare(self):
        nc = self.nc

        def din(name, shape):
            return nc.dram_tensor(name, list(shape), F32, kind="ExternalInput").ap()

        self.xT = din("xT", [D, TT])
        self.prm_d = din("prm", [128, NPRM])
        self.w_in = din("w_in", [2, D, 9216])
        self.w_sc_out = din("w_sc_out", [2, D, D])
        self.w_cf_out = din("w_cf_out", [2, D, D])
        self.w_pool_out = din("w_pool_out", [2, D, D])
        self.w_o = din("w_o", [2, D, D])
        self.pool_w = din("pool_w", [2, 4, 256, 256])
        self.dense_w1 = din("dense_w1", [1, D, DFF])
        self.dense_w3 = din("dense_w3", [1, D, DFF])
        self.dense_w2 = din("dense_w2", [1, DFF, D])
        self.moe_w1 = din("moe_w1", [1, NE, D, DFF])
        self.moe_w3 = din("moe_w3", [1, NE, D, DFF])
        self.moe_w2 = din("moe_w2", [1, NE, DFF, D])
        self.outT = nc.dram_tensor("outT", [D, TOK], F32, kind="ExternalOutput").ap()

        def sb(name, shape, dt):
            return self.es.enter_context(nc.sbuf_tensor(name, list(shape), dt))

        self.X = sb("X", [128, NCH, TT], F32)
        self.WORK = sb("WORK", [128, 13056], F32)
        hw = 4352
        self.H = self.WORK[:, 0:hw].bitcast(BF16).rearrange("p (c t) -> p c t", c=NCH)
        self.T1 = self.WORK[:, hw:2 * hw].bitcast(BF16).rearrange("p (c t) -> p c t", c=NCH)
        self.T2 = self.WORK[:, 2 * hw:3 * hw].bitcast(BF16).rearrange("p (c t) -> p c t", c=NCH)
        self.H2 = self.WORK[:, 0:8448].bitcast(BF16).rearrange("p (c t) -> p c t", c=NCH)
        self.GS = self.WORK[:, 8448:8448 + 4224].bitcast(BF16).rearrange("p (c t) -> p c t", c=4)
        self.MGB = sb("mgb", [128, NCH * TH], BF16)
        self.MG = self.MGB[:, :].rearrange("p (c t) -> p c t", c=NCH)
        self.WS = [sb(f"ws{i}", [128, 4096], BF16)[:, :] for i in range(NS - 1)] + [self.MGB[:, 0:4096]]
        self.TF = [sb(f"tf{i}", [128, 512], F32) for i in range(NTF)]
        self.TB = [sb(f"tb{i}", [128, 512], BF16) for i in range(NTB)]
        self.MEAN = sb("mean", [128, 512], F32)
        self.RSTD = sb("rstd", [128, 512], F32)
        self.DG = sb("diag", [128, 3968], F32)
        dgb = self.DG[:, 0:3968].bitcast(BF16)
        self.DIAG = [dgb[:, 0:3968].rearrange("p (k m) -> p k m", k=31),
                     dgb[:, 3968:7936].rearrange("p (k m) -> p k m", k=31)]
        dga = self.DG[:, :].bitcast(BF16)
        self.HT = [dga[:, i * 1024:(i + 1) * 1024] for i in range(4)]
        self.HGS = dga[:, 0:NSLT * 1024].rearrange("p (s d) -> p s d", s=NSLT)
        self.GSR = dga[:, NSLT * 1024:NSLT * 1024 + 4 * BLK].rearrange("p (j t) -> p j t", j=4)
        hgw = NCH * BLK // 2
        self.HG = self.WORK[:, 0:hgw].bitcast(BF16).rearrange("p (c t) -> p c t", c=NCH)
        yw = NSLT * 1024
        self.YACC = [self.WORK[:, hgw + i * yw:hgw + (i + 1) * yw].rearrange("p (s d) -> p s d", s=NSLT)
                     for i in range(2)]
        assert hgw + 2 * yw <= 13056 and NSLT * 1024 + 4 * BLK <= 7936
        self.GT = [self.WORK[:, i * 1024:(i + 1) * 1024] for i in range(8)]
        if self.debug:
            self.HD = nc.dram_tensor("hd_scr", [NE * CAP, D], BF16, kind="ExternalOutput").ap()
            self.YD = nc.dram_tensor("yd_scr", [NE * CAP, D], F32, kind="ExternalOutput").ap()
            self.DBG = nc.dram_tensor("dbg_i", [128, 40], I32, kind="ExternalOutput").ap()
        else:
            self.HD = nc.dram_tensor("hd_scr", [NE * CAP, D], BF16).ap()
            self.YD = nc.dram_tensor("yd_scr", [NE * CAP, D], F32).ap()
        self.RT = sb("rt", [128, 4 * NTT * NE + NTT * NE // 2], F32)
        self.ZT = self.RT[:, 0:512].bitcast(BF16)
        self.UTB = sb("utb", [128, 128], BF16)
        nte = NTT * NE

        def r3(lo):
            return self.RT[:, lo:lo + nte].rearrange("p (t e) -> p t e", e=NE)
        self.CNT, self.OFF, self.POS, self.TMP = r3(0), r3(nte), r3(2 * nte), r3(3 * nte)
        self.MSKB = self.RT[:, 4 * nte:4 * nte + nte // 2].bitcast(BF16).rearrange("p (t e) -> p t e", e=NE)
        self.D1 = sb("d1", [128, NTT], F32)
        self.D2 = sb("d2", [128, NTT], F32)
        self.DI1 = sb("di1", [128, NTT], I32)
        self.DI2 = sb("di2", [128, NTT], I32)
        self.CNTI = sb("cnti", [128, NE], I32)
        self.CMAXF = sb("cmaxf", [128, 1], F32)
        self.CMAXI = sb("cmaxi", [128, 1], I32)
        self.IDF = sb("idf", [128, 128], F32)
        self.IDB = sb("idb", [128, 128], BF16)
        self.ONESB = sb("onesb", [128, 128], BF16)
        self.PRM = sb("prmsb", [128, NPRM], F32)
        self.LG = sb("lg", [128, 16, NE], F32)
        self.L2 = sb("l2", [128, 16, NE], F32)
        self.EQ1 = sb("eq1", [128, 16, NE], F32)
        self.EQ2 = sb("eq2", [128, 16, NE], F32)
        self.M1 = sb("m1", [128, 16], F32)
        self.M2 = sb("m2", [128, 16], F32)
        self.P1 = sb("p1", [128, 16], F32)
        self.P2 = sb("p2", [128, 16], F32)
        self.PS = [self.es.enter_context(nc.psum_tensor(f"ps{i}", [128, 512], F32)) for i in range(8)]
        self.sems = {}

    def reset_rot(self):
        self.ib = 0
        self.iht = 0
        self.igt = 0
        self.mg_live = False
        self.cur_region = None
        self.itf = 0
        self.itb = 0
        self.wk = 0
        self.w_issued = 0
        self.w_rel = []
        self.idg = 0

    def bank(self):
        i = self.ib % 8
        self.ib += 1
        return i

    def tf(self):
        i = self.itf % NTF
        self.itf += 1
        return i

    def tb(self):
        i = self.itb % NTB
        self.itb += 1
        return i

    def ht(self):
        i = self.iht % 4
        self.iht += 1
        return i

    def gt(self):
        i = self.igt % 8
        self.igt += 1
        return i

    def add(self, eng, fn, reads=(), writes=(), dma=None, ndma=1):
        if self.dry:
            return
        if self.cur_region is not None:
            reads = list(reads) + [("cnti",)]
        return self.S.add(eng, fn, reads, writes, dma, ndma, region=self.cur_region)

    def wget(self, issue_fn):
        k = self.wk
        self.wk += 1
        if self.dry:
            slot = self.rr % self.ring_n
            self.rr = slot + 1
            self.plan.append((issue_fn, self.cur_region))
            self.slot_plan.append(slot)
            return k, self.WS[slot]
        self.pump()
        assert self.w_issued > k, "weight ring deadlock: too many slots held"
        return k, self.WS[self.slot_plan[k]]

    def wrel(self, k):
        if self.dry:
            return
        self.w_rel.append(k)
        self.pump()

    def pump(self):
        while self.w_issued < len(self.plan):
            j = self.w_issued
            if self.prev_same[j] is not None and self.prev_same[j] not in self.w_rel:
                break
            slot = self.slot_plan[j]
            if slot == NS - 1 and self.mg_live:
                break
            fn, reg = self.plan[j]
            ws = self.WS[slot]

            def f(eng, sem, fn=fn, ws=ws):
                return fn(eng, ws, sem)
            self.S.add("pool", f, reads=([("cnti",)] if reg is not None else ()), writes=[("w", slot)],
                       dma=("w", slot), ndma=fn.ndma, region=reg)
            self.w_issued += 1

    def ld_cols(self, src2d, c0, ncols=512):
        def fn(eng, ws, sem):
            dst = ws[:, 0:8 * ncols].rearrange("p (k c) -> p k c", k=8)
            src = src2d[:, c0:c0 + ncols].rearrange("(k p) c -> p k c", p=128)
            return eng.dma_start(out=dst, in_=src).then_inc(sem, 16)
        fn.ndma = 1
        return fn

    def ld_rows(self, src2d, r0):
        def fn(eng, ws, sem):
            dst = ws[:, :].rearrange("p (f d) -> p f d", f=4)
            src = src2d[r0:r0 + 512, :].rearrange("(f p) d -> p f d", p=128)
            return eng.dma_start(out=dst, in_=src).then_inc(sem, 16)
        fn.ndma = 1
        return fn

    def ld_poolw(self, src3d):
        def fn(eng, ws, sem):
            dst = ws[:, 0:2048].rearrange("p (k c) -> p k c", k=8)
            src = src3d.rearrange("g (k p) e -> p (g k) e", p=128)
            return eng.dma_start(out=dst, in_=src).then_inc(sem, 16)
        fn.ndma = 1
        return fn

    def mm_group(self, bank, n, pairs, reads):
        ps = self.PS[bank]
        nc = self.nc

        def fn(eng, pairs=pairs, ps=ps, n=n):
            last = None
            m = pairs[0][0].shape[-1]
            for i, (l, r) in enumerate(pairs):
                last = eng.matmul(ps[0:m, 0:n], lhsT=l, rhs=r, start=(i == 0), stop=(i == len(pairs) - 1))
            return last
        self.add("pe", fn, reads=reads, writes=[("ps", bank)])

    def proj(self, wsl, k, j, rhs_buf, rname, c0, n, bank, extra=()):
        wv = wsl[:, 0:4096].rearrange("p (k c) -> p k c", k=8)
        pairs = [(wv[:, kc, j * 128:(j + 1) * 128], rhs_buf[:, kc, c0:c0 + n]) for kc in range(NCH)]
        reads = [("w", self.slot_plan[k])] + list(extra)
        for kc in range(NCH):
            reads += U(rname, kc, c0, c0 + n)
        self.mm_group(bank, n, pairs, reads)

    def st_init(self):
        nc = self.nc
        S = self
        for (lo, hi) in ((1024, TT), (0, 1024)):
            for c in range(NCH):
                def f(eng, sem, c=c, lo=lo, hi=hi):
                    return eng.dma_start(out=S.X[:, c, lo:hi], in_=S.xT[c * 128:(c + 1) * 128, lo:hi]).then_inc(sem, 16)
                self.add("sp" if c % 2 == 0 else "act", f, writes=U("X", c, lo, hi), dma=("xl", c, lo))

        def f(eng, sem):
            return eng.dma_start(out=S.PRM[:, :], in_=S.prm_d[:, :]).then_inc(sem, 16)
        self.add("sp", f, writes=[("prm",)], dma=("prm",))

        def f(eng):
            eng.memset(S.ONESB[:, :], 1.0)
            return eng.memset(S.IDF[:, :], 0.0)
        self.add("pool", f, writes=[("idf0",), ("ones",)])

        def f(eng):
            return eng.affine_select(out=S.IDF[:, :], in_=S.IDF[:, :], compare_op=ALU.not_equal, fill=1.0,
                                     base=0, pattern=[[-1, 128]], channel_multiplier=1)
        self.add("pool", f, reads=[("idf0",)], writes=[("idf",)])

        def f(eng):
            return eng.tensor_copy(out=S.IDB[:, :], in_=S.IDF[:, :])
        self.add("pool", f, reads=[("idf",)], writes=[("idb",)])

        if self.stop != "full":
            return

        def f(eng):
            eng.memset(S.ZT[:, :], 0.0)
            return eng.tensor_copy(out=S.UTB[:, :], in_=S.PRM[:, P_UT:P_UT + 128])
        self.add("pool", f, reads=[("prm",)], writes=[("zt",), ("utb",)])
        assert (NE * CAP) % 128 == 0
        hdv = S.HD.rearrange("(q p) d -> q p d", p=128)
        nq = NE * CAP // 128
        per = 20
        for q0 in range(0, nq, per):
            def f(eng, sem, q0=q0):
                last = None
                for q in range(q0, min(nq, q0 + per)):
                    last = eng.dma_start(out=hdv[q], in_=S.ZT[:, :]).then_inc(sem, 16)
                return last
            self.add("sp", f, reads=[("zt",)], writes=[("hdz",)], dma=("zi",), ndma=min(nq, q0 + per) - q0)

    def st_norm(self, blocks, xoff, hbuf, hname, gcol, mask=False, router=False):
        S = self
        for (c0, n) in blocks:
            g0 = xoff + c0
            bk = self.bank()
            for c in range(NCH):
                t = self.tb()

                def f(eng, c=c, t=t, g0=g0, n=n):
                    return eng.activation(out=S.TB[t][:, 0:n], in_=S.X[:, c, g0:g0 + n], func=AF.Square)
                self.add("act", f, reads=U("X", c, g0, g0 + n), writes=[("tb", t)])

                def f(eng, c=c, t=t, n=n, bk=bk):
                    return eng.matmul(S.PS[bk][:, 0:n], lhsT=S.ONESB[:, :], rhs=S.TB[t][:, 0:n],
                                      start=(c == 0), stop=(c == NCH - 1))
                self.add("pe", f, reads=[("tb", t), ("ones",)], writes=[("ps", bk)])
            t1 = self.tf()

            def f(eng, t1=t1, n=n, bk=bk):
                return eng.activation(out=S.TF[t1][:, 0:n], in_=S.PS[bk][:, 0:n], func=AF.Sqrt,
                                      scale=1.0 / D, bias=S.EPSR[:, 0:1])
            self.add("act", f, reads=[("ps", bk), ("eps",)], writes=[("tf", t1)])

            def f(eng, t1=t1, n=n):
                return eng.reciprocal(out=S.RSTD[:, 0:n], in_=S.TF[t1][:, 0:n])
            self.add("dve", f, reads=[("tf", t1)], writes=[("rstd",)])
            if mask and c0 < HALO:
                def f(eng, c0=c0, n=n):
                    return eng.tensor_tensor(out=S.RSTD[:, 0:n], in0=S.RSTD[:, 0:n],
                                             in1=S.PRM[:, P_MASK + c0:P_MASK + c0 + n], op=ALU.mult)
                self.add("dve", f, reads=[("rstd",), ("prm",)], writes=[("rstd",)])
            if router:
                rb = self.bank()
            for c in range(NCH):
                if not router:
                    def f(eng, c=c, c0=c0, g0=g0, n=n):
                        return eng.scalar_tensor_tensor(out=hbuf[:, c, c0:c0 + n], in0=S.X[:, c, g0:g0 + n],
                                                        scalar=S.PRM[:, gcol + c:gcol + c + 1],
                                                        in1=S.RSTD[:, 0:n], op0=ALU.mult, op1=ALU.mult)
                    self.add("dve", f, reads=U("X", c, g0, g0 + n) + [("rstd",), ("prm",)],
                             writes=U(hname, c, c0, c0 + n))
                else:
                    t2 = self.tf()

                    def f(eng, c=c, g0=g0, n=n, t2=t2):
                        return eng.scalar_tensor_tensor(out=S.TF[t2][:, 0:n], in0=S.X[:, c, g0:g0 + n],
                                                        scalar=S.PRM[:, gcol + c:gcol + c + 1],
                                                        in1=S.RSTD[:, 0:n], op0=ALU.mult, op1=ALU.mult)
                    self.add("dve", f, reads=U("X", c, g0, g0 + n) + [("rstd",), ("prm",)], writes=[("tf", t2)])

                    def f(eng, c=c, c0=c0, n=n, t2=t2):
                        return eng.activation(out=hbuf[:, c, c0:c0 + n], in_=S.TF[t2][:, 0:n], func=AF.Copy)
                    self.add("act", f, reads=[("tf", t2)], writes=U(hname, c, c0, c0 + n))

                    def f(eng, c=c, n=n, t2=t2, rb=rb):
                        return eng.matmul(S.PS[rb][0:NE, 0:n], lhsT=S.PRM[:, P_ROUT + c * NE:P_ROUT + (c + 1) * NE],
                                          rhs=S.TF[t2][:, 0:n], start=(c == 0), stop=(c == NCH - 1))
                    self.add("pe", f, reads=[("tf", t2), ("prm",)], writes=[("ps", rb)])
            if router:
                t3 = self.tf()

                def f(eng, n=n, t3=t3, rb=rb):
                    return eng.activation(out=S.TF[t3][0:NE, 0:n], in_=S.PS[rb][0:NE, 0:n], func=AF.Copy)
                self.add("act", f, reads=[("ps", rb)], writes=[("tf", t3)])
                tb_ = self.bank()
                nt = n // 128

                def f(eng, t3=t3, tb_=tb_, nt=nt):
                    last = None
                    for j in range(nt):
                        last = eng.transpose(out=S.PS[tb_][:, j * NE:(j + 1) * NE],
                                             in_=S.TF[t3][0:NE, j * 128:(j + 1) * 128],
                                             identity=S.IDF[0:NE, 0:NE])
                    return last
                self.add("pe", f, reads=[("tf", t3), ("idf",)], writes=[("ps", tb_)])
                j0 = (g0 - HALO) // 128

                def f(eng, tb_=tb_, nt=nt, j0=j0):
                    return eng.tensor_copy(out=S.LG[:, j0:j0 + nt, :],
                                           in_=S.PS[tb_][:, 0:nt * NE].rearrange("p (j e) -> p j e", e=NE))
                self.add("dve", f, reads=[("ps", tb_)], writes=[("lg", j0)])

    def blocks_of(self, first):
        b = []
        if first < HALO:
            b.append((first, HALO - first))
        b += [(HALO, 512), (HALO + 512, 512)]
        return b

    def st_outproj(self, l, w_out, gbase, post, xoff, first, last):
        S = self
        mgres = [("w", NS - 1)]
        pending = None
        for grp in range(2):
            kw, wsl = self.wget(self.ld_cols(w_out[l], grp * 512))
            kg, gsl = self.wget(self.ld_cols(self.w_in[l], gbase + grp * 512))
            for (c0, n) in post:
                for j in range(4):
                    d = grp * 4 + j
                    by = self.bank()
                    self.proj(wsl, kw, j, S.T1, "T1", c0, n, by)
                    bg = self.bank()
                    self.proj(gsl, kg, j, S.H, "H", c0, n, bg)
                    t = self.tf()

                    def f(eng, t=t, n=n, bg=bg):
                        return eng.activation(out=S.TF[t][:, 0:n], in_=S.PS[bg][:, 0:n], func=AF.Sigmoid)
                    self.add("act", f, reads=[("ps", bg)], writes=[("tf", t)])
                    if first:
                        def f(eng, t=t, n=n, by=by, d=d, c0=c0):
                            return eng.tensor_tensor(out=S.MG[:, d, c0:c0 + n], in0=S.PS[by][:, 0:n],
                                                     in1=S.TF[t][:, 0:n], op=ALU.mult)
                        self.add("dve", f, reads=[("ps", by), ("tf", t)], writes=U("MG", d, c0, c0 + n) + mgres)
                    else:
                        def f(eng, t=t, n=n, by=by):
                            return eng.tensor_tensor(out=S.TF[t][:, 0:n], in0=S.PS[by][:, 0:n],
                                                     in1=S.TF[t][:, 0:n], op=ALU.mult)
                        self.add("dve", f, reads=[("ps", by), ("tf", t)], writes=[("tf", t)])
                        if pending is not None:
                            pending()

                        def pend(t=t, n=n, d=d, c0=c0):
                            def f(eng):
                                return eng.tensor_tensor(out=S.MG[:, d, c0:c0 + n], in0=S.MG[:, d, c0:c0 + n],
                                                         in1=S.TF[t][:, 0:n], op=ALU.add)
                            self.add("dve", f, reads=[("tf", t)] + U("MG", d, c0, c0 + n),
                                     writes=U("MG", d, c0, c0 + n) + mgres)
                        pending = pend
            self.wrel(kw)
            self.wrel(kg)
        if pending is not None:
            pending()
        if not last:
            return
        for grp in range(2):
            ko, osl = self.wget(self.ld_cols(self.w_o[l], grp * 512))
            for (c0, n) in post:
                for j in range(4):
                    e = grp * 4 + j
                    b = self.bank()
                    self.proj(osl, ko, j, S.MG, "MG", c0, n, b, extra=mgres)
                    g0 = xoff + c0

                    def f(eng, e=e, g0=g0, n=n, b=b):
                        return eng.tensor_tensor(out=S.X[:, e, g0:g0 + n], in0=S.X[:, e, g0:g0 + n],
                                                 in1=S.PS[b][:, 0:n], op=ALU.add)
                    self.add("dve", f, reads=[("ps", b)] + U("X", e, g0, g0 + n), writes=U("X", e, g0, g0 + n))
            self.wrel(ko)

    def build_diag(self, col0, ntap):
        S = self
        db = self.idg % 2
        self.idg += 1

        def f(eng, db=db, col0=col0, ntap=ntap):
            return eng.tensor_tensor(out=S.DIAG[db][:, 0:ntap, :],
                                     in0=S.IDB[:, :].unsqueeze(1).to_broadcast([128, ntap, 128]),
                                     in1=S.PRM[:, col0:col0 + ntap].unsqueeze(2).to_broadcast([128, ntap, 128]),
                                     op=ALU.mult)
        self.add("dve", f, reads=[("idb",), ("prm",)], writes=[("diag", db, 0)])
        return db

    def st_mixer_half(self, l, half, ci0, po0):
        S = self
        xoff = 0 if half == 0 else 1024
        ci = self.blocks_of(ci0)
        post = self.blocks_of(po0)
        pl = l * P_LAYER
        win = self.w_in[l]
        self.st_norm(ci, xoff, S.H, "H", pl + P_NMG, mask=(half == 0))

        for grp in range(2):
            k, sl = self.wget(self.ld_cols(win, C_PU + grp * 512))
            for (c0, n) in ci:
                for j in range(4):
                    c = grp * 4 + j
                    b = self.bank()
                    self.proj(sl, k, j, S.H, "H", c0, n, b)

                    def f(eng, c=c, c0=c0, n=n, b=b):
                        return eng.activation(out=S.T2[:, c, c0:c0 + n], in_=S.PS[b][:, 0:n], func=AF.Copy)
                    self.add("act", f, reads=[("ps", b)], writes=U("T2", c, c0, c0 + n))
            self.wrel(k)
        kp, psl = self.wget(self.ld_poolw(self.pool_w[l]))
        pwv = psl[:, 0:2048].rearrange("p (k c) -> p k c", k=8)
        def pool_win(g):
            w = WINS[g]
            for cc in range(2):
                c = 2 * g + cc
                for (c0, n) in post:
                    b = self.bank()
                    pairs = [(S.IDB[:, :], S.T2[:, c, c0 - jj:c0 - jj + n]) for jj in range(w)]
                    self.mm_group(b, n, pairs, [("idb",)] + U("T2", c, c0 - w + 1, c0 + n))

                    def f(eng, c=c, c0=c0, n=n, b=b, w=w):
                        return eng.scalar_tensor_tensor(out=S.T1[:, c, c0:c0 + n], in0=S.PS[b][:, 0:n],
                                                        scalar=1.0 / w, in1=S.T2[:, c, c0:c0 + n],
                                                        op0=ALU.mult, op1=ALU.subtract)
                    self.add("dve", f, reads=[("ps", b)] + U("T2", c, c0, c0 + n), writes=U("T1", c, c0, c0 + n))
                    if half == 0 and c0 == HALO:
                        t = self.tf()

                        def f(eng, t=t, b=b, g=g):
                            return eng.tensor_tensor(out=S.TF[t][:, 0:16], in0=S.PS[b][:, 0:16],
                                                     in1=S.PRM[:, P_INVC + g * 16:P_INVC + (g + 1) * 16], op=ALU.mult)
                        self.add("dve", f, reads=[("ps", b), ("prm",)], writes=[("tf", t)])

                        def f(eng, t=t, c=c):
                            return eng.tensor_tensor(out=S.T1[:, c, HALO:HALO + 16], in0=S.TF[t][:, 0:16],
                                                     in1=S.T2[:, c, HALO:HALO + 16], op=ALU.subtract)
                        self.add("dve", f, reads=[("tf", t)] + U("T2", c, HALO, HALO + 16),
                                 writes=U("T1", c, HALO, HALO + 16))

        def pool_mix(g):
            for (c0, n) in post:
                bs = []
                for e2 in range(2):
                    b = self.bank()
                    bs.append(b)
                    pairs = [(pwv[:, g * 2 + kc, e2 * 128:(e2 + 1) * 128], S.T1[:, 2 * g + kc, c0:c0 + n])
                             for kc in range(2)]
                    rd = [("w", self.slot_plan[kp])] + U("T1", 2 * g, c0, c0 + n) + U("T1", 2 * g + 1, c0, c0 + n)
                    self.mm_group(b, n, pairs, rd)
                for e2 in range(2):
                    ch = 2 * g + e2

                    def f(eng, ch=ch, c0=c0, n=n, b=bs[e2]):
                        return eng.activation(out=S.T1[:, ch, c0:c0 + n], in_=S.PS[b][:, 0:n], func=AF.Copy,
                                              scale=S.PRM[:, pl + P_PSC + ch:pl + P_PSC + ch + 1])
                    self.add("act", f, reads=[("ps", bs[e2]), ("prm",)], writes=U("T1", ch, c0, c0 + n))

        pool_win(0)
        for g in range(4):
            if g + 1 < 4:
                pool_win(g + 1)
            pool_mix(g)
        self.wrel(kp)
        self.st_outproj(l, self.w_pool_out, C_GC, post, xoff, True, False)

        for grp in range(2):
            kc_, csl = self.wget(self.ld_cols(win, C_SCC + grp * 512))
            kx_, xsl = self.wget(self.ld_cols(win, C_SCX + grp * 512))
            for j in range(4):
                c = grp * 4 + j
                for (c0, n) in ci:
                    b1 = self.bank()
                    self.proj(csl, kc_, j, S.H, "H", c0, n, b1)
                    b2 = self.bank()
                    self.proj(xsl, kx_, j, S.H, "H", c0, n, b2)
                    t = self.tf()

                    def f(eng, t=t, n=n, b1=b1):
                        return eng.activation(out=S.TF[t][:, 0:n], in_=S.PS[b1][:, 0:n], func=AF.Copy)
                    self.add("act", f, reads=[("ps", b1)], writes=[("tf", t)])

                    def f(eng, t=t, n=n, b2=b2, c=c, c0=c0):
                        return eng.tensor_tensor(out=S.T2[:, c, c0:c0 + n], in0=S.PS[b2][:, 0:n],
                                                 in1=S.TF[t][:, 0:n], op=ALU.mult)
                    self.add("dve", f, reads=[("ps", b2), ("tf", t)], writes=U("T2", c, c0, c0 + n))
            self.wrel(kc_)
            self.wrel(kx_)
        for grp in range(2):
            kb_, bsl = self.wget(self.ld_cols(win, C_SCB + grp * 512))
            for j in range(4):
                c = grp * 4 + j
                db = self.build_diag(pl + P_SCW + c * 3, 3)
                for (c0, n) in post:
                    b1 = self.bank()
                    pairs = [(S.DIAG[db][:, kk, :], S.T2[:, c, c0 - 2 + kk:c0 - 2 + kk + n]) for kk in range(3)]
                    self.mm_group(b1, n, pairs, [("diag", db, 0)] + U("T2", c, c0 - 2, c0 + n))
                    b2 = self.bank()
                    self.proj(bsl, kb_, j, S.H, "H", c0, n, b2)
                    t = self.tf()

                    def f(eng, t=t, n=n, b2=b2):
                        return eng.activation(out=S.TF[t][:, 0:n], in_=S.PS[b2][:, 0:n], func=AF.Copy)
                    self.add("act", f, reads=[("ps", b2)], writes=[("tf", t)])

                    def f(eng, t=t, n=n, b1=b1, c=c, c0=c0):
                        return eng.tensor_tensor(out=S.T1[:, c, c0:c0 + n], in0=S.PS[b1][:, 0:n],
                                                 in1=S.TF[t][:, 0:n], op=ALU.mult)
                    self.add("dve", f, reads=[("ps", b1), ("tf", t)], writes=U("T1", c, c0, c0 + n))
            self.wrel(kb_)
        self.st_outproj(l, self.w_sc_out, C_GA, post, xoff, False, False)

        for grp in range(2):
            kg_, gsl = self.wget(self.ld_cols(win, C_CFG + grp * 512))
            kv_, vsl = self.wget(self.ld_cols(win, C_CFV + grp * 512))
            for j in range(4):
                c = grp * 4 + j
                for (c0, n) in ci:
                    b1 = self.bank()
                    self.proj(gsl, kg_, j, S.H, "H", c0, n, b1)
                    b2 = self.bank()
                    self.proj(vsl, kv_, j, S.H, "H", c0, n, b2)
                    t = self.tf()

                    def f(eng, t=t, n=n, b1=b1):
                        return eng.activation(out=S.TF[t][:, 0:n], in_=S.PS[b1][:, 0:n], func=AF.Sigmoid)
                    self.add("act", f, reads=[("ps", b1)], writes=[("tf", t)])

                    def f(eng, t=t, n=n, b2=b2, c=c, c0=c0):
                        return eng.tensor_tensor(out=S.T2[:, c, c0:c0 + n], in0=S.PS[b2][:, 0:n],
                                                 in1=S.TF[t][:, 0:n], op=ALU.mult)
                    self.add("dve", f, reads=[("ps", b2), ("tf", t)], writes=U("T2", c, c0, c0 + n))
            self.wrel(kg_)
            self.wrel(kv_)
        for c in range(NCH):
            db = self.build_diag(pl + P_CFW + c * 31, 31)
            for (c0, n) in post:
                b = self.bank()
                pairs = [(S.DIAG[db][:, kk, :], S.T2[:, c, c0 - 30 + kk:c0 - 30 + kk + n]) for kk in range(31)]
                self.mm_group(b, n, pairs, [("diag", db, 0)] + U("T2", c, c0 - 30, c0 + n))

                def f(eng, c=c, c0=c0, n=n, b=b):
                    return eng.activation(out=S.T1[:, c, c0:c0 + n], in_=S.PS[b][:, 0:n], func=AF.Identity,
                                          bias=S.PRM[:, pl + P_CFB + c:pl + P_CFB + c + 1])
                self.add("act", f, reads=[("ps", b), ("prm",)], writes=U("T1", c, c0, c0 + n))
        for (c0, n) in post:
            b1 = self.bank()
            b2 = self.bank()
            for c in range(NCH):
                t = self.tb()

                def f(eng, c=c, t=t, c0=c0, n=n):
                    return eng.activation(out=S.TB[t][:, 0:n], in_=S.T1[:, c, c0:c0 + n], func=AF.Square)
                self.add("act", f, reads=U("T1", c, c0, c0 + n), writes=[("tb", t)])

                def f(eng, c=c, t=t, c0=c0, n=n, b1=b1, b2=b2):
                    eng.matmul(S.PS[b1][:, 0:n], lhsT=S.ONESB[:, :], rhs=S.T1[:, c, c0:c0 + n],
                               start=(c == 0), stop=(c == NCH - 1))
                    return eng.matmul(S.PS[b2][:, 0:n], lhsT=S.ONESB[:, :], rhs=S.TB[t][:, 0:n],
                                      start=(c == 0), stop=(c == NCH - 1))
                self.add("pe", f, reads=[("tb", t), ("ones",)] + U("T1", c, c0, c0 + n),
                         writes=[("ps", b1), ("ps", b2)])

            def f(eng, n=n, b1=b1):
                return eng.activation(out=S.MEAN[:, 0:n], in_=S.PS[b1][:, 0:n], func=AF.Copy, scale=1.0 / D)
            self.add("act", f, reads=[("ps", b1)], writes=[("mean",)])
            ta = self.tf()

            def f(eng, n=n, ta=ta):
                return eng.tensor_tensor(out=S.TF[ta][:, 0:n], in0=S.MEAN[:, 0:n], in1=S.MEAN[:, 0:n], op=ALU.mult)
            self.add("dve", f, reads=[("mean",)], writes=[("tf", ta)])
            tv = self.tf()

            def f(eng, n=n, ta=ta, tv=tv, b2=b2):
                return eng.scalar_tensor_tensor(out=S.TF[tv][:, 0:n], in0=S.PS[b2][:, 0:n], scalar=1.0 / D,
                                                in1=S.TF[ta][:, 0:n], op0=ALU.mult, op1=ALU.subtract)
            self.add("dve", f, reads=[("ps", b2), ("tf", ta)], writes=[("tf", tv)])
            tr = self.tf()

            def f(eng, n=n, tv=tv, tr=tr):
                return eng.activation(out=S.TF[tr][:, 0:n], in_=S.TF[tv][:, 0:n], func=AF.Sqrt,
                                      bias=S.EPSL[:, 0:1])
            self.add("act", f, reads=[("tf", tv), ("eps",)], writes=[("tf", tr)])

            def f(eng, n=n, tr=tr):
                return eng.reciprocal(out=S.RSTD[:, 0:n], in_=S.TF[tr][:, 0:n])
            self.add("dve", f, reads=[("tf", tr)], writes=[("rstd",)])
            for c in range(NCH):
                t1 = self.tf()

                def f(eng, c=c, c0=c0, n=n, t1=t1):
                    return eng.tensor_tensor(out=S.TF[t1][:, 0:n], in0=S.T1[:, c, c0:c0 + n], in1=S.MEAN[:, 0:n],
                                             op=ALU.subtract)
                self.add("dve", f, reads=U("T1", c, c0, c0 + n) + [("mean",)], writes=[("tf", t1)])
                t2 = self.tf()

                def f(eng, n=n, t1=t1, t2=t2):
                    return eng.tensor_tensor(out=S.TF[t2][:, 0:n], in0=S.TF[t1][:, 0:n], in1=S.RSTD[:, 0:n],
                                             op=ALU.mult)
                self.add("dve", f, reads=[("tf", t1), ("rstd",)], writes=[("tf", t2)])

                def f(eng, c=c, c0=c0, n=n, t2=t2):
                    return eng.activation(out=S.T1[:, c, c0:c0 + n], in_=S.TF[t2][:, 0:n], func=AF.Silu,
                                          scale=S.PRM[:, pl + P_LNG + c:pl + P_LNG + c + 1],
                                          bias=S.PRM[:, pl + P_LNB + c:pl + P_LNB + c + 1])
                self.add("act", f, reads=[("tf", t2), ("prm",)], writes=U("T1", c, c0, c0 + n))
        self.st_outproj(l, self.w_cf_out, C_GB, post, xoff, False, True)

    def st_ffn(self, l, blocks, experts):
        S = self
        for (w1, w3, w2) in experts:
            for fg in range(NFG):
                k1, s1 = self.wget(self.ld_cols(w1, fg * 512))
                k3, s3 = self.wget(self.ld_cols(w3, fg * 512))
                k2, s2 = self.wget(self.ld_rows(w2, fg * 512))
                for (c0, n) in blocks:
                    for j in range(4):
                        ba = self.bank()
                        self.proj(s1, k1, j, S.H2, "H2", c0, n, ba)
                        bc = self.bank()
                        self.proj(s3, k3, j, S.H2, "H2", c0, n, bc)
                        t = self.tf()

                        def f(eng, t=t, n=n, ba=ba):
                            return eng.activation(out=S.TF[t][:, 0:n], in_=S.PS[ba][:, 0:n], func=AF.Silu)
                        self.add("act", f, reads=[("ps", ba)], writes=[("tf", t)])

                        def f(eng, t=t, n=n, bc=bc, j=j, c0=c0):
                            return eng.tensor_tensor(out=S.GS[:, j, c0:c0 + n], in0=S.PS[bc][:, 0:n],
                                                     in1=S.TF[t][:, 0:n], op=ALU.mult)
                        self.add("dve", f, reads=[("ps", bc), ("tf", t)], writes=U("GS", j, c0, c0 + n))
                self.wrel(k1)
                self.wrel(k3)
                w2v = s2[:, :].rearrange("p (f d) -> p f d", f=4)
                for (c0, n) in blocks:
                    for d in range(NCH):
                        b = self.bank()
                        pairs = [(w2v[:, j, d * 128:(d + 1) * 128], S.GS[:, j, c0:c0 + n]) for j in range(4)]
                        rd = [("w", self.slot_plan[k2])]
                        for j in range(4):
                            rd += U("GS", j, c0, c0 + n)
                        self.mm_group(b, n, pairs, rd)

                        def f(eng, d=d, c0=c0, n=n, b=b):
                            return eng.tensor_tensor(out=S.X[:, d, c0:c0 + n], in0=S.X[:, d, c0:c0 + n],
                                                     in1=S.PS[b][:, 0:n], op=ALU.add)
                        self.add("dve", f, reads=[("ps", b)] + U("X", d, c0, c0 + n), writes=U("X", d, c0, c0 + n))
                self.wrel(k2)

    def st_route(self):
        S = self
        allg = [("lg", j0) for j0 in range(0, 16, 4)]

        def bc(ap):
            return ap.unsqueeze(2).to_broadcast([128, NTT, NE])

        def f(eng):
            return eng.tensor_reduce(out=S.M1[:, :], in_=S.LG[:, :, :], axis=AX.X, op=ALU.max)
        self.add("dve", f, reads=allg, writes=[("m1",)])

        def f(eng):
            return eng.tensor_tensor(out=S.EQ1[:, :, :], in0=S.LG[:, :, :], in1=bc(S.M1[:, :]), op=ALU.is_equal)
        self.add("dve", f, reads=allg + [("m1",)], writes=[("eq1",)])

        def f(eng):
            return eng.scalar_tensor_tensor(out=S.L2[:, :, :], in0=S.EQ1[:, :, :], scalar=-1e30, in1=S.LG[:, :, :],
                                            op0=ALU.mult, op1=ALU.add)
        self.add("dve", f, reads=allg + [("eq1",)], writes=[("l2",)])

        def f(eng):
            return eng.tensor_reduce(out=S.M2[:, :], in_=S.L2[:, :, :], axis=AX.X, op=ALU.max)
        self.add("dve", f, reads=[("l2",)], writes=[("m2",)])

        def f(eng):
            return eng.tensor_tensor(out=S.EQ2[:, :, :], in0=S.L2[:, :, :], in1=bc(S.M2[:, :]), op=ALU.is_equal)
        self.add("dve", f, reads=[("l2",), ("m2",)], writes=[("eq2",)])

        def f(eng):
            return eng.tensor_tensor(out=S.M2[:, :], in0=S.M1[:, :], in1=S.M2[:, :], op=ALU.subtract)
        self.add("dve", f, reads=[("m1",), ("m2",), ("eq2",)], writes=[("dd",)])

        def f(eng):
            eng.activation(out=S.P1[:, :], in_=S.M2[:, :], func=AF.Sigmoid)
            return eng.activation(out=S.P2[:, :], in_=S.M2[:, :], func=AF.Sigmoid, scale=-1.0)
        self.add("act", f, reads=[("dd",)], writes=[("p12",)])

        def f(eng):
            return eng.tensor_tensor(out=S.MSKB[:, :, :], in0=S.EQ1[:, :, :], in1=S.EQ2[:, :, :], op=ALU.add)
        self.add("dve", f, reads=[("eq1",), ("eq2",), ("hdz",)], writes=[("mskb",)])
        ba = self.bank()
        bc_ = self.bank()
        mflat = S.MSKB[:, :, :].rearrange("p t e -> p (t e)")

        def f(eng, ba=ba, bc_=bc_):
            eng.matmul(S.PS[ba][:, 0:NTT * NE], lhsT=S.UTB[:, :], rhs=mflat, start=True, stop=True)
            return eng.matmul(S.PS[bc_][:, 0:NTT * NE], lhsT=S.ONESB[:, :], rhs=mflat, start=True, stop=True)
        self.add("pe", f, reads=[("mskb",), ("utb",), ("ones",)], writes=[("ps", ba), ("ps", bc_)])

        def v3(ap):
            return ap.rearrange("p (t e) -> p t e", e=NE)

        def f(eng, bc_=bc_):
            return eng.tensor_copy(out=S.CNT[:, :, :], in_=v3(S.PS[bc_][:, 0:NTT * NE]))
        self.add("dve", f, reads=[("ps", bc_)], writes=[("cntb",)])
        bufs = [(S.CNT, "cntb"), (S.OFF, "offb"), (S.TMP, "tmpb"), (S.OFF, "offb"), (S.TMP, "tmpb")]
        for k, sh in enumerate((1, 2, 4, 8)):
            (src, sn), (dst, dn) = bufs[k], bufs[k + 1]

            def f(eng, src=src, dst=dst, sh=sh):
                eng.tensor_copy(out=dst[:, 0:sh, :], in_=src[:, 0:sh, :])
                return eng.tensor_tensor(out=dst[:, sh:NTT, :], in0=src[:, sh:NTT, :], in1=src[:, 0:NTT - sh, :], op=ALU.add)
            self.add("dve", f, reads=[(sn,)], writes=[(dn,)])

        def f(eng):
            return eng.tensor_copy(out=S.CNTI[:, :], in_=S.TMP[:, NTT - 1, :])
        self.add("dve", f, reads=[("tmpb",)], writes=[("cnti0",)])

        def f(eng):
            return eng.tensor_reduce(out=S.CMAXF[:, :], in_=S.TMP[:, NTT - 1, :], axis=AX.X, op=ALU.max)
        self.add("dve", f, reads=[("tmpb",)], writes=[("cmaxf",)])

        def f(eng):
            return eng.tensor_copy(out=S.CMAXI[:, :], in_=S.CMAXF[:, :])
        self.cnti_op = self.add("dve", f, reads=[("cmaxf",), ("cnti0",)], writes=[("cnti",)])

        def f(eng):
            return eng.tensor_tensor(out=S.OFF[:, :, :], in0=S.TMP[:, :, :], in1=S.CNT[:, :, :], op=ALU.subtract)
        self.add("dve", f, reads=[("tmpb",), ("cntb",)], writes=[("offb",)])

        def f(eng):
            return eng.tensor_tensor(out=S.CNT[:, :, :], in0=S.OFF[:, :, :],
                                     in1=S.PRM[:, P_ECAP:P_ECAP + NE].unsqueeze(1).to_broadcast([128, NTT, NE]),
                                     op=ALU.add)
        self.add("dve", f, reads=[("offb",), ("prm",)], writes=[("cntb",)])

        def f(eng, ba=ba):
            return eng.tensor_tensor(out=S.POS[:, :, :], in0=v3(S.PS[ba][:, 0:NTT * NE]), in1=S.CNT[:, :, :], op=ALU.add)
        self.add("dve", f, reads=[("ps", ba), ("cntb",)], writes=[("pos",)])

        def f(eng):
            return eng.tensor_tensor(out=S.TMP[:, :, :], in0=S.EQ1[:, :, :], in1=S.POS[:, :, :], op=ALU.mult)
        self.add("dve", f, reads=[("pos",), ("eq1",), ("cnti",)], writes=[("tmpb",)])

        def f(eng):
            return eng.tensor_reduce(out=S.D1[:, :], in_=S.TMP[:, :, :], axis=AX.X, op=ALU.add)
        self.add("dve", f, reads=[("tmpb",)], writes=[("d1",)])

        def f(eng):
            return eng.tensor_tensor(out=S.OFF[:, :, :], in0=S.EQ2[:, :, :], in1=S.POS[:, :, :], op=ALU.mult)
        self.add("dve", f, reads=[("pos",), ("eq2",)], writes=[("offb",)])

        def f(eng):
            return eng.tensor_reduce(out=S.D2[:, :], in_=S.OFF[:, :, :], axis=AX.X, op=ALU.add)
        self.add("dve", f, reads=[("offb",)], writes=[("d2",)])

        def f(eng):
            return eng.tensor_copy(out=S.DI1[:, :], in_=S.D1[:, :])
        self.add("dve", f, reads=[("d1",)], writes=[("di1",)])

        def f(eng):
            return eng.tensor_copy(out=S.DI2[:, :], in_=S.D2[:, :])
        self.add("dve", f, reads=[("d2",)], writes=[("di",)])
        if self.debug:
            def f(eng, sem):
                eng.dma_start(out=S.DBG[:, 0:16], in_=S.DI1[:, :]).then_inc(sem, 16)
                eng.dma_start(out=S.DBG[:, 16:32], in_=S.DI2[:, :]).then_inc(sem, 16)
                return eng.dma_start(out=S.DBG[:, 32:40], in_=S.CNTI[:, :]).then_inc(sem, 16)
            self.add("sp", f, reads=[("di",), ("di1",), ("cnti",)], writes=[("dbg",)], dma=("dbg",), ndma=3)

    def st_scatter(self):
        S = self
        for tt in range(NTT):
            t0 = HALO + tt * 128
            bs = [self.bank(), self.bank()]

            def f(eng, t0=t0, bs=bs):
                last = None
                for kc in range(NCH):
                    last = eng.matmul(S.PS[bs[kc // 4]][:, (kc % 4) * 128:(kc % 4 + 1) * 128],
                                      lhsT=S.H2[:, kc, t0:t0 + 128], rhs=S.IDB[:, :], start=True, stop=True)
                return last
            rd = [("idb",)]
            for kc in range(NCH):
                rd += U("H2", kc, t0, t0 + 128)
            self.add("pe", f, reads=rd, writes=[("ps", bs[0]), ("ps", bs[1])])
            i = self.ht()

            def f(eng, i=i, b=bs[0]):
                return eng.activation(out=S.HT[i][:, 0:512], in_=S.PS[b][:, 0:512], func=AF.Copy)
            self.add("act", f, reads=[("ps", bs[0])], writes=[("ht", i, 0)])

            def f(eng, i=i, b=bs[1]):
                return eng.tensor_copy(out=S.HT[i][:, 512:1024], in_=S.PS[b][:, 0:512])
            self.add("dve", f, reads=[("ps", bs[1])], writes=[("ht", i, 1)])

            def f(eng, sem, i=i, tt=tt):
                eng.indirect_dma_start(out=S.HD[:, :], out_offset=bass.IndirectOffsetOnAxis(ap=S.DI1[:, tt:tt + 1], axis=0),
                                       in_=S.HT[i], in_offset=None, bounds_check=S.rb_pool,
                                       oob_is_err=False).then_inc(sem, 16)
                return eng.indirect_dma_start(out=S.HD[:, :],
                                              out_offset=bass.IndirectOffsetOnAxis(ap=S.DI2[:, tt:tt + 1], axis=0),
                                              in_=S.HT[i], in_offset=None, bounds_check=S.rb_pool,
                                              oob_is_err=False).then_inc(sem, 16)
            self.add("pool", f, reads=[("ht", i, 0), ("ht", i, 1), ("di",), ("di1",), ("hdz",)], writes=[("hd", tt)],
                     dma=("scat", tt % 4), ndma=2)

    def pass_load(self, e, r):
        S = self
        row0 = e * CAP + r * BLK

        nfull = BLK // 128
        rem = BLK - nfull * 128

        def f(eng, sem, row0=row0):
            last = eng.dma_start(out=S.HGS[:, 0:nfull, :],
                                 in_=S.HD[row0:row0 + nfull * 128, :].rearrange("(s p) d -> p s d", p=128)).then_inc(sem, 16)
            if rem:
                last = eng.dma_start(out=S.HGS[0:rem, nfull, :],
                                     in_=S.HD[row0 + nfull * 128:row0 + BLK, :]).then_inc(sem, 16)
            return last
        self.add("sp", f, reads=[("hdz",)] + [("hd", tt) for tt in range(NTT)], writes=[("hgs",)], dma=("hgl",),
                 ndma=2 if rem else 1)

    def st_pass(self, e, r, yi, prefetch=None):
        S = self
        row0 = e * CAP + r * BLK
        if prefetch is None or not prefetch[0]:
            self.pass_load(e, r)
        alt = 0
        for kc in range(NCH):
            for (c0, n) in CB:
                b = self.bank()

                def f(eng, kc=kc, c0=c0, n=n, b=b):
                    last = None
                    for (sl, rows) in STL:
                        if not (c0 <= sl * 128 < c0 + n):
                            continue
                        o = sl * 128 - c0
                        last = eng.matmul(S.PS[b][:, o:o + rows], lhsT=S.HGS[0:rows, sl, kc * 128:(kc + 1) * 128],
                                          rhs=S.IDB[0:rows, 0:rows], start=True, stop=True)
                    return last
                self.add("pe", f, reads=[("hgs",), ("idb",)], writes=[("ps", b)])
                if alt % 2 == 0:
                    def f(eng, kc=kc, c0=c0, n=n, b=b):
                        return eng.activation(out=S.HG[:, kc, c0:c0 + n], in_=S.PS[b][:, 0:n], func=AF.Copy)
                    self.add("act", f, reads=[("ps", b)], writes=U("HG", kc, c0, c0 + n))
                else:
                    def f(eng, kc=kc, c0=c0, n=n, b=b):
                        return eng.tensor_copy(out=S.HG[:, kc, c0:c0 + n], in_=S.PS[b][:, 0:n])
                    self.add("dve", f, reads=[("ps", b)], writes=U("HG", kc, c0, c0 + n))
                alt += 1
        if prefetch is not None and prefetch[1] is not None:
            self.pass_load(prefetch[1], 0)
        w1, w3, w2 = self.moe_w1[0, e], self.moe_w3[0, e], self.moe_w2[0, e]
        Y = S.YACC[yi]
        for fg in range(NFG):
            k1, s1 = self.wget(self.ld_cols(w1, fg * 512))
            k3, s3 = self.wget(self.ld_cols(w3, fg * 512))
            k2, s2 = self.wget(self.ld_rows(w2, fg * 512))
            for (c0, n) in CB:
                for j in range(4):
                    ba = self.bank()
                    self.proj(s1, k1, j, S.HG, "HG", c0, n, ba)
                    bc = self.bank()
                    self.proj(s3, k3, j, S.HG, "HG", c0, n, bc)
                    t = self.tf()

                    def f(eng, t=t, n=n, ba=ba):
                        return eng.activation(out=S.TF[t][:, 0:n], in_=S.PS[ba][:, 0:n], func=AF.Silu)
                    self.add("act", f, reads=[("ps", ba)], writes=[("tf", t)])

                    def f(eng, t=t, n=n, bc=bc, j=j, c0=c0):
                        return eng.tensor_tensor(out=S.GSR[:, j, c0:c0 + n], in0=S.PS[bc][:, 0:n],
                                                 in1=S.TF[t][:, 0:n], op=ALU.mult)
                    self.add("dve", f, reads=[("ps", bc), ("tf", t)], writes=U("GSR", j, c0, c0 + n))
            self.wrel(k1)
            self.wrel(k3)
            w2v = s2[:, :].rearrange("p (f d) -> p f d", f=4)
            for (sl, rows) in STL:
                for hf in range(2):
                    b = self.bank()
                    pairs = [(S.GSR[:, j, sl * 128:sl * 128 + rows], w2v[:, j, hf * 512:(hf + 1) * 512]) for j in range(4)]
                    rd = [("w", self.slot_plan[k2])]
                    for j in range(4):
                        rd += U("GSR", j, sl * 128, sl * 128 + rows)
                    self.mm_group(b, 512, pairs, rd)
                    yres = ("yacc", yi, sl, hf)
                    if fg == 0:
                        def f(eng, sl=sl, hf=hf, b=b, Y=Y, rows=rows):
                            return eng.activation(out=Y[0:rows, sl, hf * 512:(hf + 1) * 512], in_=S.PS[b][0:rows, 0:512],
                                                  func=AF.Copy)
                        self.add("act", f, reads=[("ps", b)], writes=[yres])
                    else:
                        def f(eng, sl=sl, hf=hf, b=b, Y=Y, rows=rows):
                            return eng.tensor_tensor(out=Y[0:rows, sl, hf * 512:(hf + 1) * 512],
                                                     in0=Y[0:rows, sl, hf * 512:(hf + 1) * 512], in1=S.PS[b][0:rows, 0:512],
                                                     op=ALU.add)
                        self.add("dve", f, reads=[("ps", b), yres], writes=[yres])
            self.wrel(k2)

        nfull = BLK // 128
        rem = BLK - nfull * 128

        def f(eng, sem, row0=row0, Y=Y):
            last = eng.dma_start(out=S.YD[row0:row0 + nfull * 128, :].rearrange("(s p) d -> p s d", p=128),
                                 in_=Y[:, 0:nfull, :]).then_inc(sem, 16)
            if rem:
                last = eng.dma_start(out=S.YD[row0 + nfull * 128:row0 + BLK, :], in_=Y[0:rem, nfull, :]).then_inc(sem, 16)
            return last
        self.add("sp", f, reads=[("yacc", yi, sl, hf) for sl in range(NSLT) for hf in range(2)], writes=[("yd",)],
                 dma=("yst",), ndma=2 if rem else 1)

    def st_combine(self):
        S = self
        for tt in range(NTT):
            t0 = HALO + tt * 128
            g1 = self.gt()
            g2 = self.gt()

            def f(eng, sem, g1=g1, g2=g2, tt=tt):
                eng.indirect_dma_start(out=S.GT[g1], out_offset=None, in_=S.YD[:, :],
                                       in_offset=bass.IndirectOffsetOnAxis(ap=S.DI1[:, tt:tt + 1], axis=0),
                                       bounds_check=S.rb_pool, oob_is_err=False).then_inc(sem, 16)
                return eng.indirect_dma_start(out=S.GT[g2], out_offset=None, in_=S.YD[:, :],
                                              in_offset=bass.IndirectOffsetOnAxis(ap=S.DI2[:, tt:tt + 1], axis=0),
                                              bounds_check=S.rb_pool, oob_is_err=False).then_inc(sem, 16)
            self.add("pool", f, reads=[("yd",), ("di",), ("di1",)], writes=[("gt", g1), ("gt", g2)], dma=("gath", tt % 4),
                     ndma=2)

            def f(eng, g1=g1, tt=tt):
                return eng.activation(out=S.GT[g1], in_=S.GT[g1], func=AF.Copy, scale=S.P1[:, tt:tt + 1])
            self.add("act", f, reads=[("gt", g1), ("p12",)], writes=[("gt", g1)])

            def f(eng, g1=g1, g2=g2, tt=tt):
                return eng.scalar_tensor_tensor(out=S.GT[g1], in0=S.GT[g2], scalar=S.P2[:, tt:tt + 1], in1=S.GT[g1],
                                                op0=ALU.mult, op1=ALU.add)
            self.add("dve", f, reads=[("gt", g1), ("gt", g2), ("p12",)], writes=[("gt", g1)])
            for hf in range(2):
                b = self.bank()

                def f(eng, g1=g1, hf=hf, b=b):
                    last = None
                    for q in range(4):
                        kc = hf * 4 + q
                        last = eng.transpose(out=S.PS[b][:, q * 128:(q + 1) * 128], in_=S.GT[g1][:, kc * 128:(kc + 1) * 128],
                                             identity=S.IDF[:, :])
                    return last
                self.add("pe", f, reads=[("gt", g1), ("idf",)], writes=[("ps", b)])

                def f(eng, hf=hf, b=b, t0=t0):
                    return eng.tensor_tensor(out=S.X[:, hf * 4:(hf + 1) * 4, t0:t0 + 128],
                                             in0=S.X[:, hf * 4:(hf + 1) * 4, t0:t0 + 128],
                                             in1=S.PS[b][:, 0:512].rearrange("p (q t) -> p q t", q=4), op=ALU.add)
                xr = []
                for kc in range(hf * 4, hf * 4 + 4):
                    xr += U("X", kc, t0, t0 + 128)
                self.add("dve", f, reads=[("ps", b)] + xr, writes=xr)
            if tt % 4 == 3:
                self.st_final(True, only=tt // 4, finish=(tt == NTT - 1))

    def st_final(self, norm, only=None, finish=True):
        S = self
        mains = [(HALO + i * 512, 512) for i in range(4)]
        ov = self.outT.rearrange("(c p) t -> p c t", p=128)
        for bi, (c0, n) in enumerate(mains):
            if only is not None and bi != only:
                continue
            if norm:
                bk = self.bank()
                for c in range(NCH):
                    t = self.tb()

                    def f(eng, c=c, t=t, c0=c0, n=n):
                        return eng.activation(out=S.TB[t][:, 0:n], in_=S.X[:, c, c0:c0 + n], func=AF.Square)
                    self.add("act", f, reads=U("X", c, c0, c0 + n), writes=[("tb", t)])

                    def f(eng, c=c, t=t, n=n, bk=bk):
                        return eng.matmul(S.PS[bk][:, 0:n], lhsT=S.ONESB[:, :], rhs=S.TB[t][:, 0:n],
                                          start=(c == 0), stop=(c == NCH - 1))
                    self.add("pe", f, reads=[("tb", t), ("ones",)], writes=[("ps", bk)])
                t1 = self.tf()

                def f(eng, t1=t1, n=n, bk=bk):
                    return eng.activation(out=S.TF[t1][:, 0:n], in_=S.PS[bk][:, 0:n], func=AF.Sqrt,
                                          scale=1.0 / D, bias=S.EPSR[:, 0:1])
                self.add("act", f, reads=[("ps", bk), ("eps",)], writes=[("tf", t1)])

                def f(eng, t1=t1, n=n):
                    return eng.reciprocal(out=S.RSTD[:, 0:n], in_=S.TF[t1][:, 0:n])
                self.add("dve", f, reads=[("tf", t1)], writes=[("rstd",)])
                for c in range(NCH):
                    def f(eng, c=c, c0=c0, n=n):
                        return eng.scalar_tensor_tensor(out=S.X[:, c, c0:c0 + n], in0=S.X[:, c, c0:c0 + n],
                                                        scalar=S.PRM[:, P_FIN + c:P_FIN + c + 1],
                                                        in1=S.RSTD[:, 0:n], op0=ALU.mult, op1=ALU.mult)
                    self.add("dve", f, reads=U("X", c, c0, c0 + n) + [("rstd",), ("prm",)],
                             writes=U("X", c, c0, c0 + n))
            rd = []
            for c in range(NCH):
                rd += U("X", c, c0, c0 + n)

            def f(eng, sem, c0=c0, n=n):
                return eng.dma_start(out=ov[:, :, c0 - HALO:c0 - HALO + n], in_=S.X[:, :, c0:c0 + n]).then_inc(sem, 16)
            self.add("sp", f, reads=rd, writes=[("out", bi)], dma=("out", bi))
        if not finish:
            return

        def f(eng):
            return None
        self.add("sp", f, reads=[("out", i) for i in range(4)], writes=[("done",)])

    def program(self):
        S = self
        mains = [(HALO + i * 512, 512) for i in range(4)]
        self.st_init()
        order = ["mix0", "l0", "mix1", "full"]
        lim = order.index(self.stop)
        self.ring_n = NS - 1
        self.mg_live = True
        self.st_mixer_half(0, 1, 32, 64)
        self.st_mixer_half(0, 0, 0, 32)
        self.mg_live = False
        if lim >= 1:
            if not self.dry:
                self.S.barrier()
            fb = [(32, 32)] + mains
            self.ring_n = NS
            self.st_norm(fb, 0, S.H2, "H2", P_NFG)
            self.st_ffn(0, fb, [(self.dense_w1[0], self.dense_w3[0], self.dense_w2[0])])
        if lim >= 2:
            if not self.dry:
                self.S.barrier()
            self.ring_n = NS - 1
            self.mg_live = True
            self.st_mixer_half(1, 1, 32, 64)
            self.st_mixer_half(1, 0, 32, 64)
            self.mg_live = False
        if lim >= 3:
            if not self.dry:
                self.S.barrier()
            self.ring_n = NS
            self.st_norm(mains, 0, S.H2, "H2", P_LAYER + P_NFG, router=True)
            self.st_route()
            self.st_scatter()
            if not self.dry:
                self.S.barrier()
            for e in range(NE):
                self.st_pass(e, 0, e % 2, prefetch=(e > 0, e + 1 if e + 1 < NE else None))
            yi = 0
            for e in range(NE):
                for r in range(1, NRND):
                    self.cur_region = (e, r)
                    self.st_pass(e, r, yi)
                    yi ^= 1
                    self.cur_region = None
            if not self.dry:
                self.S.barrier()
            self.st_combine()
        else:
            self.st_final(norm=False)

    def build(self):
        nc = self.nc
        self.declare()
        self.EPSR = self.es.enter_context(nc.sbuf_tensor("epsr", [128, 1], F32))
        self.EPSL = self.es.enter_context(nc.sbuf_tensor("epsl", [128, 1], F32))
        self.dry = True
        self.slot_plan = []
        self.rr = 0
        self.ring_n = NS
        self.reset_rot()
        self.program()
        last = {}
        self.prev_same = []
        for j, sl in enumerate(self.slot_plan):
            self.prev_same.append(last.get(sl))
            last[sl] = j
        self.dry = False
        self.reset_rot()
        S = self

        def f(eng):
            eng.memset(S.EPSR[:, :], RMS_EPS)
            return eng.memset(S.EPSL[:, :], LN_EPS)
        self.add("dve", f, writes=[("eps",)])
        self.program()
        assert self.wk == len(self.plan)
        self.emit()
        return nc

    def emit(self):
        nc = self.nc
        ops = self.S.ops
        es = self.es
        eng_sem = {}
        for e in Sched.COMPUTE:
            eng_sem[e] = es.enter_context(nc.semaphore(f"s_{e}"))
        dma_sem = {}
        for op in ops:
            if op.dma is not None and op.dma not in dma_sem:
                dma_sem[op.dma] = es.enter_context(nc.semaphore("d_" + "_".join(str(x) for x in op.dma)))
        if getattr(self, "cnti_op", None) is not None:
            ops[self.cnti_op].signals = True
        cnt = {e: 0 for e in Sched.COMPUTE}
        for op in ops:
            if op.dma is not None:
                op.sem = dma_sem[op.dma]
            elif op.eng in cnt:
                if op.signals:
                    cnt[op.eng] += 1
                    op.val = cnt[op.eng]
                op.sem = eng_sem[op.eng]
            else:
                assert not op.signals, "queue-engine non-dma op cannot signal"
        by = {e: [] for e in ("pe", "act", "dve", "pool", "sp")}
        for op in ops:
            by[op.eng].append(op)
        block = es.enter_context(nc.Block())

        cnti_op = getattr(self, "cnti_op", None)
        S = self

        def emit_op(eng, op, known):
            for d in op.deps:
                Dp = ops[d]
                key = id(Dp.sem)
                if known.get(key, 0) >= Dp.val:
                    continue
                eng.wait_ge(Dp.sem, Dp.val)
                known[key] = Dp.val
            if op.dma is not None:
                op.fn(eng, op.sem)
            else:
                inst = op.fn(eng)
                if op.signals:
                    inst.then_inc(op.sem, 1)

        def emit_comp(eng, ename, grp):
            nsig = sum(1 for op in grp if op.dma is None and op.signals)
            if nsig:
                eng.drain().then_inc(eng_sem[ename], nsig)
            dk = {}
            for op in grp:
                if op.dma is not None:
                    dk.setdefault(op.dma, []).append(op)
            for kd, lst2 in dk.items():
                before = lst2[0].val - 16 * lst2[0].ndma
                if before > 0:
                    eng.wait_ge(dma_sem[kd], before)
                eng.sem_inc(dma_sem[kd], 16 * sum(o.ndma for o in lst2))

        def emit_region(eng, ename, grp, known, rc):
            e, r = grp[0].region
            eng.reg_load(rc, S.CNTI[0:1, e:e + 1])
            with eng.If_lt(rc, r * BLK + 1):
                emit_comp(eng, ename, grp)
            with eng.Else():
                k2 = dict(known)
                for op in grp:
                    emit_op(eng, op, k2)

        def emit_run(eng, ename, run_ops, known, rc):
            Dp = ops[cnti_op]
            key = id(Dp.sem)
            if known.get(key, 0) < Dp.val:
                eng.wait_ge(Dp.sem, Dp.val)
                known[key] = Dp.val
            eng.reg_load(rc, S.CMAXI[0:1, 0:1])
            with eng.If_lt(rc, BLK + 1):
                emit_comp(eng, ename, run_ops)
            with eng.Else():
                i = 0
                while i < len(run_ops):
                    j = i
                    while j < len(run_ops) and run_ops[j].region == run_ops[i].region:
                        j += 1
                    emit_region(eng, ename, run_ops[i:j], known, rc)
                    i = j

        def run(eng, lst, ename):
            known = {}
            with eng.register("rc_" + ename) as rc:
                i = 0
                while i < len(lst):
                    op = lst[i]
                    if op.region is None:
                        emit_op(eng, op, known)
                        i += 1
                        continue
                    j = i
                    while j < len(lst) and lst[j].region is not None:
                        j += 1
                    emit_run(eng, ename, lst[i:j], known, rc)
                    i = j

        @block.tensor
        def _(eng):
            run(eng, by["pe"], "pe")

        @block.scalar
        def _(eng):
            run(eng, by["act"], "act")

        @block.vector
        def _(eng):
            run(eng, by["dve"], "dve")

        @block.gpsimd
        def _(eng):
            with eng.register("rb_rows") as rb:
                eng.reg_mov(rb, NE * CAP - 1)
                S.rb_pool = rb
                run(eng, by["pool"], "pool")

        @block.sync
        def _(eng):
            run(eng, by["sp"], "sp")
        es.close()


def pack_params(inp, core):
    P = np.zeros((128, NPRM), np.float32)

    def vec(v):
        return np.ascontiguousarray(v.reshape(NCH, 128).T)
    for l in range(2):
        b = l * P_LAYER
        P[:, b + P_NMG:b + P_NMG + 8] = vec(inp["norm_mix_g"][l])
        P[:, b + P_SCW:b + P_SCW + 24] = inp["sc_conv_w"][l].reshape(3, NCH, 128).transpose(2, 1, 0).reshape(128, 24)
        P[:, b + P_CFW:b + P_CFW + 248] = inp["cf_conv_w"][l].reshape(31, NCH, 128).transpose(2, 1, 0).reshape(128, 248)
        P[:, b + P_CFB:b + P_CFB + 8] = vec(inp["cf_conv_b"][l])
        P[:, b + P_LNG:b + P_LNG + 8] = vec(inp["cf_ln_g"][l])
        P[:, b + P_LNB:b + P_LNB + 8] = vec(inp["cf_ln_b"][l])
        P[:, b + P_PSC:b + P_PSC + 8] = vec(inp["pool_scale"][l])
        P[:, b + P_NFG:b + P_NFG + 8] = vec(inp["norm_ffn_g"][l])
    P[:, P_FIN:P_FIN + 8] = vec(inp["norm_final_g"])
    P[:, P_ROUT:P_ROUT + 64] = inp["moe_router"][0].reshape(NCH, 128, NE).transpose(1, 0, 2).reshape(128, 64)
    first = (core % 4 == 0)
    P[:, P_MASK:P_MASK + 64] = 0.0 if first else 1.0
    for g, w in enumerate(WINS):
        for i in range(16):
            cntv = min(i + 1, w) if first else w
            P[:, P_INVC + g * 16 + i] = 1.0 / cntv
    P[:, P_ECAP:P_ECAP + NE] = (np.arange(NE, dtype=np.float32) * CAP)[None, :]
    P[:, P_UT:P_UT + 128] = np.triu(np.ones((128, 128), np.float32), 1)
    return P


_CACHE = {}


def run(inputs, stop="full"):
    inp = {k: np.asarray(v, dtype=np.float32) for k, v in inputs.items()}
    if stop not in _CACHE:
        _CACHE[stop] = Builder(stop).build()
    nc = _CACHE[stop]
    x = inp["x"]
    shared = {k: np.ascontiguousarray(inp[k]) for k in
              ("w_in", "w_sc_out", "w_cf_out", "w_pool_out", "w_o", "pool_w", "dense_w1", "dense_w3", "dense_w2",
               "moe_w1", "moe_w3", "moe_w2")}
    in_maps = []
    for core in range(8):
        b, q = divmod(core, 4)
        t0 = q * TOK
        xs = np.zeros((TT, D), np.float32)
        if q > 0:
            xs[:HALO] = x[b, t0 - HALO:t0]
        xs[HALO:] = x[b, t0:t0 + TOK]
        m = dict(shared)
        m["xT"] = np.ascontiguousarray(xs.T)
        m["prm"] = pack_params(inp, core)
        in_maps.append(m)
    res = run_bass_kernel_spmd(nc, in_maps, core_ids=list(range(8)))
    out = np.zeros((2, SEQ, D), np.float32)
    for core in range(8):
        b, q = divmod(core, 4)
        out[b, q * TOK:(q + 1) * TOK] = res.results[core]["outT"].T
    return out


def kernel(**inputs):
    return run(inputs, "full")
```
